# Optimizing a Trainium2 kernel written in Bass

```python
import jax
import jax.numpy as jnp
from jax import lax
import numpy as np

D_MODEL = 1024
BATCH = 8
SEQ = 2048
DEPTH = 2

GRID_W = 64
CTX_LEN = 256
EPS = 1e-6
N_MOD = 6

SSD_HEADS = 16
SSD_HEAD_DIM = 64
SSD_INNER = SSD_HEADS * SSD_HEAD_DIM
SSD_GROUPS = 4
SSD_STATE = 128
SSD_CONV = 5
SSD_CHUNK = 128
SSD_BC = SSD_GROUPS * SSD_STATE
SSD_CONV_DIM = SSD_INNER + 2 * SSD_BC
SSD_STATE_COLS = SSD_CONV_DIM + 2 * SSD_HEADS

CF_CH = 1024
CF_KERNEL = 31

AB_IN = SSD_INNER + SSD_STATE_COLS + 2 * CF_CH
AB_OUT = SSD_INNER + CF_CH

HG_HEADS = 8
HG_DK = 128
HG_DV = D_MODEL // HG_HEADS
HG_KEY = HG_HEADS * HG_DK
HG_VAL = HG_HEADS * HG_DV
HG_CHUNK = 64
HG_IN = HG_KEY + 2 * HG_VAL + 2 * HG_KEY

N_EXPERTS = 16
N_EXPERT_GROUPS = 4
EXPERTS_PER_GROUP = N_EXPERTS // N_EXPERT_GROUPS
TOP_K = 2
D_EXPERT = 512

N_EVEN = (DEPTH + 1) // 2
N_ODD = DEPTH // 2

kernel_name = "hybrid_ssd_conformer_hgrn2_moe_dit"


def rmsnorm(x, g):
    xf = x.astype(jnp.float32)
    y = xf * lax.rsqrt(jnp.mean(xf * xf, axis=-1, keepdims=True) + EPS)
    return (y * g.astype(jnp.float32)).astype(x.dtype)


def layernorm(x, g, b):
    xf = x.astype(jnp.float32)
    mu = jnp.mean(xf, axis=-1, keepdims=True)
    xc = xf - mu
    var = jnp.mean(xc * xc, axis=-1, keepdims=True)
    return (xc * lax.rsqrt(var + EPS) * g.astype(jnp.float32) + b.astype(jnp.float32)).astype(x.dtype)


def modulation(cond, w, b):
    m = jax.nn.silu(cond) @ w + b
    return m.reshape(cond.shape[:-1] + (N_MOD, D_MODEL))


def dwconv(x, w, b):
    k = w.shape[0]
    y = lax.conv_general_dilated(x, w[:, None, :].astype(x.dtype), window_strides=(1,),
                                 padding=[(k // 2, k // 2)], dimension_numbers=("NWC", "WIO", "NWC"),
                                 feature_group_count=x.shape[-1])
    return y + b.astype(y.dtype)


def _flip(t):
    return jnp.flip(t, axis=1)


def _same(t):
    return t


def to_col_major(t, rows):
    b, _, ch = t.shape
    return t.reshape(b, rows, GRID_W, ch).transpose(0, 2, 1, 3).reshape(b, rows * GRID_W, ch)


def from_col_major(t, rows):
    b, _, ch = t.shape
    return t.reshape(b, GRID_W, rows, ch).transpose(0, 2, 1, 3).reshape(b, rows * GRID_W, ch)


def _carry_step(s, inp):
    dec, st = inp
    return dec * s + st, s


def ssd_scan(xs, dt, a_neg, bm, cm, s0, with_output):
    bsz, seqlen, nh, hp = xs.shape
    ng, ns = bm.shape[2], bm.shape[3]
    rep = nh // ng
    nc = seqlen // SSD_CHUNK
    la = (dt * a_neg).reshape(bsz, nc, SSD_CHUNK, ng, rep)
    acum = jnp.cumsum(la, axis=2)
    xdt = (xs.astype(jnp.float32) * dt[..., None]).reshape(bsz, nc, SSD_CHUNK, ng, rep, hp)
    bc = bm.astype(jnp.float32).reshape(bsz, nc, SSD_CHUNK, ng, ns)
    to_end = jnp.exp(acum[:, :, -1:] - acum)
    chunk_states = jnp.einsum("bcsgn,bcsgr,bcsgrp->bcgrpn", bc, to_end, xdt).reshape(bsz, nc, nh, hp, ns)
    chunk_decay = jnp.exp(acum[:, :, -1]).reshape(bsz, nc, nh)[..., None, None]
    s_final, s_in = lax.scan(_carry_step, s0, (jnp.moveaxis(chunk_decay, 1, 0), jnp.moveaxis(chunk_states, 1, 0)))
    if not with_output:
        return None, s_final
    s_in = jnp.moveaxis(s_in, 0, 1).reshape(bsz, nc, ng, rep, hp, ns)
    cc = cm.astype(jnp.float32).reshape(bsz, nc, SSD_CHUNK, ng, ns)
    causal = jnp.tril(jnp.ones((SSD_CHUNK, SSD_CHUNK), dtype=bool))[:, :, None, None]
    seg = acum[:, :, :, None] - acum[:, :, None, :]
    decay = jnp.exp(jnp.where(causal, seg, -jnp.inf))
    cb = jnp.einsum("bclgn,bcsgn->bclsg", cc, bc)
    y_diag = jnp.einsum("bclsg,bclsgr,bcsgrp->bclgrp", cb, decay, xdt)
    y_off = jnp.einsum("bclgn,bcgrpn,bclgr->bclgrp", cc, s_in, jnp.exp(acum))
    return (y_diag + y_off).reshape(bsz, seqlen, nh, hp), s_final


def ssd_inputs(p_state, conv_w, conv_b, dt_bias):
    b, n, _ = p_state.shape
    xbc = jax.nn.silu(dwconv(p_state[..., :SSD_CONV_DIM], conv_w, conv_b))
    xs = xbc[..., :SSD_INNER].reshape(b, n, SSD_HEADS, SSD_HEAD_DIM)
    bm = xbc[..., SSD_INNER:SSD_INNER + SSD_BC].reshape(b, n, SSD_GROUPS, SSD_STATE)
    cm = xbc[..., SSD_INNER + SSD_BC:].reshape(b, n, SSD_GROUPS, SSD_STATE)
    dt_raw = p_state[..., SSD_CONV_DIM:].astype(jnp.float32).reshape(b, n, 2, SSD_HEADS)
    dt = jax.nn.softplus(dt_raw + dt_bias.astype(jnp.float32))
    return xs, bm, cm, dt


def ssd_readout(y, xs, z, d_skip, norm_g):
    b, n = y.shape[0], y.shape[1]
    y = y + d_skip.astype(jnp.float32)[:, None] * xs.astype(jnp.float32)
    y = y.reshape(b, n, SSD_INNER) * jax.nn.silu(z.astype(jnp.float32))
    return rmsnorm(y, norm_g)


def conformer_conv(v, gate, dw_w, dw_b, ln_g, ln_b, rows):
    u = v * jax.nn.sigmoid(gate)
    b, n, ch = u.shape
    if rows is not None:
        u = u.reshape(b * rows, GRID_W, ch)
    u = dwconv(u, dw_w, dw_b).reshape(b, n, ch)
    return jax.nn.silu(layernorm(u, ln_g, ln_b))


def even_mixer(hc, hl, w_in, conv_w, conv_b, dt_bias, a_log, d_skip, ssd_g,
               cf_w, cf_b, cf_lng, cf_lnb, w_out, rows, need_ctx):
    a_neg = -jnp.exp(a_log.astype(jnp.float32))
    s_lo, s_hi = SSD_INNER, SSD_INNER + SSD_STATE_COLS
    pl = hl @ w_in
    if need_ctx:
        pc = hc @ w_in
        pc_state = pc[..., s_lo:s_hi]
    else:
        pc_state = hc @ w_in[:, s_lo:s_hi]
    xs_l, b_l, c_l, dt_l = ssd_inputs(pl[..., s_lo:s_hi], conv_w, conv_b, dt_bias)
    xs_c, b_c, c_c, dt_c = ssd_inputs(pc_state, conv_w, conv_b, dt_bias)
    bsz = hl.shape[0]
    ys_c, ys_l = [], []
    for d in range(2):
        flip = _flip if d else _same
        s0 = jnp.zeros((bsz, SSD_HEADS, SSD_HEAD_DIM, SSD_STATE), jnp.float32)
        y_c, s_ctx = ssd_scan(flip(xs_c), flip(dt_c[:, :, d]), a_neg[d], flip(b_c), flip(c_c), s0, need_ctx)
        y_l, _ = ssd_scan(flip(xs_l), flip(dt_l[:, :, d]), a_neg[d], flip(b_l), flip(c_l), s_ctx, True)
        ys_l.append(flip(y_l))
        if need_ctx:
            ys_c.append(flip(y_c))
    cf0 = SSD_INNER + SSD_STATE_COLS
    o_ssd = ssd_readout(ys_l[0] + ys_l[1], xs_l, pl[..., :SSD_INNER], d_skip, ssd_g)
    o_cf = conformer_conv(pl[..., cf0:cf0 + CF_CH], pl[..., cf0 + CF_CH:], cf_w, cf_b, cf_lng, cf_lnb, rows)
    y_lat = (jnp.concatenate([o_ssd, o_cf.astype(o_ssd.dtype)], axis=-1) @ w_out).astype(hl.dtype)
    y_ctx = None
    if need_ctx:
        oc_ssd = ssd_readout(ys_c[0] + ys_c[1], xs_c, pc[..., :SSD_INNER], d_skip, ssd_g)
        oc_cf = conformer_conv(pc[..., cf0:cf0 + CF_CH], pc[..., cf0 + CF_CH:], cf_w, cf_b, cf_lng, cf_lnb, None)
        y_ctx = (jnp.concatenate([oc_ssd, oc_cf.astype(oc_ssd.dtype)], axis=-1) @ w_out).astype(hc.dtype)
    return y_ctx, y_lat


def hgrn2_scan(q, k, v, logf, s0, with_output):
    bsz, seqlen, nh, dk = k.shape
    dv = v.shape[-1]
    nc = seqlen // HG_CHUNK

    def chunks(t):
        return t.astype(jnp.float32).reshape(bsz, nc, HG_CHUNK, nh, t.shape[-1])

    kc, vc = chunks(k), chunks(v)
    gcum = jnp.cumsum(chunks(logf), axis=2)
    g_end = gcum[:, :, -1]
    k_end = kc * jnp.exp(g_end[:, :, None] - gcum)
    chunk_states = jnp.einsum("bcshk,bcshv->bchkv", k_end, vc)
    s_final, s_in = lax.scan(_carry_step, s0, (jnp.moveaxis(jnp.exp(g_end)[..., None], 1, 0),
                                               jnp.moveaxis(chunk_states, 1, 0)))
    if not with_output:
        return None, s_final
    s_in = jnp.moveaxis(s_in, 0, 1)
    qc = chunks(q)
    g_mid = gcum[:, :, HG_CHUNK // 2 - 1:HG_CHUNK // 2]
    q_rel = qc * jnp.exp(gcum - g_mid)
    k_rel = kc * jnp.exp(g_mid - gcum)
    causal = jnp.tril(jnp.ones((HG_CHUNK, HG_CHUNK), dtype=bool))
    att = jnp.where(causal, jnp.einsum("bclhk,bcshk->bchls", q_rel, k_rel), 0.0)
    o_intra = jnp.einsum("bchls,bcshv->bclhv", att, vc)
    o_inter = jnp.einsum("bclhk,bchkv->bclhv", qc * jnp.exp(gcum), s_in)
    return (o_intra + o_inter).reshape(bsz, seqlen, nh, dv), s_final


def hgrn2_gates(p_state, lb):
    b, n, _ = p_state.shape
    v = p_state[..., :HG_VAL].reshape(b, n, HG_HEADS, HG_DV)
    f_raw = p_state[..., HG_VAL:].astype(jnp.float32).reshape(b, n, 2, HG_HEADS, HG_DK)
    lbh = lb.reshape(HG_HEADS, HG_DK)
    f = lbh + (1.0 - lbh) * jax.nn.sigmoid(f_raw)
    return v, 1.0 - f, jnp.log(f)


def hgrn2_readout(o, g, norm_g):
    b, n = o.shape[0], o.shape[1]
    return rmsnorm(o, norm_g).reshape(b, n, HG_VAL) * jax.nn.silu(g.astype(jnp.float32))


def odd_mixer(hc, hl, w_in, lb, norm_g, w_out, rows, need_ctx):
    st0 = HG_KEY + HG_VAL
    bsz, n_lat = hl.shape[0], hl.shape[1]
    pl = to_col_major(hl, rows) @ w_in
    q_l = jax.nn.silu(pl[..., :HG_KEY]).reshape(bsz, n_lat, HG_HEADS, HG_DK)
    v_l, k_l, lf_l = hgrn2_gates(pl[..., st0:], lb)
    if need_ctx:
        pc = hc @ w_in
        q_c = jax.nn.silu(pc[..., :HG_KEY]).reshape(bsz, hc.shape[1], HG_HEADS, HG_DK)
        pc_state = pc[..., st0:]
    else:
        pc_state = hc @ w_in[:, st0:]
    v_c, k_c, lf_c = hgrn2_gates(pc_state, lb)
    os_c, os_l = [], []
    for d in range(2):
        flip = _flip if d else _same
        s0 = jnp.zeros((bsz, HG_HEADS, HG_DK, HG_DV), jnp.float32)
        qc_d = flip(q_c) if need_ctx else None
        o_c, s_ctx = hgrn2_scan(qc_d, flip(k_c[:, :, d]), flip(v_c), flip(lf_c[:, :, d]), s0, need_ctx)
        o_l, _ = hgrn2_scan(flip(q_l), flip(k_l[:, :, d]), flip(v_l), flip(lf_l[:, :, d]), s_ctx, True)
        os_l.append(flip(o_l))
        if need_ctx:
            os_c.append(flip(o_c))
    y_lat = from_col_major(hgrn2_readout(os_l[0] + os_l[1], pl[..., HG_KEY:st0], norm_g) @ w_out, rows).astype(hl.dtype)
    y_ctx = None
    if need_ctx:
        y_ctx = (hgrn2_readout(os_c[0] + os_c[1], pc[..., HG_KEY:st0], norm_g) @ w_out).astype(hc.dtype)
    return y_ctx, y_lat


def moe(h, router_w, router_b, w_gate, w_up, w_down):
    shp = h.shape
    t = h.reshape(-1, D_MODEL)
    scores = jax.nn.sigmoid((t @ router_w).astype(jnp.float32))
    sel = scores + router_b.astype(jnp.float32)
    grouped = sel.reshape(-1, N_EXPERT_GROUPS, EXPERTS_PER_GROUP)
    group_score = lax.top_k(grouped, TOP_K)[0].sum(-1)
    g_idx = jnp.argmax(group_score, axis=-1)
    idx = jnp.broadcast_to(g_idx[:, None, None], (t.shape[0], 1, EXPERTS_PER_GROUP))
    in_group = jnp.take_along_axis(grouped, idx, axis=1)[:, 0]
    _, local = lax.top_k(in_group, TOP_K)
    expert_idx = g_idx[:, None] * EXPERTS_PER_GROUP + local
    wts = jnp.take_along_axis(scores, expert_idx, axis=1)
    wts = wts / jnp.sum(wts, axis=-1, keepdims=True)
    combine = jnp.sum(jax.nn.one_hot(expert_idx, N_EXPERTS, dtype=jnp.float32) * wts[..., None], axis=1)

    def expert(acc, p):
        wg, wu, wd, gate_col = p
        y = (jax.nn.silu(t @ wg) * (t @ wu)) @ wd
        return acc + gate_col[:, None] * y, None

    out, _ = lax.scan(expert, jnp.zeros(t.shape, jnp.float32), (w_gate, w_up, w_down, combine.T))
    return out.reshape(shp).astype(h.dtype)


def setup_inputs(seed: int = 0) -> dict:
    key = jax.random.key(seed)
    ks = jax.random.split(key, 32)
    f32 = jnp.float32

    def dense(k, shape, fan_in, scale=1.0):
        return scale * fan_in ** -0.5 * jax.random.normal(k, shape, f32)

    def gain(k, shape):
        return 1.0 + 0.1 * jax.random.normal(k, shape, f32)

    def small(k, shape, s=0.02):
        return s * jax.random.normal(k, shape, f32)

    dt0 = jnp.exp(jax.random.uniform(ks[14], (N_EVEN, 2, SSD_HEADS), f32, np.log(1e-3), np.log(1e-1)))
    return {
        "x": jax.random.normal(ks[0], (BATCH, SEQ, D_MODEL), f32),
        "c": jax.random.normal(ks[1], (BATCH, D_MODEL), f32),
        "ctx": jax.random.normal(ks[2], (BATCH, CTX_LEN, D_MODEL), f32),
        "c_ctx": jax.random.normal(ks[3], (D_MODEL,), f32),
        "mod_w": dense(ks[4], (DEPTH, D_MODEL, N_MOD * D_MODEL), D_MODEL, 0.5),
        "mod_b": small(ks[5], (DEPTH, N_MOD * D_MODEL)),
        "norm_mix_g": gain(ks[6], (DEPTH, D_MODEL)),
        "norm_ffn_g": gain(ks[7], (DEPTH, D_MODEL)),
        "router_w": dense(ks[8], (D_MODEL, N_EXPERTS), D_MODEL),
        "router_b": small(ks[9], (N_EXPERTS,), 0.01),
        "moe_w_gate": dense(ks[10], (DEPTH, N_EXPERTS, D_MODEL, D_EXPERT), D_MODEL),
        "moe_w_up": dense(ks[11], (DEPTH, N_EXPERTS, D_MODEL, D_EXPERT), D_MODEL),
        "moe_w_down": dense(ks[12], (DEPTH, N_EXPERTS, D_EXPERT, D_MODEL), D_EXPERT),
        "ab_w_in": dense(ks[13], (N_EVEN, D_MODEL, AB_IN), D_MODEL),
        "ssd_conv_w": dense(ks[15], (N_EVEN, SSD_CONV, SSD_CONV_DIM), SSD_CONV),
        "ssd_conv_b": small(ks[16], (N_EVEN, SSD_CONV_DIM)),
        "ssd_dt_bias": dt0 + jnp.log(-jnp.expm1(-dt0)),
        "ssd_a_log": jnp.log(jax.random.uniform(ks[17], (N_EVEN, 2, SSD_HEADS), f32, 1.0, 16.0)),
        "ssd_d": gain(ks[18], (N_EVEN, SSD_HEADS)),
        "ssd_norm_g": gain(ks[19], (N_EVEN, SSD_INNER)),
        "cf_dw_w": dense(ks[20], (N_EVEN, CF_KERNEL, CF_CH), CF_KERNEL),
        "cf_dw_b": small(ks[21], (N_EVEN, CF_CH)),
        "cf_ln_g": gain(ks[22], (N_EVEN, CF_CH)),
        "cf_ln_b": small(ks[23], (N_EVEN, CF_CH)),
        "ab_w_out": dense(ks[24], (N_EVEN, AB_OUT, D_MODEL), AB_OUT),
        "hg_w_in": dense(ks[25], (N_ODD, D_MODEL, HG_IN), D_MODEL),
        "hg_lb": small(ks[26], (DEPTH, HG_KEY), 0.1),
        "hg_norm_g": gain(ks[27], (N_ODD, HG_DV)),
        "hg_w_out": dense(ks[28], (N_ODD, HG_VAL, D_MODEL), HG_VAL),
        "final_norm_g": gain(ks[29], (D_MODEL,)),
    }


def reference(x, c, ctx, c_ctx, mod_w, mod_b, norm_mix_g, norm_ffn_g, router_w, router_b,
              moe_w_gate, moe_w_up, moe_w_down, ab_w_in, ssd_conv_w, ssd_conv_b, ssd_dt_bias,
              ssd_a_log, ssd_d, ssd_norm_g, cf_dw_w, cf_dw_b, cf_ln_g, cf_ln_b, ab_w_out,
              hg_w_in, hg_lb, hg_norm_g, hg_w_out, final_norm_g):
    rows = x.shape[1] // GRID_W
    lb_all = jnp.cumsum(jax.nn.softmax(hg_lb.astype(jnp.float32), axis=0), axis=0)
    lb_all = lb_all - lb_all[0]
    xl, xc = x, ctx
    for l in range(DEPTH):
        last = l == DEPTH - 1
        j = l // 2
        m_l = modulation(c, mod_w[l], mod_b[l])[:, :, None, :]
        m_c = modulation(c_ctx, mod_w[l], mod_b[l])
        hl = rmsnorm(xl, norm_mix_g[l]) * (1.0 + m_l[:, 1]) + m_l[:, 0]
        hc = rmsnorm(xc, norm_mix_g[l]) * (1.0 + m_c[1]) + m_c[0]
        if l % 2 == 0:
            y_c, y_l = even_mixer(hc, hl, ab_w_in[j], ssd_conv_w[j], ssd_conv_b[j], ssd_dt_bias[j],
                                  ssd_a_log[j], ssd_d[j], ssd_norm_g[j], cf_dw_w[j], cf_dw_b[j],
                                  cf_ln_g[j], cf_ln_b[j], ab_w_out[j], rows, not last)
        else:
            y_c, y_l = odd_mixer(hc, hl, hg_w_in[j], lb_all[l], hg_norm_g[j], hg_w_out[j], rows, not last)
        xl = xl + m_l[:, 2] * y_l
        hl = rmsnorm(xl, norm_ffn_g[l]) * (1.0 + m_l[:, 4]) + m_l[:, 3]
        xl = xl + m_l[:, 5] * moe(hl, router_w, router_b, moe_w_gate[l], moe_w_up[l], moe_w_down[l])
        if not last:
            xc = xc + m_c[2] * y_c
            hc = rmsnorm(xc, norm_ffn_g[l]) * (1.0 + m_c[4]) + m_c[3]
            xc = xc + m_c[5] * moe(hc, router_w, router_b, moe_w_gate[l], moe_w_up[l], moe_w_down[l])
    return rmsnorm(xl, final_norm_g)
```

```python
import numpy as np
from contextlib import ExitStack
import concourse.bass as bass
import concourse.mybir as mybir
from concourse.bass_utils import run_bass_kernel_spmd

F32 = mybir.dt.float32
BF16 = mybir.dt.bfloat16
AF = mybir.ActivationFunctionType
ALU = mybir.AluOpType
AX = mybir.AxisListType

D = 1024
TL = 2048
TC = 256
T = TL + TC
NT = T // 128
EPS = 1e-6
NEG = -30000.0


class Buf:
    __slots__ = ("name", "t", "writer", "readers", "ld", "st", "onchip", "excl")

    def __init__(self, name, t, onchip):
        self.name = name
        self.t = t
        self.writer = None
        self.readers = []
        self.ld = None
        self.st = None
        self.onchip = onchip
        self.excl = False

    def __getitem__(self, k):
        return self.t[k]


class FW:
    ENG = ("pe", "act", "dve", "pool", "sp")

    def __init__(self, nc, es, n_dma_sems=90):
        self.nc = nc
        self.es = es
        self.scopes = [es]
        self.eng = {"pe": nc.tensor, "act": nc.scalar, "dve": nc.vector, "pool": nc.gpsimd, "sp": nc.sync}
        self.sems = {}
        self.cnt = {}
        for e in self.ENG:
            self.sems[e] = es.enter_context(nc.semaphore("s_" + e))
            self.cnt[e] = 0
        self.free_dma = []
        for i in range(n_dma_sems):
            k = "d%d" % i
            self.sems[k] = es.enter_context(nc.semaphore("s_" + k))
            self.cnt[k] = 0
            self.free_dma.append(k)
        self.phase_dma = []
        self.seen = {e: {} for e in self.ENG}
        self.bufs = []
        self.n_inst = 0
        self.uid = 0
        self.log = {e: [] for e in self.ENG}

    def push(self):
        s = ExitStack()
        self.scopes.append(s)
        s._bufs0 = len(self.bufs)

    def pop(self):
        self.barrier()
        s = self.scopes.pop()
        del self.bufs[s._bufs0:]
        s.close()

    def _nm(self, name):
        self.uid += 1
        return "%s_%d" % (name, self.uid)

    def sbuf(self, name, shape, dtype):
        t = self.scopes[-1].enter_context(self.nc.sbuf_tensor(self._nm(name), list(shape), dtype))
        b = Buf(name, t, True)
        self.bufs.append(b)
        return b

    def psum(self, name, shape, dtype):
        t = self.scopes[-1].enter_context(self.nc.psum_tensor(self._nm(name), list(shape), dtype))
        b = Buf(name, t, True)
        b.excl = True
        self.bufs.append(b)
        return b

    def dram(self, name, t):
        b = Buf(name, t, False)
        self.bufs.append(b)
        return b

    def _need(self, e, tok, waits):
        if tok is None:
            return
        k, v = tok
        if self.seen[e].get(k, 0) >= v:
            return
        if waits.get(k, 0) < v:
            waits[k] = v

    def _deps(self, e, reads, writes, pe_accum=False, attach=False):
        waits = {}
        for b in reads:
            self._need(e, b.writer, waits)
            if b.excl:
                for r in b.readers:
                    if r[0] != e:
                        self._need(e, r, waits)
        for b in writes:
            if not (e == "pe" and b.writer is not None and b.writer[0] == "pe"):
                self._need(e, b.writer, waits)
            for r in b.readers:
                self._need(e, r, waits)
        items = list(waits.items())
        self.pending = None
        if attach and items:
            self.pending = items.pop()
        for k, v in items:
            self.eng[e].wait_ge(self.sems[k], v)
            self.seen[e][k] = v
            self.log[e].append(("w", k, v))
        if self.pending is not None:
            k, v = self.pending
            self.seen[e][k] = v
            self.log[e].append(("w", k, v))

    def op(self, e, fn, reads=(), writes=(), inc=True, pe_accum=False):
        self._deps(e, reads, writes, pe_accum, attach=True)
        ins = fn(self.eng[e])
        if self.pending is not None:
            ins._wait_ge(self.sems[self.pending[0]], self.pending[1])
        self.n_inst += 1
        tok = (e, self.cnt[e] + 1)
        if inc:
            self.cnt[e] += 1
            ins.then_inc(self.sems[e], 1)
            self.log[e].append(("i", e, 1))
        for b in reads:
            b.readers.append(tok)
            if len(b.readers) > 24:
                b.readers = _compact(b.readers)
        for b in writes:
            b.writer = tok
            b.readers = []
        return ins

    def _dma_sem(self):
        k = self.free_dma.pop()
        self.phase_dma.append(k)
        return k

    def dma(self, q, out_ap, in_ap, src, dst, **kw):
        self._deps(q, [src], [dst], attach=True)
        pend = self.pending
        if dst.onchip:
            if dst.ld is None:
                dst.ld = self._dma_sem()
            k = dst.ld
        else:
            if src.st is None:
                src.st = self._dma_sem()
            k = src.st
        self.cnt[k] += 16
        ins = self.eng[q].dma_start(out=out_ap, in_=in_ap, **kw)
        if pend is not None:
            ins._wait_ge(self.sems[pend[0]], pend[1])
        ins.then_inc(self.sems[k], 16)
        self.log[q].append(("i", k, 16))
        self.n_inst += 1
        tok = (k, self.cnt[k])
        src.readers.append(tok)
        if len(src.readers) > 24:
            src.readers = _compact(src.readers)
        if dst.onchip:
            dst.writer = tok
            dst.readers = []
        return ins

    def barrier(self):
        sp = self.eng["sp"]
        keys = [k for k in self.ENG if k != "sp"] + self.phase_dma
        for k in keys:
            v = self.cnt[k]
            if v > 0 and self.seen["sp"].get(k, 0) < v:
                sp.wait_ge(self.sems[k], v)
                self.seen["sp"][k] = v
                self.log["sp"].append(("w", k, v))
        self.cnt["sp"] += 1
        sp.nop().then_inc(self.sems["sp"], 1)
        self.log["sp"].append(("i", "sp", 1))
        v = self.cnt["sp"]
        for e in self.ENG:
            if e == "sp":
                continue
            self.log[e].append(("w", "sp", v))
            self.eng[e].wait_ge(self.sems["sp"], v)
            self.seen[e]["sp"] = v
            for k in keys:
                self.seen[e][k] = self.cnt[k]
        for b in self.bufs:
            b.writer = None
            b.readers = []
            b.ld = None
            b.st = None
        self.free_dma.extend(self.phase_dma)
        self.phase_dma = []


def simulate(fw):
    pos = {e: 0 for e in fw.ENG}
    val = {}
    prog = True
    while prog:
        prog = False
        for e in fw.ENG:
            L = fw.log[e]
            while pos[e] < len(L):
                kind, k, v = L[pos[e]]
                if kind == "w":
                    if val.get(k, 0) < v:
                        break
                else:
                    val[k] = val.get(k, 0) + v
                pos[e] += 1
                prog = True
    stuck = {e: (pos[e], len(fw.log[e]), fw.log[e][pos[e]], val.get(fw.log[e][pos[e]][1], 0))
             for e in fw.ENG if pos[e] < len(fw.log[e])}
    return stuck


def _compact(toks):
    m = {}
    for k, v in toks:
        if m.get(k, 0) < v:
            m[k] = v
    return list(m.items())


class KB:
    def __init__(self, nc, fw):
        self.nc = nc
        self.fw = fw
        self.banks = [fw.psum("bank%d" % i, [128, 512], F32) for i in range(8)]
        self.bi = 0
        self.dq = 0

    def bank(self):
        b = self.banks[self.bi]
        self.bi = (self.bi + 1) % 8
        return b

    def q(self):
        self.dq ^= 1
        return "sp" if self.dq else "act"

    def mm(self, out, lhsT, rhs, start, stop, reads, wr, inc=None):
        if inc is None:
            inc = stop
        return self.fw.op("pe", lambda e: e.matmul(out, lhsT, rhs, start=start, stop=stop),
                          reads=reads, writes=[wr], inc=inc, pe_accum=not start)

    def tr(self, out, in_, ident, reads, wr, inc=True, first=False):
        return self.fw.op("pe", lambda e: e.transpose(out, in_, ident), reads=reads, writes=[wr],
                          inc=inc, pe_accum=not first)

    def act(self, out, in_, func, reads, writes, **kw):
        return self.fw.op("act", lambda e: e.activation(out, in_, func, **kw), reads=reads, writes=writes)

    def tt(self, out, a, b, op, reads, writes, eng="dve"):
        return self.fw.op(eng, lambda e: e.tensor_tensor(out, a, b, op), reads=reads, writes=writes)

    def ts(self, out, a, s1, s2, op0, op1, reads, writes, eng="dve", **kw):
        return self.fw.op(eng, lambda e: e.tensor_scalar(out, a, s1, s2, op0, op1, **kw), reads=reads, writes=writes)

    def stt(self, out, a, s, b, op0, op1, reads, writes):
        return self.fw.op("dve", lambda e: e.scalar_tensor_tensor(out, a, s, b, op0, op1), reads=reads, writes=writes)

    def cp(self, out, a, reads, writes, eng="dve"):
        return self.fw.op(eng, lambda e: e.tensor_copy(out, a), reads=reads, writes=writes)

    def memset(self, ap, val, writes, eng="pool"):
        return self.fw.op(eng, lambda e: e.memset(ap, val), reads=[], writes=writes)


def bc(ap, shape):
    return ap.broadcast_to(list(shape))


VEC_SPEC = [("modb0", 48), ("modb1", 48), ("gmix0", 8), ("gmix1", 8), ("gffn0", 8), ("gffn1", 8),
            ("convw", 80), ("convb", 16), ("ssdg", 8), ("cfw", 248), ("cfb", 8), ("cflg", 8), ("cflb", 8),
            ("hglb0", 8), ("hglb1", 8)]
VOFF = {}
_o = 0
for _n, _c in VEC_SPEC:
    VOFF[_n] = _o
    _o += _c
NV = _o
ROW_SPEC = [("modb0", 6144), ("modb1", 6144), ("dtb", 32), ("alog", 32), ("ssdd", 16), ("rb", 16),
            ("fng", 1024), ("hgng", 128)]
ROFF = {}
_o = 0
for _n, _c in ROW_SPEC:
    ROFF[_n] = _o
    _o += _c
NR = _o
CONST_SPEC = [("ident", 128), ("triU", 128), ("triL", 128), ("negF", 128), ("negB", 128), ("ones", 128),
              ("mF64", 64), ("mB64", 64)]
COFF = {}
_o = 0
for _n, _c in CONST_SPEC:
    COFF[_n] = _o
    _o += _c
NCON = _o


def _col(v):
    v = np.asarray(v, np.float32).reshape(-1, 128)
    return np.ascontiguousarray(v.T)


def pack_shared(I):
    vec = np.zeros((128, NV), np.float32)

    def put(n, a):
        vec[:, VOFF[n]:VOFF[n] + a.shape[1]] = a
    put("modb0", _col(I["mod_b"][0])); put("modb1", _col(I["mod_b"][1]))
    put("gmix0", _col(I["norm_mix_g"][0])); put("gmix1", _col(I["norm_mix_g"][1]))
    put("gffn0", _col(I["norm_ffn_g"][0])); put("gffn1", _col(I["norm_ffn_g"][1]))
    cw = np.asarray(I["ssd_conv_w"][0], np.float32)
    put("convw", np.ascontiguousarray(cw.reshape(5, 16, 128).transpose(2, 1, 0)).reshape(128, 80))
    put("convb", _col(I["ssd_conv_b"][0]))
    put("ssdg", _col(I["ssd_norm_g"][0]))
    fw_ = np.asarray(I["cf_dw_w"][0], np.float32)
    put("cfw", np.ascontiguousarray(fw_.reshape(31, 8, 128).transpose(2, 1, 0)).reshape(128, 248))
    put("cfb", _col(I["cf_dw_b"][0])); put("cflg", _col(I["cf_ln_g"][0])); put("cflb", _col(I["cf_ln_b"][0]))
    put("hglb0", _col(I["hg_lb"][0])); put("hglb1", _col(I["hg_lb"][1]))
    row = np.zeros((1, NR), np.float32)

    def putr(n, a):
        a = np.asarray(a, np.float32).reshape(-1)
        row[0, ROFF[n]:ROFF[n] + a.size] = a
    putr("modb0", I["mod_b"][0]); putr("modb1", I["mod_b"][1]); putr("dtb", I["ssd_dt_bias"][0])
    putr("alog", I["ssd_a_log"][0]); putr("ssdd", I["ssd_d"][0]); putr("rb", I["router_b"])
    putr("fng", I["final_norm_g"]); putr("hgng", I["hg_norm_g"][0])
    con = np.zeros((128, NCON), np.float32)
    i = np.arange(128)
    k, l = i[:, None], i[None, :]

    def putc(n, a):
        con[:a.shape[0], COFF[n]:COFF[n] + a.shape[1]] = a
    putc("ident", (k == l).astype(np.float32))
    putc("triU", (k <= l).astype(np.float32))
    putc("triL", (k >= l).astype(np.float32))
    putc("negF", np.where(k <= l, 0.0, NEG).astype(np.float32))
    putc("negB", np.where(k >= l, 0.0, NEG).astype(np.float32))
    putc("ones", np.ones((128, 128), np.float32))
    putc("mF64", (k[:64] <= l[:, :64]).astype(np.float32))
    putc("mB64", (k[:64] >= l[:, :64]).astype(np.float32))
    return vec, row, con


class Prog:
    def __init__(self, dbg=()):
        self.dbg = set(dbg)
        nc = self.nc = bass.Bass("TRN2", target_bir_lowering=False)
        self.es = ExitStack()
        self.fw = FW(nc, self.es)
        self.k = KB(nc, self.fw)
        self.dr = {}

    def inp(self, name, shape, dtype=F32):
        t = self.nc.dram_tensor(name, list(shape), dtype, kind="ExternalInput")
        self.dr[name] = self.fw.dram(name, t)
        return self.dr[name]

    def scratch(self, name, shape, dtype=F32, out=False):
        kind = "ExternalOutput" if (out or name in self.dbg) else "Internal"
        t = self.nc.dram_tensor(name, list(shape), dtype, kind=kind)
        self.dr[name] = self.fw.dram(name, t)
        return self.dr[name]

    def declare_io(self):
        self.inp("x", [TL, D]); self.inp("ctx", [TC, D]); self.inp("cvec", [128, 8, 2])
        self.inp("vecs", [128, NV]); self.inp("rows", [1, NR]); self.inp("consts", [128, NCON])
        self.inp("mod_w", [2, D, 6 * D]); self.inp("router_w", [D, 16])
        self.inp("moe_w_gate", [2, 16, D, 512]); self.inp("moe_w_up", [2, 16, D, 512])
        self.inp("moe_w_down", [2, 16, 512, D])
        self.inp("ab_w_in", [D, 5152]); self.inp("ab_w_out", [2048, D])
        self.inp("hg_w_in", [D, 5120]); self.inp("hg_w_out", [D, D])

    def setup_consts(self):
        fw, k = self.fw, self.k
        self.con = fw.sbuf("con", [128, NCON], F32)
        fw.dma("sp", self.con[:], self.dr["consts"][:], self.dr["consts"], self.con)
        self.conb = fw.sbuf("conb", [128, NCON], BF16)
        k.cp(self.conb[:], self.con[:], [self.con], [self.conb])
        self.vec = fw.sbuf("vec", [128, NV], F32)
        fw.dma("act", self.vec[:], self.dr["vecs"][:], self.dr["vecs"], self.vec)

    def C(self, n, w=128, rows=128, b16=False):
        t = self.conb if b16 else self.con
        return t[0:rows, COFF[n]:COFF[n] + w]

    def V(self, n, j=0, w=1):
        return self.vec[:, VOFF[n] + j:VOFF[n] + j + w]

    def rowbc(self, n, w, off=0):
        return self.dr["rows"].t[0, ROFF[n] + off:ROFF[n] + off + w].partition_broadcast(128)

    fw_mod_double = True

    def phase_mod(self, l):
        self.fw.push()
        self.fw_mod_double = True
        st = self.mod_setup(l)
        for g in range(12):
            self.mod_group(st, g)
        self.mod_finish(st)
        self.fw.pop()

    def mod_setup(self, l):
        fw, k, dr = self.fw, self.k, self.dr
        modrow = self.scratch("modrow%d" % l, [2, 6 * D])
        modcol = self.scratch("modcol%d" % l, [128, 48, 2])
        cv = fw.sbuf("cv", [128, 8, 2], F32)
        fw.dma("sp", cv[:], dr["cvec"][:], dr["cvec"], cv)
        cs = fw.sbuf("cs", [128, 8, 2], BF16)
        k.act(cs[:], cv[:], AF.Silu, [cv], [cs])
        csb = fw.sbuf("csb", [128, 8, 128], BF16)
        k.cp(csb[:, :, 0:64], bc(cs[:, :, 0:1], [128, 8, 64]), [cs], [csb])
        k.cp(csb[:, :, 64:128], bc(cs[:, :, 1:2], [128, 8, 64]), [cs], [csb])
        nb_ = 2 if self.fw_mod_double else 1
        mb = [fw.sbuf("mb%d" % i, [128, 512], F32) for i in range(nb_)] * (3 - nb_)
        mcol = fw.sbuf("mcol", [128, 48, 2], F32)
        W = [fw.sbuf("modW%d" % i, [128, 8, 512], BF16) for i in range(nb_)] * (3 - nb_)
        rowb = [fw.sbuf("rowb%d" % i, [128, 512], F32) for i in range(nb_)] * (3 - nb_)
        wsrc = dr["mod_w"].t[l].rearrange("(k p) n -> p k n", p=128)
        return dict(l=l, modrow=modrow, modcol=modcol, cs=cs, csb=csb, mb=mb, mcol=mcol, W=W, rowb=rowb, wsrc=wsrc)

    def mod_group(self, st, g):
        fw, k, dr = self.fw, self.k, self.dr
        l, modrow, cs, csb, mcol = st["l"], st["modrow"], st["cs"], st["csb"], st["mcol"]
        w = st["W"][g % 2]
        fw.dma("pool", w[:], st["wsrc"][:, :, g * 512:(g + 1) * 512], dr["mod_w"], w)
        mb = st["mb"][g % 2]
        fw.dma("act", mb[:], self.rowbc("modb%d" % l, 512, off=g * 512), dr["rows"], mb)
        P = k.bank()
        for kk in range(8):
            k.mm(P[:, :], csb[:, kk, :], w[:, kk, :], kk == 0, kk == 7, [csb, w], P)
        rb = st["rowb"][g % 2]
        k.tt(rb[:], P[:, :], mb[:], ALU.add, [P, mb], [rb])
        fw.dma("sp", modrow.t[0:1, g * 512:(g + 1) * 512], rb[0:1, :], rb, modrow)
        fw.dma("act", modrow.t[1:2, g * 512:(g + 1) * 512], rb[64:65, :], rb, modrow)
        P2 = k.bank()
        for fc in range(4):
            for kk in range(8):
                k.mm(P2[:, fc * 2:fc * 2 + 2], w[:, kk, fc * 128:(fc + 1) * 128], cs[:, kk, :],
                     kk == 0, kk == 7, [cs, w], P2)
        k.tt(mcol[:, g * 4:(g + 1) * 4, :], P2[:, 0:8].rearrange("p (a b) -> p a b", b=2),
             bc(self.V("modb%d" % l, g * 4, 4).unsqueeze(2), [128, 4, 2]), ALU.add, [P2, self.vec], [mcol])

    def mod_finish(self, st):
        self.fw.dma("sp", st["modcol"].t[:], st["mcol"][:], st["mcol"], st["modcol"])


def make_inmaps(I, cores):
    vec, row, con = pack_shared(I)
    shared = {"vecs": vec, "rows": row, "consts": con}
    for n in ("mod_w", "router_w", "moe_w_gate", "moe_w_up", "moe_w_down", "hg_w_out"):
        shared[n] = np.ascontiguousarray(np.asarray(I[n], np.float32))
    shared["ab_w_in"] = np.ascontiguousarray(np.asarray(I["ab_w_in"][0], np.float32))
    shared["ab_w_out"] = np.ascontiguousarray(np.asarray(I["ab_w_out"][0], np.float32))
    shared["hg_w_in"] = np.ascontiguousarray(np.asarray(I["hg_w_in"][0], np.float32))
    shared["hg_w_out"] = np.ascontiguousarray(np.asarray(I["hg_w_out"][0], np.float32))
    maps = []
    for b in cores:
        m = dict(shared)
        m["x"] = np.ascontiguousarray(np.asarray(I["x"][b], np.float32))
        m["ctx"] = np.ascontiguousarray(np.asarray(I["ctx"][b], np.float32))
        cv = np.stack([_col(I["c"][b]), _col(I["c_ctx"])], axis=-1)
        m["cvec"] = np.ascontiguousarray(cv.astype(np.float32))
        maps.append(m)
    return maps


def _wsrc(dt, col0, ncols):
    return dt.rearrange("(k p) n -> p k n", p=128)[:, :, col0:col0 + ncols]


class Prog2(Prog):
    def load_AB(self, l, which, gname):
        fw, k = self.fw, self.k
        mc = fw.sbuf("mc", [128, 48, 2], F32)
        fw.dma("sp", mc[:], self.dr["modcol%d" % l].t[:], self.dr["modcol%d" % l], mc)
        AB = fw.sbuf("AB", [128, 2, 2, 8], F32)
        sh, sc = which
        for idx in range(2):
            k.ts(AB[:, idx, 0, :], mc[:, sc * 8:sc * 8 + 8, idx], 1.0, None, ALU.add, ALU.bypass, [mc], [AB])
            k.tt(AB[:, idx, 0, :], AB[:, idx, 0, :], self.V(gname, 0, 8), ALU.mult, [AB, self.vec], [AB])
            k.cp(AB[:, idx, 1, :], mc[:, sh * 8:sh * 8 + 8, idx], [mc], [AB])
        return AB

    def normmod_tile(self, X, AB, idx, hT, tok0, tmp):
        fw, k = self.fw, self.k
        junk, ss, xn, t3 = tmp
        k.act(junk[:], X[:], AF.Square, [X], [junk, ss], accum_out=ss[:, 0:1])
        k.act(ss[:, 1:2], ss[:, 0:1], AF.Sqrt, [ss], [ss], scale=1.0 / D, bias=self.epsb[:, 0:1])
        self.fw.op("dve", lambda e: e.reciprocal(ss[:, 2:3], ss[:, 1:2]), [ss], [ss])
        k.act(xn[:], X[:], AF.Copy, [X, ss], [xn], scale=ss[:, 2:3])
        P = k.bank()
        Pb = P[:, :].bitcast(BF16)
        for c in range(8):
            k.tr(Pb[:, c * 128:(c + 1) * 128], xn[:, c * 128:(c + 1) * 128], self.C("ident", b16=True),
                 [xn, self.conb], P, inc=(c == 7), first=(c == 0))
        Pv = Pb.rearrange("p (c t) -> p c t", t=128)
        k.tt(t3[:], Pv, bc(AB[:, idx, 0, :].unsqueeze(2), [128, 8, 128]), ALU.mult, [P, AB], [t3])
        k.tt(hT[:, :, tok0:tok0 + 128], t3[:], bc(AB[:, idx, 1, :].unsqueeze(2), [128, 8, 128]), ALU.add,
             [t3, AB], [hT])

    def norm_tmp(self):
        fw = self.fw
        return [(fw.sbuf("junk%d" % i, [128, D], BF16), fw.sbuf("ss%d" % i, [128, 4], F32), fw.sbuf("xn%d" % i, [128, D], BF16),
                 fw.sbuf("t3%d" % i, [128, 8, 128], F32)) for i in range(2)]

    def setup_consts(self):
        Prog.setup_consts(self)
        self.epsb = self.fw.sbuf("epsb", [128, 1], F32)
        self.k.memset(self.epsb[:], EPS, [self.epsb])

    def phase_normmod0(self, hT):
        fw, k, dr = self.fw, self.k, self.dr
        fw.push()
        AB = self.load_AB(0, (0, 1), "gmix0")
        tmp = self.norm_tmp()
        X = [fw.sbuf("X%d" % i, [128, D], F32) for i in range(2)]
        for i in range(NT):
            x = X[i % 2]
            if i < 16:
                fw.dma(k.q(), x[:], dr["x"].t[i * 128:(i + 1) * 128, :], dr["x"], x)
            else:
                fw.dma(k.q(), x[:], dr["ctx"].t[(i - 16) * 128:(i - 15) * 128, :], dr["ctx"], x)
            self.normmod_tile(x, AB, 0 if i < 16 else 1, hT, i * 128, tmp[i % 2])
        fw.pop()

    def phase_evenproj(self, hT):
        fw, k, dr = self.fw, self.k, self.dr
        win = dr["ab_w_in"]
        sz = self.scratch("sz_tok", [T, D])
        dtk = self.scratch("dt_tok", [T, 32])
        xtok = self.scratch("x_tok", [T, D], BF16)
        btok = self.scratch("b_tok", [T, 512], BF16)
        BT = self.scratch("BT", [4, 128, T], BF16)
        CT = self.scratch("CT", [4, 128, T], BF16)
        U = self.scratch("U_cf", [8, 128, T])
        fw.push()
        Wz = fw.sbuf("Wz", [128, 8, D], BF16)
        fw.dma("pool", Wz[:], _wsrc(win.t, 0, D), win, Wz)
        Wdt = fw.sbuf("Wdt", [128, 8, 32], BF16)
        fw.dma("pool", Wdt[:], _wsrc(win.t, 3072, 32), win, Wdt)
        dtb = fw.sbuf("dtb", [128, 32], F32)
        fw.dma("sp", dtb[:], self.rowbc("dtb", 32), dr["rows"], dtb)
        zs = [fw.sbuf("zs%d" % i, [128, D], F32) for i in range(2)]
        dts = [fw.sbuf("dts%d" % i, [128, 32], F32) for i in range(2)]
        for i in range(NT):
            z = zs[i % 2]
            for h in range(2):
                P = k.bank()
                for kk in range(8):
                    k.mm(P[:, :], hT[:, kk, i * 128:(i + 1) * 128], Wz[:, kk, h * 512:(h + 1) * 512],
                         kk == 0, kk == 7, [hT, Wz], P)
                k.act(z[:, h * 512:(h + 1) * 512], P[:, :], AF.Silu, [P], [z])
            fw.dma(k.q(), sz.t[i * 128:(i + 1) * 128, :], z[:], z, sz)
            P = k.bank()
            for kk in range(8):
                k.mm(P[:, 0:32], hT[:, kk, i * 128:(i + 1) * 128], Wdt[:, kk, :], kk == 0, kk == 7, [hT, Wdt], P)
            d = dts[i % 2]
            k.tt(d[:], P[:, 0:32], dtb[:], ALU.add, [P, dtb], [d])
            k.act(d[:], d[:], AF.Exp, [d], [d])
            k.act(d[:], d[:], AF.Ln, [d], [d], bias=1.0)
            fw.dma(k.q(), dtk.t[i * 128:(i + 1) * 128, :], d[:], d, dtk)
        Wg = [fw.sbuf("Wg%d" % i, [128, 8, 512], BF16) for i in range(2)]
        xinL = [fw.sbuf("xinL%d" % i, [128, TL + 4], F32) for i in range(2)]
        xinC = [fw.sbuf("xinC%d" % i, [128, TC + 4], F32) for i in range(2)]
        for b_ in xinL:
            k.memset(b_[:, 0:2], 0.0, [b_]); k.memset(b_[:, TL + 2:TL + 4], 0.0, [b_])
        for b_ in xinC:
            k.memset(b_[:, 0:2], 0.0, [b_]); k.memset(b_[:, TC + 2:TC + 4], 0.0, [b_])
        acc = [fw.sbuf("acc%d" % i, [128, T], F32) for i in range(2)]
        xc = [fw.sbuf("xc%d" % i, [128, T], BF16) for i in range(2)]
        stg = [fw.sbuf("stg%d" % i, [128, 4, 128], BF16) for i in range(2)]
        blocks = [(0, 512), (512, 512), (1024, 512), (1536, 512), (2048, 256)]
        si = 0
        for cc in range(16):
            if cc % 4 == 0:
                w = Wg[(cc // 4) % 2]
                fw.dma("pool", w[:], _wsrc(win.t, 1024 + cc * 128, 512), win, w)
            xl, xcx, a, o = xinL[cc % 2], xinC[cc % 2], acc[cc % 2], xc[cc % 2]
            for (t0, n) in blocks:
                P = k.bank()
                for kk in range(8):
                    k.mm(P[:, 0:n], w[:, kk, (cc % 4) * 128:(cc % 4 + 1) * 128], hT[:, kk, t0:t0 + n],
                         kk == 0, kk == 7, [hT, w], P)
                if t0 < TL:
                    k.act(xl[:, 2 + t0:2 + t0 + n], P[:, 0:n], AF.Copy, [P], [xl])
                else:
                    k.act(xcx[:, 2:2 + n], P[:, 0:n], AF.Copy, [P], [xcx])
            for (xi, lo, n) in ((xl, 0, TL), (xcx, TL, TC)):
                k.ts(a[:, lo:lo + n], xi[:, 0:n], self.V("convw", cc * 5), self.V("convb", cc), ALU.mult, ALU.add,
                     [xi, self.vec], [a])
                for kt in range(1, 5):
                    k.stt(a[:, lo:lo + n], xi[:, kt:kt + n], self.V("convw", cc * 5 + kt), a[:, lo:lo + n],
                          ALU.mult, ALU.add, [xi, self.vec, a], [a])
            k.act(o[:], a[:], AF.Silu, [a], [o])
            if cc >= 8:
                dst = BT if cc < 12 else CT
                fw.dma(k.q(), dst.t[cc % 4], o[:], o, dst)
            if cc < 12:
                for t4 in range(0, NT, 4):
                    nt = min(4, NT - t4)
                    P = k.bank()
                    Pb = P[:, :].bitcast(BF16)
                    for j in range(nt):
                        k.tr(Pb[:, j * 128:(j + 1) * 128], o[:, (t4 + j) * 128:(t4 + j + 1) * 128],
                             self.C("ident", b16=True), [o, self.conb], P, inc=(j == nt - 1), first=(j == 0))
                    s = stg[si % 2]; si += 1
                    k.cp(s[:, 0:nt, :], Pb[:, 0:nt * 128].rearrange("p (a c) -> p a c", c=128), [P], [s])
                    if cc < 8:
                        dd = xtok.t[t4 * 128:(t4 + nt) * 128, cc * 128:(cc + 1) * 128]
                        dbuf = xtok
                    else:
                        dd = btok.t[t4 * 128:(t4 + nt) * 128, (cc - 8) * 128:(cc - 7) * 128]
                        dbuf = btok
                    fw.dma(k.q(), dd.rearrange("(a p) c -> p a c", p=128), s[:, 0:nt, :], s, dbuf)
        ub = [fw.sbuf("ub%d" % i, [128, T], F32) for i in range(2)]
        sg = [fw.sbuf("sg%d" % i, [128, 512], F32) for i in range(2)]
        gi = 0
        for c in range(8):
            if c % 4 == 0:
                wv = Wg[0]; wg_ = Wg[1]
                fw.dma("pool", wv[:], _wsrc(win.t, 3104 + c * 128, 512), win, wv)
                fw.dma("pool", wg_[:], _wsrc(win.t, 4128 + c * 128, 512), win, wg_)
            u = ub[c % 2]
            for (t0, n) in blocks:
                Pv = k.bank(); Pg = k.bank()
                for kk in range(8):
                    k.mm(Pv[:, 0:n], wv[:, kk, (c % 4) * 128:(c % 4 + 1) * 128], hT[:, kk, t0:t0 + n],
                         kk == 0, kk == 7, [hT, wv], Pv)
                for kk in range(8):
                    k.mm(Pg[:, 0:n], wg_[:, kk, (c % 4) * 128:(c % 4 + 1) * 128], hT[:, kk, t0:t0 + n],
                         kk == 0, kk == 7, [hT, wg_], Pg)
                s = sg[gi % 2]; gi += 1
                k.act(s[:, 0:n], Pg[:, 0:n], AF.Sigmoid, [Pg], [s])
                k.tt(u[:, t0:t0 + n], Pv[:, 0:n], s[:, 0:n], ALU.mult, [Pv, s], [u])
            fw.dma(k.q(), U.t[c], u[:], u, U)
        fw.pop()


class Prog3(Prog2):
    def phase_ssd(self):
        fw, k, dr = self.fw, self.k, self.dr
        xtok, btok, BT, CT, dtk, sz = (dr[n] for n in ("x_tok", "b_tok", "BT", "CT", "dt_tok", "sz_tok"))
        yf = self.scratch("yf_tok", [T, D])
        oT = self.scratch("oT", [16, 128, T], BF16)
        fw.push()
        al = fw.sbuf("al", [128, 32], F32)
        fw.dma("sp", al[:], self.rowbc("alog", 32), dr["rows"], al)
        aneg = fw.sbuf("aneg", [128, 32], F32)
        k.act(aneg[:], al[:], AF.Exp, [al], [aneg])
        k.ts(aneg[:], aneg[:], -1.0, None, ALU.mult, ALU.bypass, [aneg], [aneg])
        dsk = fw.sbuf("dsk", [128, 16], F32)
        fw.dma("act", dsk[:], self.rowbc("ssdd", 16), dr["rows"], dsk)
        S32 = fw.sbuf("S32", [128, D], F32)
        Sbf = fw.sbuf("Sbf", [128, D], BF16)
        Xt = [fw.sbuf("Xt%d" % i, [128, D], BF16) for i in range(2)]
        Bt = [fw.sbuf("Bt%d" % i, [128, 512], BF16) for i in range(2)]
        BTc = [fw.sbuf("BTc%d" % i, [128, 4, 128], BF16) for i in range(2)]
        CTc = [fw.sbuf("CTc%d" % i, [128, 4, 128], BF16) for i in range(2)]
        dtc = [fw.sbuf("dtc%d" % i, [128, 16], F32) for i in range(2)]
        WK = []
        for i in range(2):
            WK.append(dict(
                la=fw.sbuf("la%d" % i, [128, 16], F32), cum=fw.sbuf("cum%d" % i, [128, 16], F32),
                rhs2=fw.sbuf("rhs2%d" % i, [128, 16, 128], F32), Dm=fw.sbuf("Dm%d" % i, [128, 16, 128], F32),
                Eb=fw.sbuf("Eb%d" % i, [128, 16, 128], BF16), cbT=fw.sbuf("cbT%d" % i, [128, 4, 128], BF16),
                M=fw.sbuf("M%d" % i, [128, 16, 128], BF16), eR=fw.sbuf("eR%d" % i, [128, 16, 128], BF16),
                ECT=fw.sbuf("ECT%d" % i, [128, 16, 128], BF16), Rl=fw.sbuf("Rl%d" % i, [128, 16], F32),
                t16=fw.sbuf("t16%d" % i, [128, 16], F32), te=fw.sbuf("te%d" % i, [128, 16], F32),
                cd=fw.sbuf("cd%d" % i, [128, 16], F32), w2=fw.sbuf("w2%d" % i, [128, 16], F32),
                xdt=fw.sbuf("xdt%d" % i, [128, 16, 64], BF16), xdte=fw.sbuf("xdte%d" % i, [128, 16, 64], BF16)))
        yst = [fw.sbuf("yst%d" % i, [128, D], F32) for i in range(2)]
        yft2 = [fw.sbuf("yft%d" % i, [128, D], F32) for i in range(2)]
        szt2 = [fw.sbuf("szt%d" % i, [128, D], F32) for i in range(2)]
        xd2 = [fw.sbuf("xd%d" % i, [128, 16, 64], F32) for i in range(2)]
        junk2 = [fw.sbuf("junk%d" % i, [128, D], BF16) for i in range(2)]
        ss2 = [fw.sbuf("ss%d" % i, [128, 4], F32) for i in range(2)]
        yn2 = [fw.sbuf("yn%d" % i, [128, D], BF16) for i in range(2)]
        ost = [fw.sbuf("ost%d" % i, [128, 8, 128], BF16) for i in range(2)]
        identb = self.C("ident", b16=True)
        for d in range(2):
            tri = self.C("triU" if d == 0 else "triL")
            neg = self.C("negF" if d == 0 else "negB")
            last = 127 if d == 0 else 0
            k.memset(S32[:], 0.0, [S32])
            k.memset(Sbf[:], 0.0, [Sbf])
            order = [16, 17] + list(range(16)) if d == 0 else [17, 16] + list(range(15, -1, -1))
            def head(ci, i):
                t0 = i * 128
                X, B_, BTt, CTt, dt_ = Xt[ci % 2], Bt[ci % 2], BTc[ci % 2], CTc[ci % 2], dtc[ci % 2]
                wk = WK[ci % 2]
                la, cum, rhs2, Dm, Eb, cbT, M, eR, ECT, Rl, t16, te, cd, w2, xdt, xdte = (wk[n_] for n_ in (
                    "la", "cum", "rhs2", "Dm", "Eb", "cbT", "M", "eR", "ECT", "Rl", "t16", "te", "cd", "w2", "xdt", "xdte"))
                fw.dma("sp", X[:], xtok.t[t0:t0 + 128, :], xtok, X)
                fw.dma("act", B_[:], btok.t[t0:t0 + 128, :], btok, B_)
                fw.dma("sp", BTt[:], BT.t[:, :, t0:t0 + 128].rearrange("g n t -> n g t"), BT, BTt)
                fw.dma("act", CTt[:], CT.t[:, :, t0:t0 + 128].rearrange("g n t -> n g t"), CT, CTt)
                fw.dma("sp", dt_[:], dtk.t[t0:t0 + 128, d * 16:(d + 1) * 16], dtk, dt_)
                k.tt(la[:], dt_[:], aneg[:, d * 16:(d + 1) * 16], ALU.mult, [dt_, aneg], [la])
                Pc = k.bank()
                k.mm(Pc[:, 0:16], tri, la[:], True, True, [self.con, la], Pc)
                k.act(cum[:], Pc[:, 0:16], AF.Copy, [Pc], [cum])
                k.tt(rhs2[:], bc(tri.unsqueeze(1), [128, 16, 128]), bc(la[:].unsqueeze(2), [128, 16, 128]), ALU.mult,
                     [self.con, la], [rhs2], eng="pool")
                Rb = []
                for g in range(4):
                    P = k.bank()
                    k.mm(P[:, :], self.C("ones"), rhs2[:, 4 * g:4 * g + 4, :].rearrange("p a b -> p (a b)"), True, True,
                         [self.con, rhs2], P)
                    Rb.append(P)
                for h in range(16):
                    P = Rb[h // 4]
                    k.stt(Dm[:, h, :], P[:, (h % 4) * 128:(h % 4 + 1) * 128], cum[:, h:h + 1], neg, ALU.subtract, ALU.add,
                          [P, cum, self.con], [Dm])
                for g in range(4):
                    P = Rb[g]
                    k.act(eR[:, 4 * g:4 * g + 4, :].rearrange("p a b -> p (a b)"), P[:, :], AF.Exp, [P], [eR])
                    k.cp(Rl[:, 4 * g:4 * g + 4], P[:, :].rearrange("p (a b) -> p a b", b=128)[:, :, last], [P], [Rl])
                k.act(Eb[:].rearrange("p a b -> p (a b)"), Dm[:].rearrange("p a b -> p (a b)"), AF.Exp, [Dm], [Eb])
                Pcb = k.bank()
                for g in range(4):
                    k.mm(Pcb[:, g * 128:(g + 1) * 128], BTt[:, g, :], CTt[:, g, :], True, True, [BTt, CTt], Pcb)
                k.act(cbT[:].rearrange("p a b -> p (a b)"), Pcb[:, :], AF.Copy, [Pcb], [cbT])
                for g in range(4):
                    k.tt(M[:, 4 * g:4 * g + 4, :], Eb[:, 4 * g:4 * g + 4, :], bc(cbT[:, g:g + 1, :], [128, 4, 128]), ALU.mult,
                         [Eb, cbT], [M])
                    k.tt(ECT[:, 4 * g:4 * g + 4, :], eR[:, 4 * g:4 * g + 4, :], bc(CTt[:, g:g + 1, :], [128, 4, 128]), ALU.mult,
                         [eR, CTt], [ECT])
                k.tt(t16[:], Rl[:], cum[:], ALU.subtract, [Rl, cum], [t16])
                k.act(te[:], t16[:], AF.Exp, [t16], [te])
                k.act(cd[:], Rl[:], AF.Exp, [Rl], [cd])
                k.tt(w2[:], dt_[:], te[:], ALU.mult, [dt_, te], [w2])
                Xv = X[:].rearrange("p (h e) -> p h e", e=64)
                k.tt(xdt[:], Xv, bc(dt_[:].unsqueeze(2), [128, 16, 64]), ALU.mult, [X, dt_], [xdt])
                k.tt(xdte[:], Xv, bc(w2[:].unsqueeze(2), [128, 16, 64]), ALU.mult, [X, w2], [xdte])

            def tail(ci, i):
                t0 = i * 128
                X, B_, dt_ = Xt[ci % 2], Bt[ci % 2], dtc[ci % 2]
                wk = WK[ci % 2]
                M, ECT, cd, xdt, xdte = (wk[n_] for n_ in ("M", "ECT", "cd", "xdt", "xdte"))
                Xv = X[:].rearrange("p (h e) -> p h e", e=64)
                Y = [k.bank(), k.bank()]
                for h in range(16):
                    P = Y[h // 8]
                    cs_ = slice((h % 8) * 64, (h % 8 + 1) * 64)
                    k.mm(P[:, cs_], M[:, h, :], xdt[:, h, :], True, False, [M, xdt], P)
                    k.mm(P[:, cs_], ECT[:, h, :], Sbf[:, h * 64:(h + 1) * 64], False, True, [ECT, Sbf], P)
                CS = [k.bank(), k.bank()]
                for g in range(4):
                    P = CS[g // 2]
                    k.mm(P[:, (g % 2) * 256:(g % 2 + 1) * 256], B_[:, g * 128:(g + 1) * 128],
                         xdte[:, 4 * g:4 * g + 4, :].rearrange("p a b -> p (a b)"), True, True, [B_, xdte], P)
                Sv = S32[:].rearrange("p (h e) -> p h e", e=64)
                k.tt(Sv, Sv, bc(cd[:].unsqueeze(2), [128, 16, 64]), ALU.mult, [S32, cd], [S32])
                for j in range(2):
                    k.tt(S32[:, j * 512:(j + 1) * 512], S32[:, j * 512:(j + 1) * 512], CS[j][:, :], ALU.add, [S32, CS[j]], [S32])
                k.cp(Sbf[:], S32[:], [S32], [Sbf])
                if d == 0:
                    ys = yst[ci % 2]
                    for j in range(2):
                        k.act(ys[:, j * 512:(j + 1) * 512], Y[j][:, :], AF.Copy, [Y[j]], [ys])
                    fw.dma("act", yf.t[t0:t0 + 128, :], ys[:], ys, yf)
                else:
                    yft, szt, xd, junk, ss, yn = yft2[ci % 2], szt2[ci % 2], xd2[ci % 2], junk2[ci % 2], ss2[ci % 2], yn2[ci % 2]
                    fw.dma("sp", yft[:], yf.t[t0:t0 + 128, :], yf, yft)
                    fw.dma("act", szt[:], sz.t[t0:t0 + 128, :], sz, szt)
                    ys = yst[ci % 2]
                    for j in range(2):
                        k.tt(ys[:, j * 512:(j + 1) * 512], Y[j][:, :], yft[:, j * 512:(j + 1) * 512], ALU.add, [Y[j], yft], [ys])
                    k.tt(xd[:], Xv, bc(dsk[:].unsqueeze(2), [128, 16, 64]), ALU.mult, [X, dsk], [xd])
                    k.tt(ys[:], ys[:], xd[:].rearrange("p a b -> p (a b)"), ALU.add, [ys, xd], [ys])
                    k.tt(ys[:], ys[:], szt[:], ALU.mult, [ys, szt], [ys])
                    k.act(junk[:], ys[:], AF.Square, [ys], [junk, ss], accum_out=ss[:, 0:1])
                    k.act(ss[:, 1:2], ss[:, 0:1], AF.Sqrt, [ss], [ss], scale=1.0 / D, bias=self.epsb[:, 0:1])
                    fw.op("dve", lambda e: e.reciprocal(ss[:, 2:3], ss[:, 1:2]), [ss], [ss])
                    k.act(yn[:], ys[:], AF.Copy, [ys, ss], [yn], scale=ss[:, 2:3])
                    P = k.bank()
                    Pb = P[:, :].bitcast(BF16)
                    for c in range(8):
                        k.tr(Pb[:, c * 128:(c + 1) * 128], yn[:, c * 128:(c + 1) * 128], identb, [yn, self.conb], P,
                             inc=(c == 7), first=(c == 0))
                    os_ = ost[ci % 2]
                    k.tt(os_[:], Pb.rearrange("p (c t) -> p c t", t=128), bc(self.V("ssdg", 0, 8).unsqueeze(2), [128, 8, 128]),
                         ALU.mult, [P, self.vec], [os_])
                    fw.dma("sp", oT.t[0:8, :, t0:t0 + 128].rearrange("c p t -> p c t"), os_[:], os_, oT)

            head(0, order[0])
            for ci, i in enumerate(order):
                if ci + 1 < len(order):
                    head(ci + 1, order[ci + 1])
                tail(ci, i)
            fw.barrier()
        fw.pop()

    def phase_conformer(self):
        fw, k, dr = self.fw, self.k, self.dr
        U, oT = dr["U_cf"], dr["oT"]
        fw.push()
        cv = [fw.sbuf("cv%d" % c, [128, T], F32) for c in range(8)]
        ub = [fw.sbuf("cu%d" % i, [128, T], F32) for i in range(2)]
        upL = [fw.sbuf("upL%d" % i, [128, 32, 94], BF16) for i in range(2)]
        upC = [fw.sbuf("upC%d" % i, [128, TC + 30], BF16) for i in range(2)]
        dg = [fw.sbuf("dg%d" % i, [128, 31, 128], BF16) for i in range(2)]
        for b_ in upL + upC:
            k.memset(b_[:], 0.0, [b_])
        identb = self.C("ident", b16=True)
        for c in range(8):
            u = ub[c % 2]; pl_, pc_, dgc = upL[c % 2], upC[c % 2], dg[c % 2]
            fw.dma(k.q(), u[:], U.t[c], U, u)
            k.cp(pl_[:, :, 15:79], u[:, 0:TL].rearrange("p (r w) -> p r w", w=64), [u], [pl_], eng="pool")
            k.act(pc_[:, 15:15 + TC], u[:, TL:T], AF.Copy, [u], [pc_])
            k.tt(dgc[:], bc(identb.unsqueeze(1), [128, 31, 128]),
                 bc(self.vec[:, VOFF["cfw"] + c * 31:VOFF["cfw"] + (c + 1) * 31].unsqueeze(2), [128, 31, 128]),
                 ALU.mult, [self.conb, self.vec], [dgc])
            for b in range(5):
                P = k.bank()
                n = 512 if b < 4 else TC
                for j in range(31):
                    rhs = pl_[:, 8 * b:8 * b + 8, j:j + 64] if b < 4 else pc_[:, j:j + TC]
                    k.mm(P[:, 0:n], dgc[:, j, :], rhs, j == 0, j == 30, [dgc, pl_ if b < 4 else pc_], P)
                k.act(cv[c][:, b * 512:b * 512 + n], P[:, 0:n], AF.Identity, [P, self.vec], [cv[c]], bias=self.V("cfb", c))
        sq = [fw.sbuf("sq%d" % i, [128, 512], F32) for i in range(2)]
        mean = fw.sbuf("mean", [128, 512], F32)
        var = fw.sbuf("var", [128, 512], F32)
        tmpb = [fw.sbuf("ct%d" % i, [128, 512], F32) for i in range(2)]
        ob = [fw.sbuf("cob%d" % i, [128, 512], BF16) for i in range(2)]
        ones = self.C("ones")
        qi = 0
        for (t0, n) in [(0, 512), (512, 512), (1024, 512), (1536, 512), (2048, 256)]:
            P1 = k.bank(); P2 = k.bank()
            for c in range(8):
                k.mm(P1[:, 0:n], ones, cv[c][:, t0:t0 + n], c == 0, c == 7, [self.con, cv[c]], P1)
            for c in range(8):
                s = sq[c % 2]
                k.act(s[:, 0:n], cv[c][:, t0:t0 + n], AF.Square, [cv[c]], [s])
                k.mm(P2[:, 0:n], ones, s[:, 0:n], c == 0, c == 7, [self.con, s], P2, inc=True)
            k.ts(mean[:, 0:n], P1[:, 0:n], 1.0 / D, None, ALU.mult, ALU.bypass, [P1], [mean])
            k.tt(var[:, 0:n], mean[:, 0:n], mean[:, 0:n], ALU.mult, [mean], [var])
            k.stt(var[:, 0:n], P2[:, 0:n], 1.0 / D, var[:, 0:n], ALU.mult, ALU.subtract, [P2, var], [var])
            k.act(var[:, 0:n], var[:, 0:n], AF.Sqrt, [var], [var], bias=self.epsb[:, 0:1])
            fw.op("dve", lambda e: e.reciprocal(var[:, 0:n], var[:, 0:n]), [var], [var])
            for c in range(8):
                tb = tmpb[c % 2]; o = ob[c % 2]
                k.tt(tb[:, 0:n], cv[c][:, t0:t0 + n], mean[:, 0:n], ALU.subtract, [cv[c], mean], [tb])
                k.tt(tb[:, 0:n], tb[:, 0:n], var[:, 0:n], ALU.mult, [tb, var], [tb])
                k.act(o[:, 0:n], tb[:, 0:n], AF.Silu, [tb, self.vec], [o], scale=self.V("cflg", c), bias=self.V("cflb", c))
                fw.dma(k.q(), oT.t[8 + c, :, t0:t0 + n], o[:, 0:n], o, oT)
        fw.pop()


class Prog4(Prog3):
    def phase_outproj(self, l, oT, nk, wname, xsrc, hT2, xmid_name, tiles):
        fw, k, dr = self.fw, self.k, self.dr
        xmid = self.scratch(xmid_name, [T, D])
        fw.push()
        W = fw.sbuf("Wout", [128, nk, D], BF16)
        fw.dma("pool", W[:], _wsrc(dr[wname].t, 0, D), dr[wname], W)
        m2 = fw.sbuf("m2", [128, 2, D], F32)
        mr = dr["modrow%d" % l]
        for idx in range(2):
            fw.dma(k.q(), m2[:, idx, :], mr.t[idx, 2 * D:3 * D].partition_broadcast(128), mr, m2)
        AB = self.load_AB(l, (3, 4), "gffn%d" % l)
        tmp = self.norm_tmp()
        ot = [fw.sbuf("ot%d" % i, [128, nk, 128], BF16) for i in range(2)]
        X = [fw.sbuf("Xo%d" % i, [128, D], F32) for i in range(2)]
        for n_, i in enumerate(tiles):
            o = ot[n_ % 2]; x = X[n_ % 2]
            idx = 0 if i < 16 else 1
            fw.dma("sp", o[:], oT.t[:, :, i * 128:(i + 1) * 128].rearrange("c p t -> p c t"), oT, o)
            for (p0, p1, sb, ap) in xsrc(i):
                fw.dma("act", x[p0:p1, :], ap, sb, x)
            for h in range(2):
                P = k.bank()
                for kk in range(nk):
                    k.mm(P[:, :], o[:, kk, :], W[:, kk, h * 512:(h + 1) * 512], kk == 0, kk == nk - 1, [o, W], P)
                hs = slice(h * 512, (h + 1) * 512)
                tq = tmp[n_ % 2]
                k.tt(tq[3][:].rearrange("p a b -> p (a b)")[:, hs], P[:, :], m2[:, idx, hs], ALU.mult, [P, m2], [tq[3]])
                k.tt(x[:, hs], x[:, hs], tq[3][:].rearrange("p a b -> p (a b)")[:, hs], ALU.add, [x, tq[3]], [x])
            fw.dma("sp", xmid.t[i * 128:(i + 1) * 128, :], x[:], x, xmid)
            self.normmod_tile(x, AB, idx, hT2, i * 128, tmp[n_ % 2])
        fw.pop()

    def phase_moe(self, l, hT2, xmid_name, xout, tiles, final=False, hook=None):
        fw, k, dr = self.fw, self.k, self.dr
        xmid = dr[xmid_name]
        nt = len(tiles)
        fw.push()
        Wr = fw.sbuf("Wr", [128, 8, 16], BF16)
        fw.dma("pool", Wr[:], _wsrc(dr["router_w"].t, 0, 16), dr["router_w"], Wr)
        rb = fw.sbuf("rb", [128, 16], F32)
        fw.dma("sp", rb[:], self.rowbc("rb", 16), dr["rows"], rb)
        sc = fw.sbuf("sc", [128, NT, 16], F32)
        sel = fw.sbuf("sel", [128, NT, 16], F32)
        for j, i in enumerate(tiles):
            P = k.bank()
            for kk in range(8):
                k.mm(P[:, 0:16], hT2[:, kk, i * 128:(i + 1) * 128], Wr[:, kk, :], kk == 0, kk == 7, [hT2, Wr], P)
            k.act(sc[:, j, :], P[:, 0:16], AF.Sigmoid, [P], [sc])
        S3 = lambda b_: b_[:, 0:nt, :]
        S4 = lambda b_: b_[:, 0:nt, :].rearrange("p t (g e) -> p t g e", e=4)
        k.tt(S3(sel), S3(sc), bc(rb[:].unsqueeze(1), [128, nt, 16]), ALU.add, [sc, rb], [sel])
        m1 = fw.sbuf("m1", [128, NT, 4], F32)
        m2_ = fw.sbuf("m2_", [128, NT, 4], F32)
        eq = fw.sbuf("eq", [128, NT, 16], F32)
        gs = fw.sbuf("gs", [128, NT, 4], F32)
        gm = fw.sbuf("gm", [128, NT, 1], F32)
        comb = fw.sbuf("comb", [128, NT, 16], F32)
        den = fw.sbuf("den", [128, NT, 1], F32)
        M3 = lambda b_: b_[:, 0:nt, :]
        fw.op("dve", lambda e: e.tensor_reduce(M3(m1), S4(sel), AX.X, ALU.max), [sel], [m1])
        k.tt(S4(eq), S4(sel), bc(M3(m1).unsqueeze(3), [128, nt, 4, 4]), ALU.is_equal, [sel, m1], [eq])
        k.stt(S3(eq), S3(eq), -1e9, S3(sel), ALU.mult, ALU.add, [eq, sel], [eq])
        fw.op("dve", lambda e: e.tensor_reduce(M3(m2_), S4(eq), AX.X, ALU.max), [eq], [m2_])
        k.tt(M3(gs), M3(m1), M3(m2_), ALU.add, [m1, m2_], [gs])
        fw.op("dve", lambda e: e.tensor_reduce(M3(gm), M3(gs), AX.X, ALU.max), [gs], [gm])
        k.tt(M3(gs), M3(gs), bc(M3(gm), [128, nt, 4]), ALU.is_equal, [gs, gm], [gs])
        k.tt(S4(eq), S4(sel), bc(M3(m2_).unsqueeze(3), [128, nt, 4, 4]), ALU.is_ge, [sel, m2_], [eq])
        k.tt(S4(eq), S4(eq), bc(M3(gs).unsqueeze(3), [128, nt, 4, 4]), ALU.mult, [eq, gs], [eq])
        k.tt(S3(comb), S3(eq), S3(sc), ALU.mult, [eq, sc], [comb])
        fw.op("dve", lambda e: e.tensor_reduce(M3(den), S3(comb), AX.X, ALU.add), [comb], [den])
        fw.op("dve", lambda e: e.reciprocal(M3(den), M3(den)), [den], [den])
        k.tt(S3(comb), S3(comb), bc(M3(den), [128, nt, 16]), ALU.mult, [comb, den], [comb])
        if "comb%d" % l in self.dbg:
            cdb = self.scratch("comb%d" % l, [128, NT, 16])
            fw.dma("sp", cdb.t[:], comb[:], comb, cdb)
        acc = fw.sbuf("acc", [128, NT, D], F32)
        k.memset(acc[:, 0:nt // 2, :], 0.0, [acc], eng="pool")
        k.memset(acc[:, nt // 2:nt, :], 0.0, [acc], eng="dve")
        Wg = [fw.sbuf("Wg%d" % i, [128, 8, 512], BF16) for i in range(2)]
        Wu = [fw.sbuf("Wu%d" % i, [128, 8, 512], BF16) for i in range(2)]
        Wd = [fw.sbuf("Wd%d" % i, [128, 4, D], BF16) for i in range(2)]
        sg = [fw.sbuf("sg%d" % i, [128, 512], F32) for i in range(2)]
        aT = [fw.sbuf("aT%d" % i, [128, 4, 512], BF16) for i in range(2)]
        blocks = [tiles[j:j + 4] for j in range(0, nt, 4)]
        si = 0
        if hook:
            fw.push()
            self.fw_mod_double = False
        hst = hook[0]() if hook else None
        for e_ in range(16):
            if hook and e_ >= 2 and e_ - 2 < 12:
                hook[1](hst, e_ - 2)
            wg, wu, wd = Wg[e_ % 2], Wu[e_ % 2], Wd[e_ % 2]
            fw.dma("pool", wg[:], _wsrc(dr["moe_w_gate"].t[l, e_], 0, 512), dr["moe_w_gate"], wg)
            fw.dma("pool", wu[:], _wsrc(dr["moe_w_up"].t[l, e_], 0, 512), dr["moe_w_up"], wu)
            fw.dma("pool", wd[:], _wsrc(dr["moe_w_down"].t[l, e_], 0, D), dr["moe_w_down"], wd)
            for bi, blk in enumerate(blocks):
                t0 = blk[0] * 128
                n = len(blk) * 128
                a = aT[bi % 2]
                for fc in range(4):
                    Pg = k.bank(); Pu = k.bank()
                    for kk in range(8):
                        k.mm(Pg[:, 0:n], wg[:, kk, fc * 128:(fc + 1) * 128], hT2[:, kk, t0:t0 + n], kk == 0, kk == 7, [wg, hT2], Pg)
                    for kk in range(8):
                        k.mm(Pu[:, 0:n], wu[:, kk, fc * 128:(fc + 1) * 128], hT2[:, kk, t0:t0 + n], kk == 0, kk == 7, [wu, hT2], Pu)
                    s = sg[si % 2]; si += 1
                    k.act(s[:, 0:n], Pg[:, 0:n], AF.Silu, [Pg], [s])
                    k.tt(a[:, fc, 0:n], Pu[:, 0:n], s[:, 0:n], ALU.mult, [Pu, s], [a])
                for jt, i in enumerate(blk):
                    j = tiles.index(i)
                    for h in range(2):
                        P = k.bank()
                        for fc in range(4):
                            k.mm(P[:, :], a[:, fc, jt * 128:(jt + 1) * 128], wd[:, fc, h * 512:(h + 1) * 512], fc == 0, fc == 3, [a, wd], P)
                        k.stt(acc[:, j, h * 512:(h + 1) * 512], P[:, :], comb[:, j, e_:e_ + 1], acc[:, j, h * 512:(h + 1) * 512],
                              ALU.mult, ALU.add, [P, comb, acc], [acc])
        if hook:
            hook[2](hst)
            fw.pop()
        m5 = fw.sbuf("m5", [128, 2, D], F32)
        mr = dr["modrow%d" % l]
        for idx in range(2):
            fw.dma(k.q(), m5[:, idx, :], mr.t[idx, 5 * D:6 * D].partition_broadcast(128), mr, m5)
        X = [fw.sbuf("Xm%d" % i, [128, D], F32) for i in range(2)]
        if final:
            fg = fw.sbuf("fg", [128, D], F32)
            fw.dma("sp", fg[:], self.rowbc("fng", D), dr["rows"], fg)
            junk = fw.sbuf("junkf", [128, D], BF16)
            ss = fw.sbuf("ssf", [128, 4], F32)
        for j, i in enumerate(tiles):
            x = X[j % 2]
            idx = 0 if i < 16 else 1
            fw.dma("sp", x[:], xmid.t[i * 128:(i + 1) * 128, :], xmid, x)
            k.tt(acc[:, j, :], acc[:, j, :], m5[:, idx, :], ALU.mult, [acc, m5], [acc])
            k.tt(x[:], x[:], acc[:, j, :], ALU.add, [x, acc], [x])
            if final:
                k.act(junk[:], x[:], AF.Square, [x], [junk, ss], accum_out=ss[:, 0:1])
                k.act(ss[:, 1:2], ss[:, 0:1], AF.Sqrt, [ss], [ss], scale=1.0 / D, bias=self.epsb[:, 0:1])
                fw.op("dve", lambda e: e.reciprocal(ss[:, 2:3], ss[:, 1:2]), [ss], [ss])
                k.stt(x[:], x[:], ss[:, 2:3], fg[:], ALU.mult, ALU.mult, [x, ss, fg], [x])
                for (p0, p1, sb, ap) in self.cm_rows(xout, i):
                    fw.dma("act", ap, x[p0:p1, :], x, xout)
            else:
                fw.dma("act", xout.t[i * 128:(i + 1) * 128, :], x[:], x, xout)
        fw.pop()


class Prog5(Prog4):
    def cm_rows(self, db, i):
        v = db.t[0:TL, :].rearrange("(r w) d -> w r d", w=64)
        return [(32 * j, 32 * j + 32, db, v[4 * i + j]) for j in range(4)]

    def phase_normmod1(self, hT, x1):
        fw, k = self.fw, self.k
        fw.push()
        AB = self.load_AB(1, (0, 1), "gmix1")
        tmp = self.norm_tmp()
        X = [fw.sbuf("X%d" % i, [128, D], F32) for i in range(2)]
        for i in range(NT):
            x = X[i % 2]
            if i < 16:
                for (p0, p1, sb, ap) in self.cm_rows(x1, i):
                    fw.dma(k.q(), x[p0:p1, :], ap, sb, x)
            else:
                fw.dma(k.q(), x[:], x1.t[i * 128:(i + 1) * 128, :], x1, x)
            self.normmod_tile(x, AB, 0 if i < 16 else 1, hT, i * 128, tmp[i % 2])
        fw.pop()

    def phase_hgproj(self, hT):
        fw, k, dr = self.fw, self.k, self.dr
        win = dr["hg_w_in"]
        QT = self.scratch("QT", [8, 128, TL])
        LF = self.scratch("LF", [2, 8, 128, T])
        KT = self.scratch("KT", [2, 8, 128, T])
        sgt = self.scratch("sg_tok", [TL, D])
        vtk = self.scratch("v_tok", [T, D], BF16)
        fw.push()
        lb = fw.sbuf("lb", [128, 8], F32)
        oml = fw.sbuf("oml", [128, 8], F32)
        k.tt(lb[:], self.V("hglb1", 0, 8), self.V("hglb0", 0, 8), ALU.subtract, [self.vec], [lb])
        k.act(lb[:], lb[:], AF.Sigmoid, [lb], [lb])
        k.ts(oml[:], lb[:], -1.0, 1.0, ALU.mult, ALU.add, [lb], [oml])
        W = [fw.sbuf("Wh%d" % i, [128, 8, 512], BF16) for i in range(2)]
        wi = 0
        stf = [fw.sbuf("stf%d" % i, [128, 512], F32) for i in range(2)]
        stb = [fw.sbuf("stb%d" % i, [128, 512], BF16) for i in range(2)]
        si = 0
        for (c0, ntl, isg) in ((1024, 16, True), (1536, 16, True), (2048, NT, False), (2560, NT, False)):
            w = W[wi % 2]; wi += 1
            fw.dma("pool", w[:], _wsrc(win.t, c0, 512), win, w)
            for i in range(ntl):
                P = k.bank()
                for kk in range(8):
                    k.mm(P[:, :], hT[:, kk, i * 128:(i + 1) * 128], w[:, kk, :], kk == 0, kk == 7, [hT, w], P)
                if isg:
                    s = stf[si % 2]; si += 1
                    k.act(s[:], P[:, :], AF.Silu, [P], [s])
                    fw.dma(k.q(), sgt.t[i * 128:(i + 1) * 128, c0 - 1024:c0 - 512], s[:], s, sgt)
                else:
                    s = stb[si % 2]; si += 1
                    k.act(s[:], P[:, :], AF.Copy, [P], [s])
                    fw.dma(k.q(), vtk.t[i * 128:(i + 1) * 128, c0 - 2048:c0 - 1536], s[:], s, vtk)
        blocks = [(0, 512), (512, 512), (1024, 512), (1536, 512), (2048, 256)]
        ob = [fw.sbuf("hob%d" % i, [128, T], F32) for i in range(2)]
        ob2 = [fw.sbuf("hob2%d" % i, [128, T], F32) for i in range(2)]
        sgm = [fw.sbuf("sgm%d" % i, [128, 512], F32) for i in range(2)]
        oi = 0
        for grp in range(6):
            c0 = (0, 512, 3072, 3584, 4096, 4608)[grp]
            w = W[wi % 2]; wi += 1
            fw.dma("pool", w[:], _wsrc(win.t, c0, 512), win, w)
            for hh in range(4):
                h = (grp % 2) * 4 + hh
                o = ob[oi % 2]; o2 = ob2[oi % 2]; oi += 1
                for (t0, n) in (blocks[:4] if grp < 2 else blocks):
                    P = k.bank()
                    for kk in range(8):
                        k.mm(P[:, 0:n], w[:, kk, hh * 128:(hh + 1) * 128], hT[:, kk, t0:t0 + n], kk == 0, kk == 7, [hT, w], P)
                    if grp < 2:
                        k.act(o[:, t0:t0 + n], P[:, 0:n], AF.Silu, [P], [o])
                    else:
                        sg_ = sgm[si % 2]; si += 1
                        k.act(sg_[:, 0:n], P[:, 0:n], AF.Sigmoid, [P], [sg_])
                        k.ts(o[:, t0:t0 + n], sg_[:, 0:n], oml[:, h:h + 1], lb[:, h:h + 1], ALU.mult, ALU.add, [sg_, oml, lb], [o])
                        k.ts(o2[:, t0:t0 + n], o[:, t0:t0 + n], -1.0, 1.0, ALU.mult, ALU.add, [o], [o2])
                if grp < 2:
                    fw.dma(k.q(), QT.t[h], o[:, 0:TL], o, QT)
                else:
                    dd = (grp - 2) // 2
                    fw.dma(k.q(), KT.t[dd, h], o2[:], o2, KT)
                    k.act(o[:], o[:], AF.Ln, [o], [o])
                    fw.dma(k.q(), LF.t[dd, h], o[:], o, LF)
        fw.pop()

    def phase_hgscan(self):
        fw, k, dr = self.fw, self.k, self.dr
        QT, LF, KT, vtk = dr["QT"], dr["LF"], dr["KT"], dr["v_tok"]
        otot = self.scratch("o_tot", [TL, D])
        fw.push()
        NCK = T // 64
        rst = fw.sbuf("rst", [128, T], F32)
        k.memset(rst[:], 1.0, [rst])
        k.memset(rst[:].rearrange("p (c l) -> p c l", l=64)[:, :, 0:1], 0.0, [rst])
        Vh = [fw.sbuf("Vh%d" % i, [64, NCK, 128], BF16) for i in range(1)] * 2
        qb = [fw.sbuf("hq%d" % i, [128, TL], F32) for i in range(1)] * 2
        lf = [fw.sbuf("hlf%d" % i, [128, T], F32) for i in range(1)] * 2
        kt = [fw.sbuf("hkt%d" % i, [128, T], F32) for i in range(1)] * 2
        G = fw.sbuf("hG", [128, T], F32)
        D1 = fw.sbuf("hD1", [128, T], F32)
        D2 = fw.sbuf("hD2", [128, T], F32)
        E = [fw.sbuf("hE%d" % i, [128, T], F32) for i in range(4)]
        qrel = [fw.sbuf("qrel%d" % i, [128, TL], BF16) for i in range(2)]
        krel = [fw.sbuf("krel%d" % i, [128, TL], BF16) for i in range(2)]
        qdec = [fw.sbuf("qdec%d" % i, [128, TL], BF16) for i in range(2)]
        kend = [fw.sbuf("kend%d" % i, [128, T], BF16) for i in range(2)]
        dec = [fw.sbuf("hdec%d" % i, [128, NCK], F32) for i in range(2)]
        S32 = [fw.sbuf("hS32%d" % i, [128, 128], F32) for i in range(2)]
        Sbf = [fw.sbuf("hSbf%d" % i, [128, 128], BF16) for i in range(2)]
        ktok = [fw.sbuf("ktok%d" % i, [64, NCK, 128], BF16) for i in range(2)]
        attT = [fw.sbuf("attT%d" % i, [64, 32, 64], BF16) for i in range(2)]
        Oall = [fw.sbuf("Oall%d" % i, [64, 32, 128], F32) for i in range(2)]
        identb = self.C("ident", b16=True)
        c3 = lambda ap: ap.rearrange("p (c l) -> p c l", l=64)
        for h in range(8):
            V = Vh[h % 2]; q = qb[h % 2]
            fw.dma("sp", V[:], vtk.t[:, h * 128:(h + 1) * 128].rearrange("(c p) v -> p c v", p=64), vtk, V)
            fw.dma("act", q[:], QT.t[h], QT, q)
            for dd in range(2):
                l_, k_ = lf[dd], kt[dd]
                fw.dma("sp", l_[:], LF.t[dd, h], LF, l_)
                fw.dma("act", k_[:], KT.t[dd, h], KT, k_)
                mid, tot = (31, 63) if dd == 0 else (32, 0)
                fw.op("dve", lambda e: e.tensor_tensor_scan(G[:], rst[:], l_[:], 0.0, ALU.mult, ALU.add), [rst, l_], [G])
                if dd == 1:
                    k.tt(D1[:], l_[:], G[:], ALU.subtract, [l_, G], [D1], eng="pool")
                    k.tt(c3(G[:]), c3(D1[:]), bc(c3(G[:])[:, :, 63:64], [128, NCK, 64]), ALU.add, [D1, G], [G])
                Gm = bc(c3(G[:])[:, :, mid:mid + 1], [128, NCK, 64])
                Gt = bc(c3(G[:])[:, :, tot:tot + 1], [128, NCK, 64])
                E0, E1, E2, E3 = E
                k.tt(c3(D1[:]), c3(G[:]), Gm, ALU.subtract, [G], [D1])
                k.tt(c3(D2[:]), Gt, c3(G[:]), ALU.subtract, [G], [D2])
                k.act(E0[:, 0:TL], D1[:, 0:TL], AF.Exp, [D1], [E0])
                k.act(E1[:, 0:TL], D1[:, 0:TL], AF.Exp, [D1], [E1], scale=-1.0)
                k.act(E2[:], D2[:], AF.Exp, [D2], [E2])
                k.act(E3[:, 0:TL], G[:, 0:TL], AF.Exp, [G], [E3])
                k.tt(qrel[dd][:], q[:], E0[:, 0:TL], ALU.mult, [q, E0], [qrel[dd]])
                k.tt(krel[dd][:], k_[:, 0:TL], E1[:, 0:TL], ALU.mult, [k_, E1], [krel[dd]], eng="pool")
                k.tt(kend[dd][:], k_[:], E2[:], ALU.mult, [k_, E2], [kend[dd]])
                k.tt(qdec[dd][:], q[:], E3[:, 0:TL], ALU.mult, [q, E3], [qdec[dd]], eng="pool")
                k.act(dec[dd][:], c3(G[:])[:, :, tot], AF.Exp, [G], [dec[dd]])
                k.memset(S32[dd][:], 0.0, [S32[dd]])
                k.memset(Sbf[dd][:], 0.0, [Sbf[dd]])
                for c0 in range(0, NCK, 8):
                    nb = min(8, NCK - c0)
                    Pt = k.bank()
                    Ptb = Pt[:, :].bitcast(BF16)
                    for j in range(nb):
                        k.tr(Ptb[0:64, j * 128:(j + 1) * 128], kend[dd][:, (c0 + j) * 64:(c0 + j + 1) * 64], identb,
                             [kend[dd], self.conb], Pt, inc=(j == nb - 1), first=(j == 0))
                    k.act(ktok[dd][:, c0:c0 + nb, :].rearrange("p a b -> p (a b)"), Ptb[0:64, 0:nb * 128], AF.Copy, [Pt], [ktok[dd]])
                mask = self.C("mF64" if dd == 0 else "mB64", w=64, rows=64)
                for c0 in range(0, 32, 8):
                    Pa = k.bank()
                    for j in range(8):
                        c = c0 + j
                        k.mm(Pa[0:64, j * 64:(j + 1) * 64], krel[dd][:, c * 64:(c + 1) * 64], qrel[dd][:, c * 64:(c + 1) * 64],
                             True, True, [krel[dd], qrel[dd]], Pa, inc=(j == 7))
                    k.tt(attT[dd][:, c0:c0 + 8, :], Pa[0:64, :].rearrange("p (a b) -> p a b", b=64),
                         bc(mask.unsqueeze(1), [64, 8, 64]), ALU.mult, [Pa, self.con], [attT[dd]])
            orders = [list(range(32, 36)) + list(range(32)), list(range(35, 31, -1)) + list(range(31, -1, -1))]
            Po = [None, None]
            Pcn = [None, None]

            def emit_pc(dd, step):
                c_ = orders[dd][step]
                Pn = k.bank()
                k.mm(Pn[:, 0:128], ktok[dd][:, c_, :], V[:, c_, :], True, True, [ktok[dd], V], Pn)
                Pcn[dd] = Pn
            for dd in range(2):
                emit_pc(dd, 0)
            for step in range(NCK):
                for dd in range(2):
                    c = orders[dd][step]
                    lat = c < 32
                    Pc = Pcn[dd]
                    if step + 1 < NCK:
                        emit_pc(dd, step + 1)
                    if lat:
                        j = c % 4
                        first = (j == 0) if dd == 0 else (j == 3)
                        last_ = (j == 3) if dd == 0 else (j == 0)
                        if first:
                            Po[dd] = k.bank()
                        P = Po[dd]
                        k.mm(P[0:64, j * 128:(j + 1) * 128], attT[dd][:, c, :], V[:, c, :], True, False, [attT[dd], V], P)
                        k.mm(P[0:64, j * 128:(j + 1) * 128], qdec[dd][:, c * 64:(c + 1) * 64], Sbf[dd][:], False, True,
                             [qdec[dd], Sbf[dd]], P, inc=True)
                        if last_:
                            cg = c - j
                            k.act(Oall[dd][:, cg:cg + 4, :].rearrange("p a b -> p (a b)"), P[0:64, :], AF.Copy, [P], [Oall[dd]])
                    k.stt(S32[dd][:], S32[dd][:], dec[dd][:, c:c + 1], Pc[:, 0:128], ALU.mult, ALU.add, [S32[dd], dec[dd], Pc], [S32[dd]])
                    k.cp(Sbf[dd][:], S32[dd][:], [S32[dd]], [Sbf[dd]])
            k.tt(Oall[0][:, 0:16, :], Oall[0][:, 0:16, :], Oall[1][:, 0:16, :], ALU.add, [Oall[0], Oall[1]], [Oall[0]])
            k.tt(Oall[0][:, 16:32, :], Oall[0][:, 16:32, :], Oall[1][:, 16:32, :], ALU.add, [Oall[0], Oall[1]], [Oall[0]], eng="pool")
            for half in range(2):
                fw.dma(k.q(), otot.t[half * 1024:(half + 1) * 1024, h * 128:(h + 1) * 128].rearrange("(c p) v -> p c v", p=64),
                       Oall[0][:, half * 16:(half + 1) * 16, :], Oall[0], otot)
        fw.pop()

    def phase_hgout(self):
        fw, k, dr = self.fw, self.k, self.dr
        otot, sgt = dr["o_tot"], dr["sg_tok"]
        oT2 = self.scratch("oT2", [8, 128, T], BF16)
        fw.push()
        gb = fw.sbuf("hgng", [128, 128], F32)
        fw.dma("sp", gb[:], self.rowbc("hgng", 128), dr["rows"], gb)
        O = [fw.sbuf("hO%d" % i, [128, 8, 128], F32) for i in range(2)]
        SG = [fw.sbuf("hSG%d" % i, [128, 8, 128], F32) for i in range(2)]
        sq2 = [fw.sbuf("hsq%d" % i, [128, 8, 128], F32) for i in range(2)]
        ss2 = [fw.sbuf("hss%d" % i, [128, 8], F32) for i in range(2)]
        on2 = [fw.sbuf("hon%d" % i, [128, 8, 128], BF16) for i in range(2)]
        stg = [fw.sbuf("hstg%d" % i, [128, 8, 128], BF16) for i in range(2)]
        identb = self.C("ident", b16=True)
        f2 = lambda b_: b_[:].rearrange("p a b -> p (a b)")
        for i in range(16):
            o, sg = O[i % 2], SG[i % 2]
            sq, ss, on = sq2[i % 2], ss2[i % 2], on2[i % 2]
            fw.dma("sp", f2(o), otot.t[i * 128:(i + 1) * 128, :], otot, o)
            fw.dma("act", f2(sg), sgt.t[i * 128:(i + 1) * 128, :], sgt, sg)
            k.tt(sq[:], o[:], o[:], ALU.mult, [o], [sq])
            fw.op("dve", lambda e: e.tensor_reduce(ss[:], sq[:], AX.X, ALU.add), [sq], [ss])
            k.act(ss[:], ss[:], AF.Sqrt, [ss], [ss], scale=1.0 / 128, bias=self.epsb[:, 0:1])
            fw.op("dve", lambda e: e.reciprocal(ss[:], ss[:]), [ss], [ss])
            k.tt(o[:], o[:], bc(ss[:].unsqueeze(2), [128, 8, 128]), ALU.mult, [o, ss], [o])
            k.tt(o[:], o[:], bc(gb[:].unsqueeze(1), [128, 8, 128]), ALU.mult, [o, gb], [o])
            k.tt(on[:], o[:], sg[:], ALU.mult, [o, sg], [on])
            P = k.bank()
            Pb = P[:, :].bitcast(BF16)
            for c in range(8):
                k.tr(Pb[:, c * 128:(c + 1) * 128], on[:, c, :], identb, [on, self.conb], P, inc=(c == 7), first=(c == 0))
            s = stg[i % 2]
            k.act(f2(s), Pb, AF.Copy, [P], [s])
            fw.dma("sp", oT2.t[:, :, i * 128:(i + 1) * 128].rearrange("c p t -> p c t"), s[:], s, oT2)
        fw.pop()

    def build_full(self):
        fw, dr = self.fw, self.dr
        self.declare_io()
        out = self.scratch("out", [TL, D], out=True)
        self.setup_consts()
        self.phase_mod(0)
        fw.push()
        hT = fw.sbuf("hT", [128, 8, T], BF16)
        self.phase_normmod0(hT)
        self.phase_evenproj(hT)
        fw.pop()
        self.phase_ssd()
        self.phase_conformer()
        fw.push()
        hT2 = fw.sbuf("hT2", [128, 8, T], BF16)
        xsrc0 = lambda i: [(0, 128, dr["x"], dr["x"].t[i * 128:(i + 1) * 128, :])] if i < 16 else \
            [(0, 128, dr["ctx"], dr["ctx"].t[(i - 16) * 128:(i - 15) * 128, :])]
        self.phase_outproj(0, dr["oT"], 16, "ab_w_out", xsrc0, hT2, "xmid0", list(range(NT)))
        x1 = self.scratch("x1", [T, D])
        self.phase_moe(0, hT2, "xmid0", x1, list(range(NT)),
                       hook=(lambda: self.mod_setup(1), self.mod_group, self.mod_finish))
        fw.pop()
        fw.push()
        hT = fw.sbuf("hTb", [128, 8, T], BF16)
        self.phase_normmod1(hT, x1)
        self.phase_hgproj(hT)
        fw.pop()
        self.phase_hgscan()
        self.phase_hgout()
        fw.push()
        hT2 = fw.sbuf("hT2b", [128, 8, T], BF16)
        self.phase_outproj(1, dr["oT2"], 8, "hg_w_out", lambda i: self.cm_rows(x1, i), hT2, "xmid1", list(range(16)))
        self.phase_moe(1, hT2, "xmid1", out, list(range(16)), final=True)
        fw.pop()
        fw.barrier()
        st = simulate(fw)
        assert not st, "sync deadlock: %r" % (st,)
        return self.nc


_CACHE = {}


def kernel(**inputs):
    n = 8
    if "nc" not in _CACHE:
        _CACHE["nc"] = Prog5().build_full()
    nc = _CACHE["nc"]
    maps = make_inmaps(inputs, list(range(n)))
    res = run_bass_kernel_spmd(nc, maps, core_ids=list(range(n)))
    return np.stack([np.asarray(r["out"], np.float32) for r in res.results], axis=0)
```

```python
import numpy as np
from contextlib import ExitStack
import concourse.bass as bass
import concourse.mybir as mybir
from concourse.bass_utils import run_bass_kernel_spmd

F32 = mybir.dt.float32
BF16 = mybir.dt.bfloat16
AF = mybir.ActivationFunctionType
ALU = mybir.AluOpType
AX = mybir.AxisListType

D = 1024
TL = 2048
TC = 256
T = TL + TC
NT = T // 128
EPS = 1e-6
NEG = -30000.0


class Buf:
    __slots__ = ("name", "t", "writer", "readers", "ld", "st", "onchip", "excl")

    def __init__(self, name, t, onchip):
        self.name = name
        self.t = t
        self.writer = None
        self.readers = []
        self.ld = None
        self.st = None
        self.onchip = onchip
        self.excl = False

    def __getitem__(self, k):
        return self.t[k]


class FW:
    ENG = ("pe", "act", "dve", "pool", "sp")

    def __init__(self, nc, es, n_dma_sems=90):
        self.nc = nc
        self.es = es
        self.scopes = [es]
        self.eng = {"pe": nc.tensor, "act": nc.scalar, "dve": nc.vector, "pool": nc.gpsimd, "sp": nc.sync}
        self.sems = {}
        self.cnt = {}
        for e in self.ENG:
            self.sems[e] = es.enter_context(nc.semaphore("s_" + e))
            self.cnt[e] = 0
        self.free_dma = []
        for i in range(n_dma_sems):
            k = "d%d" % i
            self.sems[k] = es.enter_context(nc.semaphore("s_" + k))
            self.cnt[k] = 0
            self.free_dma.append(k)
        self.phase_dma = []
        self.seen = {e: {} for e in self.ENG}
        self.bufs = []
        self.n_inst = 0
        self.uid = 0
        self.log = {e: [] for e in self.ENG}

    def push(self):
        s = ExitStack()
        self.scopes.append(s)
        s._bufs0 = len(self.bufs)

    def pop(self):
        self.barrier()
        s = self.scopes.pop()
        del self.bufs[s._bufs0:]
        s.close()

    def _nm(self, name):
        self.uid += 1
        return "%s_%d" % (name, self.uid)

    def sbuf(self, name, shape, dtype):
        t = self.scopes[-1].enter_context(self.nc.sbuf_tensor(self._nm(name), list(shape), dtype))
        b = Buf(name, t, True)
        self.bufs.append(b)
        return b

    def psum(self, name, shape, dtype):
        t = self.scopes[-1].enter_context(self.nc.psum_tensor(self._nm(name), list(shape), dtype))
        b = Buf(name, t, True)
        b.excl = True
        self.bufs.append(b)
        return b

    def dram(self, name, t):
        b = Buf(name, t, False)
        self.bufs.append(b)
        return b

    def _need(self, e, tok, waits):
        if tok is None:
            return
        k, v = tok
        if self.seen[e].get(k, 0) >= v:
            return
        if waits.get(k, 0) < v:
            waits[k] = v

    def _deps(self, e, reads, writes, pe_accum=False, attach=False):
        waits = {}
        for b in reads:
            self._need(e, b.writer, waits)
            if b.excl:
                for r in b.readers:
                    if r[0] != e:
                        self._need(e, r, waits)
        for b in writes:
            if not (e == "pe" and b.writer is not None and b.writer[0] == "pe"):
                self._need(e, b.writer, waits)
            for r in b.readers:
                self._need(e, r, waits)
        items = list(waits.items())
        self.pending = None
        if attach and items:
            self.pending = items.pop()
        for k, v in items:
            self.eng[e].wait_ge(self.sems[k], v)
            self.seen[e][k] = v
            self.log[e].append(("w", k, v))
        if self.pending is not None:
            k, v = self.pending
            self.seen[e][k] = v
            self.log[e].append(("w", k, v))

    def op(self, e, fn, reads=(), writes=(), inc=True, pe_accum=False):
        self._deps(e, reads, writes, pe_accum, attach=True)
        ins = fn(self.eng[e])
        if self.pending is not None:
            ins._wait_ge(self.sems[self.pending[0]], self.pending[1])
        self.n_inst += 1
        tok = (e, self.cnt[e] + 1)
        if inc:
            self.cnt[e] += 1
            ins.then_inc(self.sems[e], 1)
            self.log[e].append(("i", e, 1))
        for b in reads:
            b.readers.append(tok)
            if len(b.readers) > 24:
                b.readers = _compact(b.readers)
        for b in writes:
            b.writer = tok
            b.readers = []
        return ins

    def _dma_sem(self):
        k = self.free_dma.pop()
        self.phase_dma.append(k)
        return k

    def dma(self, q, out_ap, in_ap, src, dst, **kw):
        self._deps(q, [src], [dst], attach=True)
        pend = self.pending
        if dst.onchip:
            if dst.ld is None:
                dst.ld = self._dma_sem()
            k = dst.ld
        else:
            if src.st is None:
                src.st = self._dma_sem()
            k = src.st
        self.cnt[k] += 16
        ins = self.eng[q].dma_start(out=out_ap, in_=in_ap, **kw)
        if pend is not None:
            ins._wait_ge(self.sems[pend[0]], pend[1])
        ins.then_inc(self.sems[k], 16)
        self.log[q].append(("i", k, 16))
        self.n_inst += 1
        tok = (k, self.cnt[k])
        src.readers.append(tok)
        if len(src.readers) > 24:
            src.readers = _compact(src.readers)
        if dst.onchip:
            dst.writer = tok
            dst.readers = []
        return ins

    def barrier(self):
        sp = self.eng["sp"]
        keys = [k for k in self.ENG if k != "sp"] + self.phase_dma
        for k in keys:
            v = self.cnt[k]
            if v > 0 and self.seen["sp"].get(k, 0) < v:
                sp.wait_ge(self.sems[k], v)
                self.seen["sp"][k] = v
                self.log["sp"].append(("w", k, v))
        self.cnt["sp"] += 1
        sp.nop().then_inc(self.sems["sp"], 1)
        self.log["sp"].append(("i", "sp", 1))
        v = self.cnt["sp"]
        for e in self.ENG:
            if e == "sp":
                continue
            self.log[e].append(("w", "sp", v))
            self.eng[e].wait_ge(self.sems["sp"], v)
            self.seen[e]["sp"] = v
            for k in keys:
                self.seen[e][k] = self.cnt[k]
        for b in self.bufs:
            b.writer = None
            b.readers = []
            b.ld = None
            b.st = None
        self.free_dma.extend(self.phase_dma)
        self.phase_dma = []


def simulate(fw):
    pos = {e: 0 for e in fw.ENG}
    val = {}
    prog = True
    while prog:
        prog = False
        for e in fw.ENG:
            L = fw.log[e]
            while pos[e] < len(L):
                kind, k, v = L[pos[e]]
                if kind == "w":
                    if val.get(k, 0) < v:
                        break
                else:
                    val[k] = val.get(k, 0) + v
                pos[e] += 1
                prog = True
    stuck = {e: (pos[e], len(fw.log[e]), fw.log[e][pos[e]], val.get(fw.log[e][pos[e]][1], 0))
             for e in fw.ENG if pos[e] < len(fw.log[e])}
    return stuck


def _compact(toks):
    m = {}
    for k, v in toks:
        if m.get(k, 0) < v:
            m[k] = v
    return list(m.items())


class KB:
    def __init__(self, nc, fw):
        self.nc = nc
        self.fw = fw
        self.banks = [fw.psum("bank%d" % i, [128, 512], F32) for i in range(8)]
        self.bi = 0
        self.dq = 0

    def bank(self):
        b = self.banks[self.bi]
        self.bi = (self.bi + 1) % 8
        return b

    def q(self):
        self.dq ^= 1
        return "sp" if self.dq else "act"

    def mm(self, out, lhsT, rhs, start, stop, reads, wr, inc=None):
        if inc is None:
            inc = stop
        return self.fw.op("pe", lambda e: e.matmul(out, lhsT, rhs, start=start, stop=stop),
                          reads=reads, writes=[wr], inc=inc, pe_accum=not start)

    def tr(self, out, in_, ident, reads, wr, inc=True, first=False):
        return self.fw.op("pe", lambda e: e.transpose(out, in_, ident), reads=reads, writes=[wr],
                          inc=inc, pe_accum=not first)

    def act(self, out, in_, func, reads, writes, **kw):
        return self.fw.op("act", lambda e: e.activation(out, in_, func, **kw), reads=reads, writes=writes)

    def tt(self, out, a, b, op, reads, writes, eng="dve"):
        return self.fw.op(eng, lambda e: e.tensor_tensor(out, a, b, op), reads=reads, writes=writes)

    def ts(self, out, a, s1, s2, op0, op1, reads, writes, eng="dve", **kw):
        return self.fw.op(eng, lambda e: e.tensor_scalar(out, a, s1, s2, op0, op1, **kw), reads=reads, writes=writes)

    def stt(self, out, a, s, b, op0, op1, reads, writes):
        return self.fw.op("dve", lambda e: e.scalar_tensor_tensor(out, a, s, b, op0, op1), reads=reads, writes=writes)

    def cp(self, out, a, reads, writes, eng="dve"):
        return self.fw.op(eng, lambda e: e.tensor_copy(out, a), reads=reads, writes=writes)

    def memset(self, ap, val, writes, eng="pool"):
        return self.fw.op(eng, lambda e: e.memset(ap, val), reads=[], writes=writes)


def bc(ap, shape):
    return ap.broadcast_to(list(shape))


VEC_SPEC = [("modb0", 48), ("modb1", 48), ("gmix0", 8), ("gmix1", 8), ("gffn0", 8), ("gffn1", 8),
            ("convw", 80), ("convb", 16), ("ssdg", 8), ("cfw", 248), ("cfb", 8), ("cflg", 8), ("cflb", 8),
            ("hglb0", 8), ("hglb1", 8)]
VOFF = {}
_o = 0
for _n, _c in VEC_SPEC:
    VOFF[_n] = _o
    _o += _c
NV = _o
ROW_SPEC = [("modb0", 6144), ("modb1", 6144), ("dtb", 32), ("alog", 32), ("ssdd", 16), ("rb", 16),
            ("fng", 1024), ("hgng", 128)]
ROFF = {}
_o = 0
for _n, _c in ROW_SPEC:
    ROFF[_n] = _o
    _o += _c
NR = _o
CONST_SPEC = [("ident", 128), ("triU", 128), ("triL", 128), ("negF", 128), ("negB", 128), ("ones", 128),
              ("mF64", 64), ("mB64", 64)]
COFF = {}
_o = 0
for _n, _c in CONST_SPEC:
    COFF[_n] = _o
    _o += _c
NCON = _o


def _col(v):
    v = np.asarray(v, np.float32).reshape(-1, 128)
    return np.ascontiguousarray(v.T)


def pack_shared(I):
    vec = np.zeros((128, NV), np.float32)

    def put(n, a):
        vec[:, VOFF[n]:VOFF[n] + a.shape[1]] = a
    put("modb0", _col(I["mod_b"][0])); put("modb1", _col(I["mod_b"][1]))
    put("gmix0", _col(I["norm_mix_g"][0])); put("gmix1", _col(I["norm_mix_g"][1]))
    put("gffn0", _col(I["norm_ffn_g"][0])); put("gffn1", _col(I["norm_ffn_g"][1]))
    cw = np.asarray(I["ssd_conv_w"][0], np.float32)
    put("convw", np.ascontiguousarray(cw.reshape(5, 16, 128).transpose(2, 1, 0)).reshape(128, 80))
    put("convb", _col(I["ssd_conv_b"][0]))
    put("ssdg", _col(I["ssd_norm_g"][0]))
    fw_ = np.asarray(I["cf_dw_w"][0], np.float32)
    put("cfw", np.ascontiguousarray(fw_.reshape(31, 8, 128).transpose(2, 1, 0)).reshape(128, 248))
    put("cfb", _col(I["cf_dw_b"][0])); put("cflg", _col(I["cf_ln_g"][0])); put("cflb", _col(I["cf_ln_b"][0]))
    put("hglb0", _col(I["hg_lb"][0])); put("hglb1", _col(I["hg_lb"][1]))
    row = np.zeros((1, NR), np.float32)

    def putr(n, a):
        a = np.asarray(a, np.float32).reshape(-1)
        row[0, ROFF[n]:ROFF[n] + a.size] = a
    putr("modb0", I["mod_b"][0]); putr("modb1", I["mod_b"][1]); putr("dtb", I["ssd_dt_bias"][0])
    putr("alog", I["ssd_a_log"][0]); putr("ssdd", I["ssd_d"][0]); putr("rb", I["router_b"])
    putr("fng", I["final_norm_g"]); putr("hgng", I["hg_norm_g"][0])
    con = np.zeros((128, NCON), np.float32)
    i = np.arange(128)
    k, l = i[:, None], i[None, :]

    def putc(n, a):
        con[:a.shape[0], COFF[n]:COFF[n] + a.shape[1]] = a
    putc("ident", (k == l).astype(np.float32))
    putc("triU", (k <= l).astype(np.float32))
    putc("triL", (k >= l).astype(np.float32))
    putc("negF", np.where(k <= l, 0.0, NEG).astype(np.float32))
    putc("negB", np.where(k >= l, 0.0, NEG).astype(np.float32))
    putc("ones", np.ones((128, 128), np.float32))
    putc("mF64", (k[:64] <= l[:, :64]).astype(np.float32))
    putc("mB64", (k[:64] >= l[:, :64]).astype(np.float32))
    return vec, row, con


class Prog:
    def __init__(self, dbg=()):
        self.dbg = set(dbg)
        nc = self.nc = bass.Bass("TRN2", target_bir_lowering=False)
        self.es = ExitStack()
        self.fw = FW(nc, self.es)
        self.k = KB(nc, self.fw)
        self.dr = {}

    def inp(self, name, shape, dtype=F32):
        t = self.nc.dram_tensor(name, list(shape), dtype, kind="ExternalInput")
        self.dr[name] = self.fw.dram(name, t)
        return self.dr[name]

    def scratch(self, name, shape, dtype=F32, out=False):
        kind = "ExternalOutput" if (out or name in self.dbg) else "Internal"
        t = self.nc.dram_tensor(name, list(shape), dtype, kind=kind)
        self.dr[name] = self.fw.dram(name, t)
        return self.dr[name]

    def declare_io(self):
        self.inp("x", [TL, D]); self.inp("ctx", [TC, D]); self.inp("cvec", [128, 8, 2])
        self.inp("vecs", [128, NV]); self.inp("rows", [1, NR]); self.inp("consts", [128, NCON])
        self.inp("mod_w", [2, D, 6 * D]); self.inp("router_w", [D, 16])
        self.inp("moe_w_gate", [2, 16, D, 512]); self.inp("moe_w_up", [2, 16, D, 512])
        self.inp("moe_w_down", [2, 16, 512, D])
        self.inp("ab_w_in", [D, 5152]); self.inp("ab_w_out", [2048, D])
        self.inp("hg_w_in", [D, 5120]); self.inp("hg_w_out", [D, D])

    def setup_consts(self):
        fw, k = self.fw, self.k
        self.con = fw.sbuf("con", [128, NCON], F32)
        fw.dma("sp", self.con[:], self.dr["consts"][:], self.dr["consts"], self.con)
        self.conb = fw.sbuf("conb", [128, NCON], BF16)
        k.cp(self.conb[:], self.con[:], [self.con], [self.conb])
        self.vec = fw.sbuf("vec", [128, NV], F32)
        fw.dma("act", self.vec[:], self.dr["vecs"][:], self.dr["vecs"], self.vec)

    def C(self, n, w=128, rows=128, b16=False):
        t = self.conb if b16 else self.con
        return t[0:rows, COFF[n]:COFF[n] + w]

    def V(self, n, j=0, w=1):
        return self.vec[:, VOFF[n] + j:VOFF[n] + j + w]

    def rowbc(self, n, w, off=0):
        return self.dr["rows"].t[0, ROFF[n] + off:ROFF[n] + off + w].partition_broadcast(128)

    fw_mod_double = True

    def phase_mod(self, l):
        self.fw.push()
        self.fw_mod_double = True
        st = self.mod_setup(l)
        for g in range(12):
            self.mod_group(st, g)
        self.mod_finish(st)
        self.fw.pop()

    def mod_setup(self, l):
        fw, k, dr = self.fw, self.k, self.dr
        modrow = self.scratch("modrow%d" % l, [2, 6 * D])
        modcol = self.scratch("modcol%d" % l, [128, 48, 2])
        cv = fw.sbuf("cv", [128, 8, 2], F32)
        fw.dma("sp", cv[:], dr["cvec"][:], dr["cvec"], cv)
        cs = fw.sbuf("cs", [128, 8, 2], BF16)
        k.act(cs[:], cv[:], AF.Silu, [cv], [cs])
        csb = fw.sbuf("csb", [128, 8, 128], BF16)
        k.cp(csb[:, :, 0:64], bc(cs[:, :, 0:1], [128, 8, 64]), [cs], [csb])
        k.cp(csb[:, :, 64:128], bc(cs[:, :, 1:2], [128, 8, 64]), [cs], [csb])
        nb_ = 2 if self.fw_mod_double else 1
        mb = [fw.sbuf("mb%d" % i, [128, 512], F32) for i in range(nb_)] * (3 - nb_)
        mcol = fw.sbuf("mcol", [128, 48, 2], F32)
        W = [fw.sbuf("modW%d" % i, [128, 8, 512], BF16) for i in range(nb_)] * (3 - nb_)
        rowb = [fw.sbuf("rowb%d" % i, [128, 512], F32) for i in range(nb_)] * (3 - nb_)
        wsrc = dr["mod_w"].t[l].rearrange("(k p) n -> p k n", p=128)
        return dict(l=l, modrow=modrow, modcol=modcol, cs=cs, csb=csb, mb=mb, mcol=mcol, W=W, rowb=rowb, wsrc=wsrc)

    def mod_group(self, st, g):
        fw, k, dr = self.fw, self.k, self.dr
        l, modrow, cs, csb, mcol = st["l"], st["modrow"], st["cs"], st["csb"], st["mcol"]
        w = st["W"][g % 2]
        fw.dma("pool", w[:], st["wsrc"][:, :, g * 512:(g + 1) * 512], dr["mod_w"], w)
        mb = st["mb"][g % 2]
        fw.dma("act", mb[:], self.rowbc("modb%d" % l, 512, off=g * 512), dr["rows"], mb)
        P = k.bank()
        for kk in range(8):
            k.mm(P[:, :], csb[:, kk, :], w[:, kk, :], kk == 0, kk == 7, [csb, w], P)
        rb = st["rowb"][g % 2]
        k.tt(rb[:], P[:, :], mb[:], ALU.add, [P, mb], [rb])
        fw.dma("sp", modrow.t[0:1, g * 512:(g + 1) * 512], rb[0:1, :], rb, modrow)
        fw.dma("act", modrow.t[1:2, g * 512:(g + 1) * 512], rb[64:65, :], rb, modrow)
        P2 = k.bank()
        for fc in range(4):
            for kk in range(8):
                k.mm(P2[:, fc * 2:fc * 2 + 2], w[:, kk, fc * 128:(fc + 1) * 128], cs[:, kk, :],
                     kk == 0, kk == 7, [cs, w], P2)
        k.tt(mcol[:, g * 4:(g + 1) * 4, :], P2[:, 0:8].rearrange("p (a b) -> p a b", b=2),
             bc(self.V("modb%d" % l, g * 4, 4).unsqueeze(2), [128, 4, 2]), ALU.add, [P2, self.vec], [mcol])

    def mod_finish(self, st):
        self.fw.dma("sp", st["modcol"].t[:], st["mcol"][:], st["mcol"], st["modcol"])


def make_inmaps(I, cores):
    vec, row, con = pack_shared(I)
    shared = {"vecs": vec, "rows": row, "consts": con}
    for n in ("mod_w", "router_w", "moe_w_gate", "moe_w_up", "moe_w_down", "hg_w_out"):
        shared[n] = np.ascontiguousarray(np.asarray(I[n], np.float32))
    shared["ab_w_in"] = np.ascontiguousarray(np.asarray(I["ab_w_in"][0], np.float32))
    shared["ab_w_out"] = np.ascontiguousarray(np.asarray(I["ab_w_out"][0], np.float32))
    shared["hg_w_in"] = np.ascontiguousarray(np.asarray(I["hg_w_in"][0], np.float32))
    shared["hg_w_out"] = np.ascontiguousarray(np.asarray(I["hg_w_out"][0], np.float32))
    maps = []
    for b in cores:
        m = dict(shared)
        m["x"] = np.ascontiguousarray(np.asarray(I["x"][b], np.float32))
        m["ctx"] = np.ascontiguousarray(np.asarray(I["ctx"][b], np.float32))
        cv = np.stack([_col(I["c"][b]), _col(I["c_ctx"])], axis=-1)
        m["cvec"] = np.ascontiguousarray(cv.astype(np.float32))
        maps.append(m)
    return maps


def _wsrc(dt, col0, ncols):
    return dt.rearrange("(k p) n -> p k n", p=128)[:, :, col0:col0 + ncols]


class Prog2(Prog):
    def load_AB(self, l, which, gname):
        fw, k = self.fw, self.k
        mc = fw.sbuf("mc", [128, 48, 2], F32)
        fw.dma("sp", mc[:], self.dr["modcol%d" % l].t[:], self.dr["modcol%d" % l], mc)
        AB = fw.sbuf("AB", [128, 2, 2, 8], F32)
        sh, sc = which
        for idx in range(2):
            k.ts(AB[:, idx, 0, :], mc[:, sc * 8:sc * 8 + 8, idx], 1.0, None, ALU.add, ALU.bypass, [mc], [AB])
            k.tt(AB[:, idx, 0, :], AB[:, idx, 0, :], self.V(gname, 0, 8), ALU.mult, [AB, self.vec], [AB])
            k.cp(AB[:, idx, 1, :], mc[:, sh * 8:sh * 8 + 8, idx], [mc], [AB])
        return AB

    def normmod_tile(self, X, AB, idx, hT, tok0, tmp):
        fw, k = self.fw, self.k
        junk, ss, xn, t3 = tmp
        k.act(junk[:], X[:], AF.Square, [X], [junk, ss], accum_out=ss[:, 0:1])
        k.act(ss[:, 1:2], ss[:, 0:1], AF.Sqrt, [ss], [ss], scale=1.0 / D, bias=self.epsb[:, 0:1])
        self.fw.op("dve", lambda e: e.reciprocal(ss[:, 2:3], ss[:, 1:2]), [ss], [ss])
        k.act(xn[:], X[:], AF.Copy, [X, ss], [xn], scale=ss[:, 2:3])
        P = k.bank()
        Pb = P[:, :].bitcast(BF16)
        for c in range(8):
            k.tr(Pb[:, c * 128:(c + 1) * 128], xn[:, c * 128:(c + 1) * 128], self.C("ident", b16=True),
                 [xn, self.conb], P, inc=(c == 7), first=(c == 0))
        Pv = Pb.rearrange("p (c t) -> p c t", t=128)
        k.tt(t3[:], Pv, bc(AB[:, idx, 0, :].unsqueeze(2), [128, 8, 128]), ALU.mult, [P, AB], [t3])
        k.tt(hT[:, :, tok0:tok0 + 128], t3[:], bc(AB[:, idx, 1, :].unsqueeze(2), [128, 8, 128]), ALU.add,
             [t3, AB], [hT])

    def norm_tmp(self):
        fw = self.fw
        return [(fw.sbuf("junk%d" % i, [128, D], BF16), fw.sbuf("ss%d" % i, [128, 4], F32), fw.sbuf("xn%d" % i, [128, D], BF16),
                 fw.sbuf("t3%d" % i, [128, 8, 128], F32)) for i in range(2)]

    def setup_consts(self):
        Prog.setup_consts(self)
        self.epsb = self.fw.sbuf("epsb", [128, 1], F32)
        self.k.memset(self.epsb[:], EPS, [self.epsb])

    def phase_normmod0(self, hT):
        fw, k, dr = self.fw, self.k, self.dr
        fw.push()
        AB = self.load_AB(0, (0, 1), "gmix0")
        tmp = self.norm_tmp()
        X = [fw.sbuf("X%d" % i, [128, D], F32) for i in range(2)]
        for i in range(NT):
            x = X[i % 2]
            if i < 16:
                fw.dma(k.q(), x[:], dr["x"].t[i * 128:(i + 1) * 128, :], dr["x"], x)
            else:
                fw.dma(k.q(), x[:], dr["ctx"].t[(i - 16) * 128:(i - 15) * 128, :], dr["ctx"], x)
            self.normmod_tile(x, AB, 0 if i < 16 else 1, hT, i * 128, tmp[i % 2])
        fw.pop()

    def phase_evenproj(self, hT):
        fw, k, dr = self.fw, self.k, self.dr
        win = dr["ab_w_in"]
        sz = self.scratch("sz_tok", [T, D])
        dtk = self.scratch("dt_tok", [T, 32])
        xtok = self.scratch("x_tok", [T, D], BF16)
        btok = self.scratch("b_tok", [T, 512], BF16)
        BT = self.scratch("BT", [4, 128, T], BF16)
        CT = self.scratch("CT", [4, 128, T], BF16)
        U = self.scratch("U_cf", [8, 128, T])
        fw.push()
        Wz = fw.sbuf("Wz", [128, 8, D], BF16)
        fw.dma("pool", Wz[:], _wsrc(win.t, 0, D), win, Wz)
        Wdt = fw.sbuf("Wdt", [128, 8, 32], BF16)
        fw.dma("pool", Wdt[:], _wsrc(win.t, 3072, 32), win, Wdt)
        dtb = fw.sbuf("dtb", [128, 32], F32)
        fw.dma("sp", dtb[:], self.rowbc("dtb", 32), dr["rows"], dtb)
        zs = [fw.sbuf("zs%d" % i, [128, D], F32) for i in range(2)]
        dts = [fw.sbuf("dts%d" % i, [128, 32], F32) for i in range(2)]
        for i in range(NT):
            z = zs[i % 2]
            for h in range(2):
                P = k.bank()
                for kk in range(8):
                    k.mm(P[:, :], hT[:, kk, i * 128:(i + 1) * 128], Wz[:, kk, h * 512:(h + 1) * 512],
                         kk == 0, kk == 7, [hT, Wz], P)
                k.act(z[:, h * 512:(h + 1) * 512], P[:, :], AF.Silu, [P], [z])
            fw.dma(k.q(), sz.t[i * 128:(i + 1) * 128, :], z[:], z, sz)
            P = k.bank()
            for kk in range(8):
                k.mm(P[:, 0:32], hT[:, kk, i * 128:(i + 1) * 128], Wdt[:, kk, :], kk == 0, kk == 7, [hT, Wdt], P)
            d = dts[i % 2]
            k.tt(d[:], P[:, 0:32], dtb[:], ALU.add, [P, dtb], [d])
            k.act(d[:], d[:], AF.Exp, [d], [d])
            k.act(d[:], d[:], AF.Ln, [d], [d], bias=1.0)
            fw.dma(k.q(), dtk.t[i * 128:(i + 1) * 128, :], d[:], d, dtk)
        Wg = [fw.sbuf("Wg%d" % i, [128, 8, 512], BF16) for i in range(2)]
        xinL = [fw.sbuf("xinL%d" % i, [128, TL + 4], F32) for i in range(2)]
        xinC = [fw.sbuf("xinC%d" % i, [128, TC + 4], F32) for i in range(2)]
        for b_ in xinL:
            k.memset(b_[:, 0:2], 0.0, [b_]); k.memset(b_[:, TL + 2:TL + 4], 0.0, [b_])
        for b_ in xinC:
            k.memset(b_[:, 0:2], 0.0, [b_]); k.memset(b_[:, TC + 2:TC + 4], 0.0, [b_])
        acc = [fw.sbuf("acc%d" % i, [128, T], F32) for i in range(2)]
        xc = [fw.sbuf("xc%d" % i, [128, T], BF16) for i in range(2)]
        stg = [fw.sbuf("stg%d" % i, [128, 4, 128], BF16) for i in range(2)]
        blocks = [(0, 512), (512, 512), (1024, 512), (1536, 512), (2048, 256)]
        si = 0
        for cc in range(16):
            if cc % 4 == 0:
                w = Wg[(cc // 4) % 2]
                fw.dma("pool", w[:], _wsrc(win.t, 1024 + cc * 128, 512), win, w)
            xl, xcx, a, o = xinL[cc % 2], xinC[cc % 2], acc[cc % 2], xc[cc % 2]
            for (t0, n) in blocks:
                P = k.bank()
                for kk in range(8):
                    k.mm(P[:, 0:n], w[:, kk, (cc % 4) * 128:(cc % 4 + 1) * 128], hT[:, kk, t0:t0 + n],
                         kk == 0, kk == 7, [hT, w], P)
                if t0 < TL:
                    k.act(xl[:, 2 + t0:2 + t0 + n], P[:, 0:n], AF.Copy, [P], [xl])
                else:
                    k.act(xcx[:, 2:2 + n], P[:, 0:n], AF.Copy, [P], [xcx])
            for (xi, lo, n) in ((xl, 0, TL), (xcx, TL, TC)):
                k.ts(a[:, lo:lo + n], xi[:, 0:n], self.V("convw", cc * 5), self.V("convb", cc), ALU.mult, ALU.add,
                     [xi, self.vec], [a])
                for kt in range(1, 5):
                    k.stt(a[:, lo:lo + n], xi[:, kt:kt + n], self.V("convw", cc * 5 + kt), a[:, lo:lo + n],
                          ALU.mult, ALU.add, [xi, self.vec, a], [a])
            k.act(o[:], a[:], AF.Silu, [a], [o])
            if cc >= 8:
                dst = BT if cc < 12 else CT
                fw.dma(k.q(), dst.t[cc % 4], o[:], o, dst)
            if cc < 12:
                for t4 in range(0, NT, 4):
                    nt = min(4, NT - t4)
                    P = k.bank()
                    Pb = P[:, :].bitcast(BF16)
                    for j in range(nt):
                        k.tr(Pb[:, j * 128:(j + 1) * 128], o[:, (t4 + j) * 128:(t4 + j + 1) * 128],
                             self.C("ident", b16=True), [o, self.conb], P, inc=(j == nt - 1), first=(j == 0))
                    s = stg[si % 2]; si += 1
                    k.cp(s[:, 0:nt, :], Pb[:, 0:nt * 128].rearrange("p (a c) -> p a c", c=128), [P], [s])
                    if cc < 8:
                        dd = xtok.t[t4 * 128:(t4 + nt) * 128, cc * 128:(cc + 1) * 128]
                        dbuf = xtok
                    else:
                        dd = btok.t[t4 * 128:(t4 + nt) * 128, (cc - 8) * 128:(cc - 7) * 128]
                        dbuf = btok
                    fw.dma(k.q(), dd.rearrange("(a p) c -> p a c", p=128), s[:, 0:nt, :], s, dbuf)
        ub = [fw.sbuf("ub%d" % i, [128, T], F32) for i in range(2)]
        sg = [fw.sbuf("sg%d" % i, [128, 512], F32) for i in range(2)]
        gi = 0
        for c in range(8):
            if c % 4 == 0:
                wv = Wg[0]; wg_ = Wg[1]
                fw.dma("pool", wv[:], _wsrc(win.t, 3104 + c * 128, 512), win, wv)
                fw.dma("pool", wg_[:], _wsrc(win.t, 4128 + c * 128, 512), win, wg_)
            u = ub[c % 2]
            for (t0, n) in blocks:
                Pv = k.bank(); Pg = k.bank()
                for kk in range(8):
                    k.mm(Pv[:, 0:n], wv[:, kk, (c % 4) * 128:(c % 4 + 1) * 128], hT[:, kk, t0:t0 + n],
                         kk == 0, kk == 7, [hT, wv], Pv)
                for kk in range(8):
                    k.mm(Pg[:, 0:n], wg_[:, kk, (c % 4) * 128:(c % 4 + 1) * 128], hT[:, kk, t0:t0 + n],
                         kk == 0, kk == 7, [hT, wg_], Pg)
                s = sg[gi % 2]; gi += 1
                k.act(s[:, 0:n], Pg[:, 0:n], AF.Sigmoid, [Pg], [s])
                k.tt(u[:, t0:t0 + n], Pv[:, 0:n], s[:, 0:n], ALU.mult, [Pv, s], [u])
            fw.dma(k.q(), U.t[c], u[:], u, U)
        fw.pop()


class Prog3(Prog2):
    def phase_ssd(self):
        fw, k, dr = self.fw, self.k, self.dr
        xtok, btok, BT, CT, dtk, sz = (dr[n] for n in ("x_tok", "b_tok", "BT", "CT", "dt_tok", "sz_tok"))
        yf = self.scratch("yf_tok", [T, D])
        oT = self.scratch("oT", [16, 128, T], BF16)
        fw.push()
        al = fw.sbuf("al", [128, 32], F32)
        fw.dma("sp", al[:], self.rowbc("alog", 32), dr["rows"], al)
        aneg = fw.sbuf("aneg", [128, 32], F32)
        k.act(aneg[:], al[:], AF.Exp, [al], [aneg])
        k.ts(aneg[:], aneg[:], -1.0, None, ALU.mult, ALU.bypass, [aneg], [aneg])
        dsk = fw.sbuf("dsk", [128, 16], F32)
        fw.dma("act", dsk[:], self.rowbc("ssdd", 16), dr["rows"], dsk)
        S32 = fw.sbuf("S32", [128, D], F32)
        Sbf = fw.sbuf("Sbf", [128, D], BF16)
        Xt = [fw.sbuf("Xt%d" % i, [128, D], BF16) for i in range(2)]
        Bt = [fw.sbuf("Bt%d" % i, [128, 512], BF16) for i in range(2)]
        BTc = [fw.sbuf("BTc%d" % i, [128, 4, 128], BF16) for i in range(2)]
        CTc = [fw.sbuf("CTc%d" % i, [128, 4, 128], BF16) for i in range(2)]
        dtc = [fw.sbuf("dtc%d" % i, [128, 16], F32) for i in range(2)]
        WK = []
        for i in range(2):
            WK.append(dict(
                la=fw.sbuf("la%d" % i, [128, 16], F32), cum=fw.sbuf("cum%d" % i, [128, 16], F32),
                rhs2=fw.sbuf("rhs2%d" % i, [128, 16, 128], F32), Dm=fw.sbuf("Dm%d" % i, [128, 16, 128], F32),
                Eb=fw.sbuf("Eb%d" % i, [128, 16, 128], BF16), cbT=fw.sbuf("cbT%d" % i, [128, 4, 128], BF16),
                M=fw.sbuf("M%d" % i, [128, 16, 128], BF16), eR=fw.sbuf("eR%d" % i, [128, 16, 128], BF16),
                ECT=fw.sbuf("ECT%d" % i, [128, 16, 128], BF16), Rl=fw.sbuf("Rl%d" % i, [128, 16], F32),
                t16=fw.sbuf("t16%d" % i, [128, 16], F32), te=fw.sbuf("te%d" % i, [128, 16], F32),
                cd=fw.sbuf("cd%d" % i, [128, 16], F32), w2=fw.sbuf("w2%d" % i, [128, 16], F32),
                xdt=fw.sbuf("xdt%d" % i, [128, 16, 64], BF16), xdte=fw.sbuf("xdte%d" % i, [128, 16, 64], BF16)))
        yst = [fw.sbuf("yst%d" % i, [128, D], F32) for i in range(2)]
        yft = fw.sbuf("yft", [128, D], F32)
        szt = fw.sbuf("szt", [128, D], F32)
        xd = fw.sbuf("xd", [128, 16, 64], F32)
        junk = fw.sbuf("junk", [128, D], BF16)
        ss = fw.sbuf("ss", [128, 4], F32)
        yn = fw.sbuf("yn", [128, D], BF16)
        ost = [fw.sbuf("ost%d" % i, [128, 8, 128], BF16) for i in range(2)]
        identb = self.C("ident", b16=True)
        for d in range(2):
            tri = self.C("triU" if d == 0 else "triL")
            neg = self.C("negF" if d == 0 else "negB")
            last = 127 if d == 0 else 0
            k.memset(S32[:], 0.0, [S32])
            k.memset(Sbf[:], 0.0, [Sbf])
            order = [16, 17] + list(range(16)) if d == 0 else [17, 16] + list(range(15, -1, -1))
            def head(ci, i):
                t0 = i * 128
                X, B_, BTt, CTt, dt_ = Xt[ci % 2], Bt[ci % 2], BTc[ci % 2], CTc[ci % 2], dtc[ci % 2]
                wk = WK[ci % 2]
                la, cum, rhs2, Dm, Eb, cbT, M, eR, ECT, Rl, t16, te, cd, w2, xdt, xdte = (wk[n_] for n_ in (
                    "la", "cum", "rhs2", "Dm", "Eb", "cbT", "M", "eR", "ECT", "Rl", "t16", "te", "cd", "w2", "xdt", "xdte"))
                fw.dma("sp", X[:], xtok.t[t0:t0 + 128, :], xtok, X)
                fw.dma("act", B_[:], btok.t[t0:t0 + 128, :], btok, B_)
                fw.dma("sp", BTt[:], BT.t[:, :, t0:t0 + 128].rearrange("g n t -> n g t"), BT, BTt)
                fw.dma("act", CTt[:], CT.t[:, :, t0:t0 + 128].rearrange("g n t -> n g t"), CT, CTt)
                fw.dma("sp", dt_[:], dtk.t[t0:t0 + 128, d * 16:(d + 1) * 16], dtk, dt_)
                k.tt(la[:], dt_[:], aneg[:, d * 16:(d + 1) * 16], ALU.mult, [dt_, aneg], [la])
                Pc = k.bank()
                k.mm(Pc[:, 0:16], tri, la[:], True, True, [self.con, la], Pc)
                k.act(cum[:], Pc[:, 0:16], AF.Copy, [Pc], [cum])
                k.tt(rhs2[:], bc(tri.unsqueeze(1), [128, 16, 128]), bc(la[:].unsqueeze(2), [128, 16, 128]), ALU.mult,
                     [self.con, la], [rhs2])
                Rb = []
                for g in range(4):
                    P = k.bank()
                    k.mm(P[:, :], self.C("ones"), rhs2[:, 4 * g:4 * g + 4, :].rearrange("p a b -> p (a b)"), True, True,
                         [self.con, rhs2], P)
                    Rb.append(P)
                for h in range(16):
                    P = Rb[h // 4]
                    k.stt(Dm[:, h, :], P[:, (h % 4) * 128:(h % 4 + 1) * 128], cum[:, h:h + 1], neg, ALU.subtract, ALU.add,
                          [P, cum, self.con], [Dm])
                for g in range(4):
                    P = Rb[g]
                    k.act(eR[:, 4 * g:4 * g + 4, :].rearrange("p a b -> p (a b)"), P[:, :], AF.Exp, [P], [eR])
                    k.cp(Rl[:, 4 * g:4 * g + 4], P[:, :].rearrange("p (a b) -> p a b", b=128)[:, :, last], [P], [Rl])
                k.act(Eb[:].rearrange("p a b -> p (a b)"), Dm[:].rearrange("p a b -> p (a b)"), AF.Exp, [Dm], [Eb])
                Pcb = k.bank()
                for g in range(4):
                    k.mm(Pcb[:, g * 128:(g + 1) * 128], BTt[:, g, :], CTt[:, g, :], True, True, [BTt, CTt], Pcb)
                k.act(cbT[:].rearrange("p a b -> p (a b)"), Pcb[:, :], AF.Copy, [Pcb], [cbT])
                for g in range(4):
                    k.tt(M[:, 4 * g:4 * g + 4, :], Eb[:, 4 * g:4 * g + 4, :], bc(cbT[:, g:g + 1, :], [128, 4, 128]), ALU.mult,
                         [Eb, cbT], [M])
                    k.tt(ECT[:, 4 * g:4 * g + 4, :], eR[:, 4 * g:4 * g + 4, :], bc(CTt[:, g:g + 1, :], [128, 4, 128]), ALU.mult,
                         [eR, CTt], [ECT])
                k.tt(t16[:], Rl[:], cum[:], ALU.subtract, [Rl, cum], [t16])
                k.act(te[:], t16[:], AF.Exp, [t16], [te])
                k.act(cd[:], Rl[:], AF.Exp, [Rl], [cd])
                k.tt(w2[:], dt_[:], te[:], ALU.mult, [dt_, te], [w2])
                Xv = X[:].rearrange("p (h e) -> p h e", e=64)
                k.tt(xdt[:], Xv, bc(dt_[:].unsqueeze(2), [128, 16, 64]), ALU.mult, [X, dt_], [xdt])
                k.tt(xdte[:], Xv, bc(w2[:].unsqueeze(2), [128, 16, 64]), ALU.mult, [X, w2], [xdte])

            def tail(ci, i):
                t0 = i * 128
                X, B_, dt_ = Xt[ci % 2], Bt[ci % 2], dtc[ci % 2]
                wk = WK[ci % 2]
                M, ECT, cd, xdt, xdte = (wk[n_] for n_ in ("M", "ECT", "cd", "xdt", "xdte"))
                Xv = X[:].rearrange("p (h e) -> p h e", e=64)
                Y = [k.bank(), k.bank()]
                for h in range(16):
                    P = Y[h // 8]
                    cs_ = slice((h % 8) * 64, (h % 8 + 1) * 64)
                    k.mm(P[:, cs_], M[:, h, :], xdt[:, h, :], True, False, [M, xdt], P)
                    k.mm(P[:, cs_], ECT[:, h, :], Sbf[:, h * 64:(h + 1) * 64], False, True, [ECT, Sbf], P)
                CS = [k.bank(), k.bank()]
                for g in range(4):
                    P = CS[g // 2]
                    k.mm(P[:, (g % 2) * 256:(g % 2 + 1) * 256], B_[:, g * 128:(g + 1) * 128],
                         xdte[:, 4 * g:4 * g + 4, :].rearrange("p a b -> p (a b)"), True, True, [B_, xdte], P)
                Sv = S32[:].rearrange("p (h e) -> p h e", e=64)
                k.tt(Sv, Sv, bc(cd[:].unsqueeze(2), [128, 16, 64]), ALU.mult, [S32, cd], [S32])
                for j in range(2):
                    k.tt(S32[:, j * 512:(j + 1) * 512], S32[:, j * 512:(j + 1) * 512], CS[j][:, :], ALU.add, [S32, CS[j]], [S32])
                k.cp(Sbf[:], S32[:], [S32], [Sbf])
                if d == 0:
                    ys = yst[ci % 2]
                    for j in range(2):
                        k.act(ys[:, j * 512:(j + 1) * 512], Y[j][:, :], AF.Copy, [Y[j]], [ys])
                    fw.dma("act", yf.t[t0:t0 + 128, :], ys[:], ys, yf)
                else:
                    fw.dma("sp", yft[:], yf.t[t0:t0 + 128, :], yf, yft)
                    fw.dma("act", szt[:], sz.t[t0:t0 + 128, :], sz, szt)
                    ys = yst[ci % 2]
                    for j in range(2):
                        k.tt(ys[:, j * 512:(j + 1) * 512], Y[j][:, :], yft[:, j * 512:(j + 1) * 512], ALU.add, [Y[j], yft], [ys])
                    k.tt(xd[:], Xv, bc(dsk[:].unsqueeze(2), [128, 16, 64]), ALU.mult, [X, dsk], [xd])
                    k.tt(ys[:], ys[:], xd[:].rearrange("p a b -> p (a b)"), ALU.add, [ys, xd], [ys])
                    k.tt(ys[:], ys[:], szt[:], ALU.mult, [ys, szt], [ys])
                    k.act(junk[:], ys[:], AF.Square, [ys], [junk, ss], accum_out=ss[:, 0:1])
                    k.act(ss[:, 1:2], ss[:, 0:1], AF.Sqrt, [ss], [ss], scale=1.0 / D, bias=self.epsb[:, 0:1])
                    fw.op("dve", lambda e: e.reciprocal(ss[:, 2:3], ss[:, 1:2]), [ss], [ss])
                    k.act(yn[:], ys[:], AF.Copy, [ys, ss], [yn], scale=ss[:, 2:3])
                    P = k.bank()
                    Pb = P[:, :].bitcast(BF16)
                    for c in range(8):
                        k.tr(Pb[:, c * 128:(c + 1) * 128], yn[:, c * 128:(c + 1) * 128], identb, [yn, self.conb], P,
                             inc=(c == 7), first=(c == 0))
                    os_ = ost[ci % 2]
                    k.tt(os_[:], Pb.rearrange("p (c t) -> p c t", t=128), bc(self.V("ssdg", 0, 8).unsqueeze(2), [128, 8, 128]),
                         ALU.mult, [P, self.vec], [os_])
                    fw.dma("sp", oT.t[0:8, :, t0:t0 + 128].rearrange("c p t -> p c t"), os_[:], os_, oT)

            head(0, order[0])
            for ci, i in enumerate(order):
                if ci + 1 < len(order):
                    head(ci + 1, order[ci + 1])
                tail(ci, i)
            fw.barrier()
        fw.pop()

    def phase_conformer(self):
        fw, k, dr = self.fw, self.k, self.dr
        U, oT = dr["U_cf"], dr["oT"]
        fw.push()
        cv = [fw.sbuf("cv%d" % c, [128, T], F32) for c in range(8)]
        ub = [fw.sbuf("cu%d" % i, [128, T], F32) for i in range(2)]
        upL = [fw.sbuf("upL%d" % i, [128, 32, 94], BF16) for i in range(2)]
        upC = [fw.sbuf("upC%d" % i, [128, TC + 30], BF16) for i in range(2)]
        dg = [fw.sbuf("dg%d" % i, [128, 31, 128], BF16) for i in range(2)]
        for b_ in upL + upC:
            k.memset(b_[:], 0.0, [b_])
        identb = self.C("ident", b16=True)
        for c in range(8):
            u = ub[c % 2]; pl_, pc_, dgc = upL[c % 2], upC[c % 2], dg[c % 2]
            fw.dma(k.q(), u[:], U.t[c], U, u)
            k.cp(pl_[:, :, 15:79], u[:, 0:TL].rearrange("p (r w) -> p r w", w=64), [u], [pl_], eng="pool")
            k.act(pc_[:, 15:15 + TC], u[:, TL:T], AF.Copy, [u], [pc_])
            k.tt(dgc[:], bc(identb.unsqueeze(1), [128, 31, 128]),
                 bc(self.vec[:, VOFF["cfw"] + c * 31:VOFF["cfw"] + (c + 1) * 31].unsqueeze(2), [128, 31, 128]),
                 ALU.mult, [self.conb, self.vec], [dgc])
            for b in range(5):
                P = k.bank()
                n = 512 if b < 4 else TC
                for j in range(31):
                    rhs = pl_[:, 8 * b:8 * b + 8, j:j + 64] if b < 4 else pc_[:, j:j + TC]
                    k.mm(P[:, 0:n], dgc[:, j, :], rhs, j == 0, j == 30, [dgc, pl_ if b < 4 else pc_], P)
                k.act(cv[c][:, b * 512:b * 512 + n], P[:, 0:n], AF.Identity, [P, self.vec], [cv[c]], bias=self.V("cfb", c))
        sq = [fw.sbuf("sq%d" % i, [128, 512], F32) for i in range(2)]
        mean = fw.sbuf("mean", [128, 512], F32)
        var = fw.sbuf("var", [128, 512], F32)
        tmpb = [fw.sbuf("ct%d" % i, [128, 512], F32) for i in range(2)]
        ob = [fw.sbuf("cob%d" % i, [128, 512], BF16) for i in range(2)]
        ones = self.C("ones")
        qi = 0
        for (t0, n) in [(0, 512), (512, 512), (1024, 512), (1536, 512), (2048, 256)]:
            P1 = k.bank(); P2 = k.bank()
            for c in range(8):
                k.mm(P1[:, 0:n], ones, cv[c][:, t0:t0 + n], c == 0, c == 7, [self.con, cv[c]], P1)
            for c in range(8):
                s = sq[c % 2]
                k.act(s[:, 0:n], cv[c][:, t0:t0 + n], AF.Square, [cv[c]], [s])
                k.mm(P2[:, 0:n], ones, s[:, 0:n], c == 0, c == 7, [self.con, s], P2, inc=True)
            k.ts(mean[:, 0:n], P1[:, 0:n], 1.0 / D, None, ALU.mult, ALU.bypass, [P1], [mean])
            k.tt(var[:, 0:n], mean[:, 0:n], mean[:, 0:n], ALU.mult, [mean], [var])
            k.stt(var[:, 0:n], P2[:, 0:n], 1.0 / D, var[:, 0:n], ALU.mult, ALU.subtract, [P2, var], [var])
            k.act(var[:, 0:n], var[:, 0:n], AF.Sqrt, [var], [var], bias=self.epsb[:, 0:1])
            fw.op("dve", lambda e: e.reciprocal(var[:, 0:n], var[:, 0:n]), [var], [var])
            for c in range(8):
                tb = tmpb[c % 2]; o = ob[c % 2]
                k.tt(tb[:, 0:n], cv[c][:, t0:t0 + n], mean[:, 0:n], ALU.subtract, [cv[c], mean], [tb])
                k.tt(tb[:, 0:n], tb[:, 0:n], var[:, 0:n], ALU.mult, [tb, var], [tb])
                k.act(o[:, 0:n], tb[:, 0:n], AF.Silu, [tb, self.vec], [o], scale=self.V("cflg", c), bias=self.V("cflb", c))
                fw.dma(k.q(), oT.t[8 + c, :, t0:t0 + n], o[:, 0:n], o, oT)
        fw.pop()


class Prog4(Prog3):
    def phase_outproj(self, l, oT, nk, wname, xsrc, hT2, xmid_name, tiles):
        fw, k, dr = self.fw, self.k, self.dr
        xmid = self.scratch(xmid_name, [T, D])
        fw.push()
        W = fw.sbuf("Wout", [128, nk, D], BF16)
        fw.dma("pool", W[:], _wsrc(dr[wname].t, 0, D), dr[wname], W)
        m2 = fw.sbuf("m2", [128, 2, D], F32)
        mr = dr["modrow%d" % l]
        for idx in range(2):
            fw.dma(k.q(), m2[:, idx, :], mr.t[idx, 2 * D:3 * D].partition_broadcast(128), mr, m2)
        AB = self.load_AB(l, (3, 4), "gffn%d" % l)
        tmp = self.norm_tmp()
        ot = [fw.sbuf("ot%d" % i, [128, nk, 128], BF16) for i in range(2)]
        X = [fw.sbuf("Xo%d" % i, [128, D], F32) for i in range(2)]
        for n_, i in enumerate(tiles):
            o = ot[n_ % 2]; x = X[n_ % 2]
            idx = 0 if i < 16 else 1
            fw.dma("sp", o[:], oT.t[:, :, i * 128:(i + 1) * 128].rearrange("c p t -> p c t"), oT, o)
            for (p0, p1, sb, ap) in xsrc(i):
                fw.dma("act", x[p0:p1, :], ap, sb, x)
            for h in range(2):
                P = k.bank()
                for kk in range(nk):
                    k.mm(P[:, :], o[:, kk, :], W[:, kk, h * 512:(h + 1) * 512], kk == 0, kk == nk - 1, [o, W], P)
                hs = slice(h * 512, (h + 1) * 512)
                tq = tmp[n_ % 2]
                k.tt(tq[3][:].rearrange("p a b -> p (a b)")[:, hs], P[:, :], m2[:, idx, hs], ALU.mult, [P, m2], [tq[3]])
                k.tt(x[:, hs], x[:, hs], tq[3][:].rearrange("p a b -> p (a b)")[:, hs], ALU.add, [x, tq[3]], [x])
            fw.dma("sp", xmid.t[i * 128:(i + 1) * 128, :], x[:], x, xmid)
            self.normmod_tile(x, AB, idx, hT2, i * 128, tmp[n_ % 2])
        fw.pop()

    def phase_moe(self, l, hT2, xmid_name, xout, tiles, final=False, hook=None):
        fw, k, dr = self.fw, self.k, self.dr
        xmid = dr[xmid_name]
        nt = len(tiles)
        fw.push()
        Wr = fw.sbuf("Wr", [128, 8, 16], BF16)
        fw.dma("pool", Wr[:], _wsrc(dr["router_w"].t, 0, 16), dr["router_w"], Wr)
        rb = fw.sbuf("rb", [128, 16], F32)
        fw.dma("sp", rb[:], self.rowbc("rb", 16), dr["rows"], rb)
        sc = fw.sbuf("sc", [128, NT, 16], F32)
        sel = fw.sbuf("sel", [128, NT, 16], F32)
        for j, i in enumerate(tiles):
            P = k.bank()
            for kk in range(8):
                k.mm(P[:, 0:16], hT2[:, kk, i * 128:(i + 1) * 128], Wr[:, kk, :], kk == 0, kk == 7, [hT2, Wr], P)
            k.act(sc[:, j, :], P[:, 0:16], AF.Sigmoid, [P], [sc])
        S3 = lambda b_: b_[:, 0:nt, :]
        S4 = lambda b_: b_[:, 0:nt, :].rearrange("p t (g e) -> p t g e", e=4)
        k.tt(S3(sel), S3(sc), bc(rb[:].unsqueeze(1), [128, nt, 16]), ALU.add, [sc, rb], [sel])
        m1 = fw.sbuf("m1", [128, NT, 4], F32)
        m2_ = fw.sbuf("m2_", [128, NT, 4], F32)
        eq = fw.sbuf("eq", [128, NT, 16], F32)
        gs = fw.sbuf("gs", [128, NT, 4], F32)
        gm = fw.sbuf("gm", [128, NT, 1], F32)
        comb = fw.sbuf("comb", [128, NT, 16], F32)
        den = fw.sbuf("den", [128, NT, 1], F32)
        M3 = lambda b_: b_[:, 0:nt, :]
        fw.op("dve", lambda e: e.tensor_reduce(M3(m1), S4(sel), AX.X, ALU.max), [sel], [m1])
        k.tt(S4(eq), S4(sel), bc(M3(m1).unsqueeze(3), [128, nt, 4, 4]), ALU.is_equal, [sel, m1], [eq])
        k.stt(S3(eq), S3(eq), -1e9, S3(sel), ALU.mult, ALU.add, [eq, sel], [eq])
        fw.op("dve", lambda e: e.tensor_reduce(M3(m2_), S4(eq), AX.X, ALU.max), [eq], [m2_])
        k.tt(M3(gs), M3(m1), M3(m2_), ALU.add, [m1, m2_], [gs])
        fw.op("dve", lambda e: e.tensor_reduce(M3(gm), M3(gs), AX.X, ALU.max), [gs], [gm])
        k.tt(M3(gs), M3(gs), bc(M3(gm), [128, nt, 4]), ALU.is_equal, [gs, gm], [gs])
        k.tt(S4(eq), S4(sel), bc(M3(m2_).unsqueeze(3), [128, nt, 4, 4]), ALU.is_ge, [sel, m2_], [eq])
        k.tt(S4(eq), S4(eq), bc(M3(gs).unsqueeze(3), [128, nt, 4, 4]), ALU.mult, [eq, gs], [eq])
        k.tt(S3(comb), S3(eq), S3(sc), ALU.mult, [eq, sc], [comb])
        fw.op("dve", lambda e: e.tensor_reduce(M3(den), S3(comb), AX.X, ALU.add), [comb], [den])
        fw.op("dve", lambda e: e.reciprocal(M3(den), M3(den)), [den], [den])
        k.tt(S3(comb), S3(comb), bc(M3(den), [128, nt, 16]), ALU.mult, [comb, den], [comb])
        if "comb%d" % l in self.dbg:
            cdb = self.scratch("comb%d" % l, [128, NT, 16])
            fw.dma("sp", cdb.t[:], comb[:], comb, cdb)
        acc = fw.sbuf("acc", [128, NT, D], F32)
        k.memset(acc[:, 0:nt // 2, :], 0.0, [acc], eng="pool")
        k.memset(acc[:, nt // 2:nt, :], 0.0, [acc], eng="dve")
        Wg = [fw.sbuf("Wg%d" % i, [128, 8, 512], BF16) for i in range(2)]
        Wu = [fw.sbuf("Wu%d" % i, [128, 8, 512], BF16) for i in range(2)]
        Wd = [fw.sbuf("Wd%d" % i, [128, 4, D], BF16) for i in range(2)]
        sg = [fw.sbuf("sg%d" % i, [128, 512], F32) for i in range(2)]
        aT = [fw.sbuf("aT%d" % i, [128, 4, 512], BF16) for i in range(2)]
        blocks = [tiles[j:j + 4] for j in range(0, nt, 4)]
        si = 0
        if hook:
            fw.push()
            self.fw_mod_double = False
        hst = hook[0]() if hook else None
        for e_ in range(16):
            if hook and e_ >= 2 and e_ - 2 < 12:
                hook[1](hst, e_ - 2)
            wg, wu, wd = Wg[e_ % 2], Wu[e_ % 2], Wd[e_ % 2]
            fw.dma("pool", wg[:], _wsrc(dr["moe_w_gate"].t[l, e_], 0, 512), dr["moe_w_gate"], wg)
            fw.dma("pool", wu[:], _wsrc(dr["moe_w_up"].t[l, e_], 0, 512), dr["moe_w_up"], wu)
            fw.dma("pool", wd[:], _wsrc(dr["moe_w_down"].t[l, e_], 0, D), dr["moe_w_down"], wd)
            for bi, blk in enumerate(blocks):
                t0 = blk[0] * 128
                n = len(blk) * 128
                a = aT[bi % 2]
                for fc in range(4):
                    Pg = k.bank(); Pu = k.bank()
                    for kk in range(8):
                        k.mm(Pg[:, 0:n], wg[:, kk, fc * 128:(fc + 1) * 128], hT2[:, kk, t0:t0 + n], kk == 0, kk == 7, [wg, hT2], Pg)
                    for kk in range(8):
                        k.mm(Pu[:, 0:n], wu[:, kk, fc * 128:(fc + 1) * 128], hT2[:, kk, t0:t0 + n], kk == 0, kk == 7, [wu, hT2], Pu)
                    s = sg[si % 2]; si += 1
                    k.act(s[:, 0:n], Pg[:, 0:n], AF.Silu, [Pg], [s])
                    k.tt(a[:, fc, 0:n], Pu[:, 0:n], s[:, 0:n], ALU.mult, [Pu, s], [a])
                for jt, i in enumerate(blk):
                    j = tiles.index(i)
                    for h in range(2):
                        P = k.bank()
                        for fc in range(4):
                            k.mm(P[:, :], a[:, fc, jt * 128:(jt + 1) * 128], wd[:, fc, h * 512:(h + 1) * 512], fc == 0, fc == 3, [a, wd], P)
                        k.stt(acc[:, j, h * 512:(h + 1) * 512], P[:, :], comb[:, j, e_:e_ + 1], acc[:, j, h * 512:(h + 1) * 512],
                              ALU.mult, ALU.add, [P, comb, acc], [acc])
        if hook:
            hook[2](hst)
            fw.pop()
        m5 = fw.sbuf("m5", [128, 2, D], F32)
        mr = dr["modrow%d" % l]
        for idx in range(2):
            fw.dma(k.q(), m5[:, idx, :], mr.t[idx, 5 * D:6 * D].partition_broadcast(128), mr, m5)
        X = [fw.sbuf("Xm%d" % i, [128, D], F32) for i in range(2)]
        if final:
            fg = fw.sbuf("fg", [128, D], F32)
            fw.dma("sp", fg[:], self.rowbc("fng", D), dr["rows"], fg)
            junk = fw.sbuf("junkf", [128, D], BF16)
            ss = fw.sbuf("ssf", [128, 4], F32)
        for j, i in enumerate(tiles):
            x = X[j % 2]
            idx = 0 if i < 16 else 1
            fw.dma("sp", x[:], xmid.t[i * 128:(i + 1) * 128, :], xmid, x)
            k.tt(acc[:, j, :], acc[:, j, :], m5[:, idx, :], ALU.mult, [acc, m5], [acc])
            k.tt(x[:], x[:], acc[:, j, :], ALU.add, [x, acc], [x])
            if final:
                k.act(junk[:], x[:], AF.Square, [x], [junk, ss], accum_out=ss[:, 0:1])
                k.act(ss[:, 1:2], ss[:, 0:1], AF.Sqrt, [ss], [ss], scale=1.0 / D, bias=self.epsb[:, 0:1])
                fw.op("dve", lambda e: e.reciprocal(ss[:, 2:3], ss[:, 1:2]), [ss], [ss])
                k.stt(x[:], x[:], ss[:, 2:3], fg[:], ALU.mult, ALU.mult, [x, ss, fg], [x])
                for (p0, p1, sb, ap) in self.cm_rows(xout, i):
                    fw.dma("act", ap, x[p0:p1, :], x, xout)
            else:
                fw.dma("act", xout.t[i * 128:(i + 1) * 128, :], x[:], x, xout)
        fw.pop()


class Prog5(Prog4):
    def cm_rows(self, db, i):
        v = db.t[0:TL, :].rearrange("(r w) d -> w r d", w=64)
        return [(32 * j, 32 * j + 32, db, v[4 * i + j]) for j in range(4)]

    def phase_normmod1(self, hT, x1):
        fw, k = self.fw, self.k
        fw.push()
        AB = self.load_AB(1, (0, 1), "gmix1")
        tmp = self.norm_tmp()
        X = [fw.sbuf("X%d" % i, [128, D], F32) for i in range(2)]
        for i in range(NT):
            x = X[i % 2]
            if i < 16:
                for (p0, p1, sb, ap) in self.cm_rows(x1, i):
                    fw.dma(k.q(), x[p0:p1, :], ap, sb, x)
            else:
                fw.dma(k.q(), x[:], x1.t[i * 128:(i + 1) * 128, :], x1, x)
            self.normmod_tile(x, AB, 0 if i < 16 else 1, hT, i * 128, tmp[i % 2])
        fw.pop()

    def phase_hgproj(self, hT):
        fw, k, dr = self.fw, self.k, self.dr
        win = dr["hg_w_in"]
        QT = self.scratch("QT", [8, 128, TL])
        LF = self.scratch("LF", [2, 8, 128, T])
        KT = self.scratch("KT", [2, 8, 128, T])
        sgt = self.scratch("sg_tok", [TL, D])
        vtk = self.scratch("v_tok", [T, D], BF16)
        fw.push()
        lb = fw.sbuf("lb", [128, 8], F32)
        oml = fw.sbuf("oml", [128, 8], F32)
        k.tt(lb[:], self.V("hglb1", 0, 8), self.V("hglb0", 0, 8), ALU.subtract, [self.vec], [lb])
        k.act(lb[:], lb[:], AF.Sigmoid, [lb], [lb])
        k.ts(oml[:], lb[:], -1.0, 1.0, ALU.mult, ALU.add, [lb], [oml])
        W = [fw.sbuf("Wh%d" % i, [128, 8, 512], BF16) for i in range(2)]
        wi = 0
        stf = [fw.sbuf("stf%d" % i, [128, 512], F32) for i in range(2)]
        stb = [fw.sbuf("stb%d" % i, [128, 512], BF16) for i in range(2)]
        si = 0
        for (c0, ntl, isg) in ((1024, 16, True), (1536, 16, True), (2048, NT, False), (2560, NT, False)):
            w = W[wi % 2]; wi += 1
            fw.dma("pool", w[:], _wsrc(win.t, c0, 512), win, w)
            for i in range(ntl):
                P = k.bank()
                for kk in range(8):
                    k.mm(P[:, :], hT[:, kk, i * 128:(i + 1) * 128], w[:, kk, :], kk == 0, kk == 7, [hT, w], P)
                if isg:
                    s = stf[si % 2]; si += 1
                    k.act(s[:], P[:, :], AF.Silu, [P], [s])
                    fw.dma(k.q(), sgt.t[i * 128:(i + 1) * 128, c0 - 1024:c0 - 512], s[:], s, sgt)
                else:
                    s = stb[si % 2]; si += 1
                    k.act(s[:], P[:, :], AF.Copy, [P], [s])
                    fw.dma(k.q(), vtk.t[i * 128:(i + 1) * 128, c0 - 2048:c0 - 1536], s[:], s, vtk)
        blocks = [(0, 512), (512, 512), (1024, 512), (1536, 512), (2048, 256)]
        ob = [fw.sbuf("hob%d" % i, [128, T], F32) for i in range(2)]
        ob2 = [fw.sbuf("hob2%d" % i, [128, T], F32) for i in range(2)]
        sgm = [fw.sbuf("sgm%d" % i, [128, 512], F32) for i in range(2)]
        oi = 0
        for grp in range(6):
            c0 = (0, 512, 3072, 3584, 4096, 4608)[grp]
            w = W[wi % 2]; wi += 1
            fw.dma("pool", w[:], _wsrc(win.t, c0, 512), win, w)
            for hh in range(4):
                h = (grp % 2) * 4 + hh
                o = ob[oi % 2]; o2 = ob2[oi % 2]; oi += 1
                for (t0, n) in (blocks[:4] if grp < 2 else blocks):
                    P = k.bank()
                    for kk in range(8):
                        k.mm(P[:, 0:n], w[:, kk, hh * 128:(hh + 1) * 128], hT[:, kk, t0:t0 + n], kk == 0, kk == 7, [hT, w], P)
                    if grp < 2:
                        k.act(o[:, t0:t0 + n], P[:, 0:n], AF.Silu, [P], [o])
                    else:
                        sg_ = sgm[si % 2]; si += 1
                        k.act(sg_[:, 0:n], P[:, 0:n], AF.Sigmoid, [P], [sg_])
                        k.ts(o[:, t0:t0 + n], sg_[:, 0:n], oml[:, h:h + 1], lb[:, h:h + 1], ALU.mult, ALU.add, [sg_, oml, lb], [o])
                        k.ts(o2[:, t0:t0 + n], o[:, t0:t0 + n], -1.0, 1.0, ALU.mult, ALU.add, [o], [o2])
                if grp < 2:
                    fw.dma(k.q(), QT.t[h], o[:, 0:TL], o, QT)
                else:
                    dd = (grp - 2) // 2
                    fw.dma(k.q(), KT.t[dd, h], o2[:], o2, KT)
                    k.act(o[:], o[:], AF.Ln, [o], [o])
                    fw.dma(k.q(), LF.t[dd, h], o[:], o, LF)
        fw.pop()

    def phase_hgscan(self):
        fw, k, dr = self.fw, self.k, self.dr
        QT, LF, KT, vtk = dr["QT"], dr["LF"], dr["KT"], dr["v_tok"]
        otot = self.scratch("o_tot", [TL, D])
        fw.push()
        NCK = T // 64
        rst = fw.sbuf("rst", [128, T], F32)
        k.memset(rst[:], 1.0, [rst])
        k.memset(rst[:].rearrange("p (c l) -> p c l", l=64)[:, :, 0:1], 0.0, [rst])
        Vh = [fw.sbuf("Vh%d" % i, [64, NCK, 128], BF16) for i in range(1)] * 2
        qb = [fw.sbuf("hq%d" % i, [128, TL], F32) for i in range(1)] * 2
        lf = [fw.sbuf("hlf%d" % i, [128, T], F32) for i in range(1)] * 2
        kt = [fw.sbuf("hkt%d" % i, [128, T], F32) for i in range(1)] * 2
        G = fw.sbuf("hG", [128, T], F32)
        D1 = fw.sbuf("hD1", [128, T], F32)
        D2 = fw.sbuf("hD2", [128, T], F32)
        E = [fw.sbuf("hE%d" % i, [128, T], F32) for i in range(4)]
        qrel = [fw.sbuf("qrel%d" % i, [128, TL], BF16) for i in range(2)]
        krel = [fw.sbuf("krel%d" % i, [128, TL], BF16) for i in range(2)]
        qdec = [fw.sbuf("qdec%d" % i, [128, TL], BF16) for i in range(2)]
        kend = [fw.sbuf("kend%d" % i, [128, T], BF16) for i in range(2)]
        dec = [fw.sbuf("hdec%d" % i, [128, NCK], F32) for i in range(2)]
        S32 = [fw.sbuf("hS32%d" % i, [128, 128], F32) for i in range(2)]
        Sbf = [fw.sbuf("hSbf%d" % i, [128, 128], BF16) for i in range(2)]
        ktok = [fw.sbuf("ktok%d" % i, [64, NCK, 128], BF16) for i in range(2)]
        attT = [fw.sbuf("attT%d" % i, [64, 32, 64], BF16) for i in range(2)]
        Oall = [fw.sbuf("Oall%d" % i, [64, 32, 128], F32) for i in range(2)]
        identb = self.C("ident", b16=True)
        c3 = lambda ap: ap.rearrange("p (c l) -> p c l", l=64)
        for h in range(8):
            V = Vh[h % 2]; q = qb[h % 2]
            fw.dma("sp", V[:], vtk.t[:, h * 128:(h + 1) * 128].rearrange("(c p) v -> p c v", p=64), vtk, V)
            fw.dma("act", q[:], QT.t[h], QT, q)
            for dd in range(2):
                l_, k_ = lf[dd], kt[dd]
                fw.dma("sp", l_[:], LF.t[dd, h], LF, l_)
                fw.dma("act", k_[:], KT.t[dd, h], KT, k_)
                mid, tot = (31, 63) if dd == 0 else (32, 0)
                fw.op("dve", lambda e: e.tensor_tensor_scan(G[:], rst[:], l_[:], 0.0, ALU.mult, ALU.add), [rst, l_], [G])
                if dd == 1:
                    k.tt(D1[:], l_[:], G[:], ALU.subtract, [l_, G], [D1], eng="pool")
                    k.tt(c3(G[:]), c3(D1[:]), bc(c3(G[:])[:, :, 63:64], [128, NCK, 64]), ALU.add, [D1, G], [G])
                Gm = bc(c3(G[:])[:, :, mid:mid + 1], [128, NCK, 64])
                Gt = bc(c3(G[:])[:, :, tot:tot + 1], [128, NCK, 64])
                E0, E1, E2, E3 = E
                k.tt(c3(D1[:]), c3(G[:]), Gm, ALU.subtract, [G], [D1])
                k.tt(c3(D2[:]), Gt, c3(G[:]), ALU.subtract, [G], [D2])
                k.act(E0[:, 0:TL], D1[:, 0:TL], AF.Exp, [D1], [E0])
                k.act(E1[:, 0:TL], D1[:, 0:TL], AF.Exp, [D1], [E1], scale=-1.0)
                k.act(E2[:], D2[:], AF.Exp, [D2], [E2])
                k.act(E3[:, 0:TL], G[:, 0:TL], AF.Exp, [G], [E3])
                k.tt(qrel[dd][:], q[:], E0[:, 0:TL], ALU.mult, [q, E0], [qrel[dd]])
                k.tt(krel[dd][:], k_[:, 0:TL], E1[:, 0:TL], ALU.mult, [k_, E1], [krel[dd]])
                k.tt(kend[dd][:], k_[:], E2[:], ALU.mult, [k_, E2], [kend[dd]])
                k.tt(qdec[dd][:], q[:], E3[:, 0:TL], ALU.mult, [q, E3], [qdec[dd]], eng="pool")
                k.act(dec[dd][:], c3(G[:])[:, :, tot], AF.Exp, [G], [dec[dd]])
                k.memset(S32[dd][:], 0.0, [S32[dd]])
                k.memset(Sbf[dd][:], 0.0, [Sbf[dd]])
                for c0 in range(0, NCK, 8):
                    nb = min(8, NCK - c0)
                    Pt = k.bank()
                    Ptb = Pt[:, :].bitcast(BF16)
                    for j in range(nb):
                        k.tr(Ptb[0:64, j * 128:(j + 1) * 128], kend[dd][:, (c0 + j) * 64:(c0 + j + 1) * 64], identb,
                             [kend[dd], self.conb], Pt, inc=(j == nb - 1), first=(j == 0))
                    k.act(ktok[dd][:, c0:c0 + nb, :].rearrange("p a b -> p (a b)"), Ptb[0:64, 0:nb * 128], AF.Copy, [Pt], [ktok[dd]])
                mask = self.C("mF64" if dd == 0 else "mB64", w=64, rows=64)
                for c0 in range(0, 32, 8):
                    Pa = k.bank()
                    for j in range(8):
                        c = c0 + j
                        k.mm(Pa[0:64, j * 64:(j + 1) * 64], krel[dd][:, c * 64:(c + 1) * 64], qrel[dd][:, c * 64:(c + 1) * 64],
                             True, True, [krel[dd], qrel[dd]], Pa, inc=(j == 7))
                    k.tt(attT[dd][:, c0:c0 + 8, :], Pa[0:64, :].rearrange("p (a b) -> p a b", b=64),
                         bc(mask.unsqueeze(1), [64, 8, 64]), ALU.mult, [Pa, self.con], [attT[dd]])
            orders = [list(range(32, 36)) + list(range(32)), list(range(35, 31, -1)) + list(range(31, -1, -1))]
            Po = [None, None]
            Pcn = [None, None]

            def emit_pc(dd, step):
                c_ = orders[dd][step]
                Pn = k.bank()
                k.mm(Pn[:, 0:128], ktok[dd][:, c_, :], V[:, c_, :], True, True, [ktok[dd], V], Pn)
                Pcn[dd] = Pn
            for dd in range(2):
                emit_pc(dd, 0)
            for step in range(NCK):
                for dd in range(2):
                    c = orders[dd][step]
                    lat = c < 32
                    Pc = Pcn[dd]
                    if step + 1 < NCK:
                        emit_pc(dd, step + 1)
                    if lat:
                        j = c % 4
                        first = (j == 0) if dd == 0 else (j == 3)
                        last_ = (j == 3) if dd == 0 else (j == 0)
                        if first:
                            Po[dd] = k.bank()
                        P = Po[dd]
                        k.mm(P[0:64, j * 128:(j + 1) * 128], attT[dd][:, c, :], V[:, c, :], True, False, [attT[dd], V], P)
                        k.mm(P[0:64, j * 128:(j + 1) * 128], qdec[dd][:, c * 64:(c + 1) * 64], Sbf[dd][:], False, True,
                             [qdec[dd], Sbf[dd]], P, inc=True)
                        if last_:
                            cg = c - j
                            k.act(Oall[dd][:, cg:cg + 4, :].rearrange("p a b -> p (a b)"), P[0:64, :], AF.Copy, [P], [Oall[dd]])
                    k.stt(S32[dd][:], S32[dd][:], dec[dd][:, c:c + 1], Pc[:, 0:128], ALU.mult, ALU.add, [S32[dd], dec[dd], Pc], [S32[dd]])
                    k.cp(Sbf[dd][:], S32[dd][:], [S32[dd]], [Sbf[dd]])
            k.tt(Oall[0][:, 0:16, :], Oall[0][:, 0:16, :], Oall[1][:, 0:16, :], ALU.add, [Oall[0], Oall[1]], [Oall[0]])
            k.tt(Oall[0][:, 16:32, :], Oall[0][:, 16:32, :], Oall[1][:, 16:32, :], ALU.add, [Oall[0], Oall[1]], [Oall[0]], eng="pool")
            for half in range(2):
                fw.dma(k.q(), otot.t[half * 1024:(half + 1) * 1024, h * 128:(h + 1) * 128].rearrange("(c p) v -> p c v", p=64),
                       Oall[0][:, half * 16:(half + 1) * 16, :], Oall[0], otot)
        fw.pop()

    def phase_hgout(self):
        fw, k, dr = self.fw, self.k, self.dr
        otot, sgt = dr["o_tot"], dr["sg_tok"]
        oT2 = self.scratch("oT2", [8, 128, T], BF16)
        fw.push()
        gb = fw.sbuf("hgng", [128, 128], F32)
        fw.dma("sp", gb[:], self.rowbc("hgng", 128), dr["rows"], gb)
        O = [fw.sbuf("hO%d" % i, [128, 8, 128], F32) for i in range(2)]
        SG = [fw.sbuf("hSG%d" % i, [128, 8, 128], F32) for i in range(2)]
        sq = fw.sbuf("hsq", [128, 8, 128], F32)
        ss = fw.sbuf("hss", [128, 8], F32)
        on = fw.sbuf("hon", [128, 8, 128], BF16)
        stg = [fw.sbuf("hstg%d" % i, [128, 8, 128], BF16) for i in range(2)]
        identb = self.C("ident", b16=True)
        f2 = lambda b_: b_[:].rearrange("p a b -> p (a b)")
        for i in range(16):
            o, sg = O[i % 2], SG[i % 2]
            fw.dma("sp", f2(o), otot.t[i * 128:(i + 1) * 128, :], otot, o)
            fw.dma("act", f2(sg), sgt.t[i * 128:(i + 1) * 128, :], sgt, sg)
            k.tt(sq[:], o[:], o[:], ALU.mult, [o], [sq])
            fw.op("dve", lambda e: e.tensor_reduce(ss[:], sq[:], AX.X, ALU.add), [sq], [ss])
            k.act(ss[:], ss[:], AF.Sqrt, [ss], [ss], scale=1.0 / 128, bias=self.epsb[:, 0:1])
            fw.op("dve", lambda e: e.reciprocal(ss[:], ss[:]), [ss], [ss])
            k.tt(o[:], o[:], bc(ss[:].unsqueeze(2), [128, 8, 128]), ALU.mult, [o, ss], [o])
            k.tt(o[:], o[:], bc(gb[:].unsqueeze(1), [128, 8, 128]), ALU.mult, [o, gb], [o])
            k.tt(on[:], o[:], sg[:], ALU.mult, [o, sg], [on])
            P = k.bank()
            Pb = P[:, :].bitcast(BF16)
            for c in range(8):
                k.tr(Pb[:, c * 128:(c + 1) * 128], on[:, c, :], identb, [on, self.conb], P, inc=(c == 7), first=(c == 0))
            s = stg[i % 2]
            k.act(f2(s), Pb, AF.Copy, [P], [s])
            fw.dma("sp", oT2.t[:, :, i * 128:(i + 1) * 128].rearrange("c p t -> p c t"), s[:], s, oT2)
        fw.pop()

    def build_full(self):
        fw, dr = self.fw, self.dr
        self.declare_io()
        out = self.scratch("out", [TL, D], out=True)
        self.setup_consts()
        self.phase_mod(0)
        fw.push()
        hT = fw.sbuf("hT", [128, 8, T], BF16)
        self.phase_normmod0(hT)
        self.phase_evenproj(hT)
        fw.pop()
        self.phase_ssd()
        self.phase_conformer()
        fw.push()
        hT2 = fw.sbuf("hT2", [128, 8, T], BF16)
        xsrc0 = lambda i: [(0, 128, dr["x"], dr["x"].t[i * 128:(i + 1) * 128, :])] if i < 16 else \
            [(0, 128, dr["ctx"], dr["ctx"].t[(i - 16) * 128:(i - 15) * 128, :])]
        self.phase_outproj(0, dr["oT"], 16, "ab_w_out", xsrc0, hT2, "xmid0", list(range(NT)))
        x1 = self.scratch("x1", [T, D])
        self.phase_moe(0, hT2, "xmid0", x1, list(range(NT)),
                       hook=(lambda: self.mod_setup(1), self.mod_group, self.mod_finish))
        fw.pop()
        fw.push()
        hT = fw.sbuf("hTb", [128, 8, T], BF16)
        self.phase_normmod1(hT, x1)
        self.phase_hgproj(hT)
        fw.pop()
        self.phase_hgscan()
        self.phase_hgout()
        fw.push()
        hT2 = fw.sbuf("hT2b", [128, 8, T], BF16)
        self.phase_outproj(1, dr["oT2"], 8, "hg_w_out", lambda i: self.cm_rows(x1, i), hT2, "xmid1", list(range(16)))
        self.phase_moe(1, hT2, "xmid1", out, list(range(16)), final=True)
        fw.pop()
        fw.barrier()
        st = simulate(fw)
        assert not st, "sync deadlock: %r" % (st,)
        return self.nc


_CACHE = {}


def kernel(**inputs):
    n = 8
    if "nc" not in _CACHE:
        _CACHE["nc"] = Prog5().build_full()
    nc = _CACHE["nc"]
    maps = make_inmaps(inputs, list(range(n)))
    res = run_bass_kernel_spmd(nc, maps, core_ids=list(range(n)))
    return np.stack([np.asarray(r["out"], np.float32) for r in res.results], axis=0)
```

```python
import numpy as np
from contextlib import ExitStack
import concourse.bass as bass
import concourse.mybir as mybir
from concourse.bass_utils import run_bass_kernel_spmd

F32 = mybir.dt.float32
BF16 = mybir.dt.bfloat16
AF = mybir.ActivationFunctionType
ALU = mybir.AluOpType
AX = mybir.AxisListType

D = 1024
TL = 2048
TC = 256
T = TL + TC
NT = T // 128
EPS = 1e-6
NEG = -30000.0


class Buf:
    __slots__ = ("name", "t", "writer", "readers", "ld", "st", "onchip", "excl")

    def __init__(self, name, t, onchip):
        self.name = name
        self.t = t
        self.writer = None
        self.readers = []
        self.ld = None
        self.st = None
        self.onchip = onchip
        self.excl = False

    def __getitem__(self, k):
        return self.t[k]


class FW:
    ENG = ("pe", "act", "dve", "pool", "sp")

    def __init__(self, nc, es, n_dma_sems=90):
        self.nc = nc
        self.es = es
        self.scopes = [es]
        self.eng = {"pe": nc.tensor, "act": nc.scalar, "dve": nc.vector, "pool": nc.gpsimd, "sp": nc.sync}
        self.sems = {}
        self.cnt = {}
        for e in self.ENG:
            self.sems[e] = es.enter_context(nc.semaphore("s_" + e))
            self.cnt[e] = 0
        self.free_dma = []
        for i in range(n_dma_sems):
            k = "d%d" % i
            self.sems[k] = es.enter_context(nc.semaphore("s_" + k))
            self.cnt[k] = 0
            self.free_dma.append(k)
        self.phase_dma = []
        self.seen = {e: {} for e in self.ENG}
        self.bufs = []
        self.n_inst = 0
        self.uid = 0
        self.log = {e: [] for e in self.ENG}

    def push(self):
        s = ExitStack()
        self.scopes.append(s)
        s._bufs0 = len(self.bufs)

    def pop(self):
        self.barrier()
        s = self.scopes.pop()
        del self.bufs[s._bufs0:]
        s.close()

    def _nm(self, name):
        self.uid += 1
        return "%s_%d" % (name, self.uid)

    def sbuf(self, name, shape, dtype):
        t = self.scopes[-1].enter_context(self.nc.sbuf_tensor(self._nm(name), list(shape), dtype))
        b = Buf(name, t, True)
        self.bufs.append(b)
        return b

    def psum(self, name, shape, dtype):
        t = self.scopes[-1].enter_context(self.nc.psum_tensor(self._nm(name), list(shape), dtype))
        b = Buf(name, t, True)
        b.excl = True
        self.bufs.append(b)
        return b

    def dram(self, name, t):
        b = Buf(name, t, False)
        self.bufs.append(b)
        return b

    def _need(self, e, tok, waits):
        if tok is None:
            return
        k, v = tok
        if self.seen[e].get(k, 0) >= v:
            return
        if waits.get(k, 0) < v:
            waits[k] = v

    def _deps(self, e, reads, writes, pe_accum=False, attach=False):
        waits = {}
        for b in reads:
            self._need(e, b.writer, waits)
            if b.excl:
                for r in b.readers:
                    if r[0] != e:
                        self._need(e, r, waits)
        for b in writes:
            if not (e == "pe" and b.writer is not None and b.writer[0] == "pe"):
                self._need(e, b.writer, waits)
            for r in b.readers:
                self._need(e, r, waits)
        items = list(waits.items())
        self.pending = None
        if attach and items:
            self.pending = items.pop()
        for k, v in items:
            self.eng[e].wait_ge(self.sems[k], v)
            self.seen[e][k] = v
            self.log[e].append(("w", k, v))
        if self.pending is not None:
            k, v = self.pending
            self.seen[e][k] = v
            self.log[e].append(("w", k, v))

    def op(self, e, fn, reads=(), writes=(), inc=True, pe_accum=False):
        self._deps(e, reads, writes, pe_accum, attach=True)
        ins = fn(self.eng[e])
        if self.pending is not None:
            ins._wait_ge(self.sems[self.pending[0]], self.pending[1])
        self.n_inst += 1
        tok = (e, self.cnt[e] + 1)
        if inc:
            self.cnt[e] += 1
            ins.then_inc(self.sems[e], 1)
            self.log[e].append(("i", e, 1))
        for b in reads:
            b.readers.append(tok)
            if len(b.readers) > 24:
                b.readers = _compact(b.readers)
        for b in writes:
            b.writer = tok
            b.readers = []
        return ins

    def _dma_sem(self):
        k = self.free_dma.pop()
        self.phase_dma.append(k)
        return k

    def dma(self, q, out_ap, in_ap, src, dst, **kw):
        self._deps(q, [src], [dst], attach=True)
        pend = self.pending
        if dst.onchip:
            if dst.ld is None:
                dst.ld = self._dma_sem()
            k = dst.ld
        else:
            if src.st is None:
                src.st = self._dma_sem()
            k = src.st
        self.cnt[k] += 16
        ins = self.eng[q].dma_start(out=out_ap, in_=in_ap, **kw)
        if pend is not None:
            ins._wait_ge(self.sems[pend[0]], pend[1])
        ins.then_inc(self.sems[k], 16)
        self.log[q].append(("i", k, 16))
        self.n_inst += 1
        tok = (k, self.cnt[k])
        src.readers.append(tok)
        if len(src.readers) > 24:
            src.readers = _compact(src.readers)
        if dst.onchip:
            dst.writer = tok
            dst.readers = []
        return ins

    def barrier(self):
        sp = self.eng["sp"]
        keys = [k for k in self.ENG if k != "sp"] + self.phase_dma
        for k in keys:
            v = self.cnt[k]
            if v > 0 and self.seen["sp"].get(k, 0) < v:
                sp.wait_ge(self.sems[k], v)
                self.seen["sp"][k] = v
                self.log["sp"].append(("w", k, v))
        self.cnt["sp"] += 1
        sp.nop().then_inc(self.sems["sp"], 1)
        self.log["sp"].append(("i", "sp", 1))
        v = self.cnt["sp"]
        for e in self.ENG:
            if e == "sp":
                continue
            self.log[e].append(("w", "sp", v))
            self.eng[e].wait_ge(self.sems["sp"], v)
            self.seen[e]["sp"] = v
            for k in keys:
                self.seen[e][k] = self.cnt[k]
        for b in self.bufs:
            b.writer = None
            b.readers = []
            b.ld = None
            b.st = None
        self.free_dma.extend(self.phase_dma)
        self.phase_dma = []


def simulate(fw):
    pos = {e: 0 for e in fw.ENG}
    val = {}
    prog = True
    while prog:
        prog = False
        for e in fw.ENG:
            L = fw.log[e]
            while pos[e] < len(L):
                kind, k, v = L[pos[e]]
                if kind == "w":
                    if val.get(k, 0) < v:
                        break
                else:
                    val[k] = val.get(k, 0) + v
                pos[e] += 1
                prog = True
    stuck = {e: (pos[e], len(fw.log[e]), fw.log[e][pos[e]], val.get(fw.log[e][pos[e]][1], 0))
             for e in fw.ENG if pos[e] < len(fw.log[e])}
    return stuck


def _compact(toks):
    m = {}
    for k, v in toks:
        if m.get(k, 0) < v:
            m[k] = v
    return list(m.items())


class KB:
    def __init__(self, nc, fw):
        self.nc = nc
        self.fw = fw
        self.banks = [fw.psum("bank%d" % i, [128, 512], F32) for i in range(8)]
        self.bi = 0
        self.dq = 0

    def bank(self):
        b = self.banks[self.bi]
        self.bi = (self.bi + 1) % 8
        return b

    def q(self):
        self.dq ^= 1
        return "sp" if self.dq else "act"

    def mm(self, out, lhsT, rhs, start, stop, reads, wr, inc=None):
        if inc is None:
            inc = stop
        return self.fw.op("pe", lambda e: e.matmul(out, lhsT, rhs, start=start, stop=stop),
                          reads=reads, writes=[wr], inc=inc, pe_accum=not start)

    def tr(self, out, in_, ident, reads, wr, inc=True, first=False):
        return self.fw.op("pe", lambda e: e.transpose(out, in_, ident), reads=reads, writes=[wr],
                          inc=inc, pe_accum=not first)

    def act(self, out, in_, func, reads, writes, **kw):
        return self.fw.op("act", lambda e: e.activation(out, in_, func, **kw), reads=reads, writes=writes)

    def tt(self, out, a, b, op, reads, writes, eng="dve"):
        return self.fw.op(eng, lambda e: e.tensor_tensor(out, a, b, op), reads=reads, writes=writes)

    def ts(self, out, a, s1, s2, op0, op1, reads, writes, eng="dve", **kw):
        return self.fw.op(eng, lambda e: e.tensor_scalar(out, a, s1, s2, op0, op1, **kw), reads=reads, writes=writes)

    def stt(self, out, a, s, b, op0, op1, reads, writes):
        return self.fw.op("dve", lambda e: e.scalar_tensor_tensor(out, a, s, b, op0, op1), reads=reads, writes=writes)

    def cp(self, out, a, reads, writes, eng="dve"):
        return self.fw.op(eng, lambda e: e.tensor_copy(out, a), reads=reads, writes=writes)

    def memset(self, ap, val, writes, eng="pool"):
        return self.fw.op(eng, lambda e: e.memset(ap, val), reads=[], writes=writes)


def bc(ap, shape):
    return ap.broadcast_to(list(shape))


VEC_SPEC = [("modb0", 48), ("modb1", 48), ("gmix0", 8), ("gmix1", 8), ("gffn0", 8), ("gffn1", 8),
            ("convw", 80), ("convb", 16), ("ssdg", 8), ("cfw", 248), ("cfb", 8), ("cflg", 8), ("cflb", 8),
            ("hglb0", 8), ("hglb1", 8)]
VOFF = {}
_o = 0
for _n, _c in VEC_SPEC:
    VOFF[_n] = _o
    _o += _c
NV = _o
ROW_SPEC = [("modb0", 6144), ("modb1", 6144), ("dtb", 32), ("alog", 32), ("ssdd", 16), ("rb", 16),
            ("fng", 1024), ("hgng", 128)]
ROFF = {}
_o = 0
for _n, _c in ROW_SPEC:
    ROFF[_n] = _o
    _o += _c
NR = _o
CONST_SPEC = [("ident", 128), ("triU", 128), ("triL", 128), ("negF", 128), ("negB", 128), ("ones", 128),
              ("mF64", 64), ("mB64", 64)]
COFF = {}
_o = 0
for _n, _c in CONST_SPEC:
    COFF[_n] = _o
    _o += _c
NCON = _o


def _col(v):
    v = np.asarray(v, np.float32).reshape(-1, 128)
    return np.ascontiguousarray(v.T)


def pack_shared(I):
    vec = np.zeros((128, NV), np.float32)

    def put(n, a):
        vec[:, VOFF[n]:VOFF[n] + a.shape[1]] = a
    put("modb0", _col(I["mod_b"][0])); put("modb1", _col(I["mod_b"][1]))
    put("gmix0", _col(I["norm_mix_g"][0])); put("gmix1", _col(I["norm_mix_g"][1]))
    put("gffn0", _col(I["norm_ffn_g"][0])); put("gffn1", _col(I["norm_ffn_g"][1]))
    cw = np.asarray(I["ssd_conv_w"][0], np.float32)
    put("convw", np.ascontiguousarray(cw.reshape(5, 16, 128).transpose(2, 1, 0)).reshape(128, 80))
    put("convb", _col(I["ssd_conv_b"][0]))
    put("ssdg", _col(I["ssd_norm_g"][0]))
    fw_ = np.asarray(I["cf_dw_w"][0], np.float32)
    put("cfw", np.ascontiguousarray(fw_.reshape(31, 8, 128).transpose(2, 1, 0)).reshape(128, 248))
    put("cfb", _col(I["cf_dw_b"][0])); put("cflg", _col(I["cf_ln_g"][0])); put("cflb", _col(I["cf_ln_b"][0]))
    put("hglb0", _col(I["hg_lb"][0])); put("hglb1", _col(I["hg_lb"][1]))
    row = np.zeros((1, NR), np.float32)

    def putr(n, a):
        a = np.asarray(a, np.float32).reshape(-1)
        row[0, ROFF[n]:ROFF[n] + a.size] = a
    putr("modb0", I["mod_b"][0]); putr("modb1", I["mod_b"][1]); putr("dtb", I["ssd_dt_bias"][0])
    putr("alog", I["ssd_a_log"][0]); putr("ssdd", I["ssd_d"][0]); putr("rb", I["router_b"])
    putr("fng", I["final_norm_g"]); putr("hgng", I["hg_norm_g"][0])
    con = np.zeros((128, NCON), np.float32)
    i = np.arange(128)
    k, l = i[:, None], i[None, :]

    def putc(n, a):
        con[:a.shape[0], COFF[n]:COFF[n] + a.shape[1]] = a
    putc("ident", (k == l).astype(np.float32))
    putc("triU", (k <= l).astype(np.float32))
    putc("triL", (k >= l).astype(np.float32))
    putc("negF", np.where(k <= l, 0.0, NEG).astype(np.float32))
    putc("negB", np.where(k >= l, 0.0, NEG).astype(np.float32))
    putc("ones", np.ones((128, 128), np.float32))
    putc("mF64", (k[:64] <= l[:, :64]).astype(np.float32))
    putc("mB64", (k[:64] >= l[:, :64]).astype(np.float32))
    return vec, row, con


class Prog:
    def __init__(self, dbg=()):
        self.dbg = set(dbg)
        nc = self.nc = bass.Bass("TRN2", target_bir_lowering=False)
        self.es = ExitStack()
        self.fw = FW(nc, self.es)
        self.k = KB(nc, self.fw)
        self.dr = {}

    def inp(self, name, shape, dtype=F32):
        t = self.nc.dram_tensor(name, list(shape), dtype, kind="ExternalInput")
        self.dr[name] = self.fw.dram(name, t)
        return self.dr[name]

    def scratch(self, name, shape, dtype=F32, out=False):
        kind = "ExternalOutput" if (out or name in self.dbg) else "Internal"
        t = self.nc.dram_tensor(name, list(shape), dtype, kind=kind)
        self.dr[name] = self.fw.dram(name, t)
        return self.dr[name]

    def declare_io(self):
        self.inp("x", [TL, D]); self.inp("ctx", [TC, D]); self.inp("cvec", [128, 8, 2])
        self.inp("vecs", [128, NV]); self.inp("rows", [1, NR]); self.inp("consts", [128, NCON])
        self.inp("mod_w", [2, D, 6 * D]); self.inp("router_w", [D, 16])
        self.inp("moe_w_gate", [2, 16, D, 512]); self.inp("moe_w_up", [2, 16, D, 512])
        self.inp("moe_w_down", [2, 16, 512, D])
        self.inp("ab_w_in", [D, 5152]); self.inp("ab_w_out", [2048, D])
        self.inp("hg_w_in", [D, 5120]); self.inp("hg_w_out", [D, D])

    def setup_consts(self):
        fw, k = self.fw, self.k
        self.con = fw.sbuf("con", [128, NCON], F32)
        fw.dma("sp", self.con[:], self.dr["consts"][:], self.dr["consts"], self.con)
        self.conb = fw.sbuf("conb", [128, NCON], BF16)
        k.cp(self.conb[:], self.con[:], [self.con], [self.conb])
        self.vec = fw.sbuf("vec", [128, NV], F32)
        fw.dma("act", self.vec[:], self.dr["vecs"][:], self.dr["vecs"], self.vec)

    def C(self, n, w=128, rows=128, b16=False):
        t = self.conb if b16 else self.con
        return t[0:rows, COFF[n]:COFF[n] + w]

    def V(self, n, j=0, w=1):
        return self.vec[:, VOFF[n] + j:VOFF[n] + j + w]

    def rowbc(self, n, w, off=0):
        return self.dr["rows"].t[0, ROFF[n] + off:ROFF[n] + off + w].partition_broadcast(128)

    fw_mod_double = True

    def phase_mod(self, l):
        self.fw.push()
        self.fw_mod_double = True
        st = self.mod_setup(l)
        for g in range(12):
            self.mod_group(st, g)
        self.mod_finish(st)
        self.fw.pop()

    def mod_setup(self, l):
        fw, k, dr = self.fw, self.k, self.dr
        modrow = self.scratch("modrow%d" % l, [2, 6 * D])
        modcol = self.scratch("modcol%d" % l, [128, 48, 2])
        cv = fw.sbuf("cv", [128, 8, 2], F32)
        fw.dma("sp", cv[:], dr["cvec"][:], dr["cvec"], cv)
        cs = fw.sbuf("cs", [128, 8, 2], BF16)
        k.act(cs[:], cv[:], AF.Silu, [cv], [cs])
        csb = fw.sbuf("csb", [128, 8, 128], BF16)
        k.cp(csb[:, :, 0:64], bc(cs[:, :, 0:1], [128, 8, 64]), [cs], [csb])
        k.cp(csb[:, :, 64:128], bc(cs[:, :, 1:2], [128, 8, 64]), [cs], [csb])
        nb_ = 2 if self.fw_mod_double else 1
        mb = [fw.sbuf("mb%d" % i, [128, 512], F32) for i in range(nb_)] * (3 - nb_)
        mcol = fw.sbuf("mcol", [128, 48, 2], F32)
        W = [fw.sbuf("modW%d" % i, [128, 8, 512], BF16) for i in range(nb_)] * (3 - nb_)
        rowb = [fw.sbuf("rowb%d" % i, [128, 512], F32) for i in range(nb_)] * (3 - nb_)
        wsrc = dr["mod_w"].t[l].rearrange("(k p) n -> p k n", p=128)
        return dict(l=l, modrow=modrow, modcol=modcol, cs=cs, csb=csb, mb=mb, mcol=mcol, W=W, rowb=rowb, wsrc=wsrc)

    def mod_group(self, st, g):
        fw, k, dr = self.fw, self.k, self.dr
        l, modrow, cs, csb, mcol = st["l"], st["modrow"], st["cs"], st["csb"], st["mcol"]
        w = st["W"][g % 2]
        fw.dma("pool", w[:], st["wsrc"][:, :, g * 512:(g + 1) * 512], dr["mod_w"], w)
        mb = st["mb"][g % 2]
        fw.dma("act", mb[:], self.rowbc("modb%d" % l, 512, off=g * 512), dr["rows"], mb)
        P = k.bank()
        for kk in range(8):
            k.mm(P[:, :], csb[:, kk, :], w[:, kk, :], kk == 0, kk == 7, [csb, w], P)
        rb = st["rowb"][g % 2]
        k.tt(rb[:], P[:, :], mb[:], ALU.add, [P, mb], [rb])
        fw.dma("sp", modrow.t[0:1, g * 512:(g + 1) * 512], rb[0:1, :], rb, modrow)
        fw.dma("act", modrow.t[1:2, g * 512:(g + 1) * 512], rb[64:65, :], rb, modrow)
        P2 = k.bank()
        for fc in range(4):
            for kk in range(8):
                k.mm(P2[:, fc * 2:fc * 2 + 2], w[:, kk, fc * 128:(fc + 1) * 128], cs[:, kk, :],
                     kk == 0, kk == 7, [cs, w], P2)
        k.tt(mcol[:, g * 4:(g + 1) * 4, :], P2[:, 0:8].rearrange("p (a b) -> p a b", b=2),
             bc(self.V("modb%d" % l, g * 4, 4).unsqueeze(2), [128, 4, 2]), ALU.add, [P2, self.vec], [mcol])

    def mod_finish(self, st):
        self.fw.dma("sp", st["modcol"].t[:], st["mcol"][:], st["mcol"], st["modcol"])


def make_inmaps(I, cores):
    vec, row, con = pack_shared(I)
    shared = {"vecs": vec, "rows": row, "consts": con}
    for n in ("mod_w", "router_w", "moe_w_gate", "moe_w_up", "moe_w_down", "hg_w_out"):
        shared[n] = np.ascontiguousarray(np.asarray(I[n], np.float32))
    shared["ab_w_in"] = np.ascontiguousarray(np.asarray(I["ab_w_in"][0], np.float32))
    shared["ab_w_out"] = np.ascontiguousarray(np.asarray(I["ab_w_out"][0], np.float32))
    shared["hg_w_in"] = np.ascontiguousarray(np.asarray(I["hg_w_in"][0], np.float32))
    shared["hg_w_out"] = np.ascontiguousarray(np.asarray(I["hg_w_out"][0], np.float32))
    maps = []
    for b in cores:
        m = dict(shared)
        m["x"] = np.ascontiguousarray(np.asarray(I["x"][b], np.float32))
        m["ctx"] = np.ascontiguousarray(np.asarray(I["ctx"][b], np.float32))
        cv = np.stack([_col(I["c"][b]), _col(I["c_ctx"])], axis=-1)
        m["cvec"] = np.ascontiguousarray(cv.astype(np.float32))
        maps.append(m)
    return maps


def _wsrc(dt, col0, ncols):
    return dt.rearrange("(k p) n -> p k n", p=128)[:, :, col0:col0 + ncols]


class Prog2(Prog):
    def load_AB(self, l, which, gname):
        fw, k = self.fw, self.k
        mc = fw.sbuf("mc", [128, 48, 2], F32)
        fw.dma("sp", mc[:], self.dr["modcol%d" % l].t[:], self.dr["modcol%d" % l], mc)
        AB = fw.sbuf("AB", [128, 2, 2, 8], F32)
        sh, sc = which
        for idx in range(2):
            k.ts(AB[:, idx, 0, :], mc[:, sc * 8:sc * 8 + 8, idx], 1.0, None, ALU.add, ALU.bypass, [mc], [AB])
            k.tt(AB[:, idx, 0, :], AB[:, idx, 0, :], self.V(gname, 0, 8), ALU.mult, [AB, self.vec], [AB])
            k.cp(AB[:, idx, 1, :], mc[:, sh * 8:sh * 8 + 8, idx], [mc], [AB])
        return AB

    def normmod_tile(self, X, AB, idx, hT, tok0, tmp):
        fw, k = self.fw, self.k
        junk, ss, xn, t3 = tmp
        k.act(junk[:], X[:], AF.Square, [X], [junk, ss], accum_out=ss[:, 0:1])
        k.act(ss[:, 1:2], ss[:, 0:1], AF.Sqrt, [ss], [ss], scale=1.0 / D, bias=self.epsb[:, 0:1])
        self.fw.op("dve", lambda e: e.reciprocal(ss[:, 2:3], ss[:, 1:2]), [ss], [ss])
        k.act(xn[:], X[:], AF.Copy, [X, ss], [xn], scale=ss[:, 2:3])
        P = k.bank()
        Pb = P[:, :].bitcast(BF16)
        for c in range(8):
            k.tr(Pb[:, c * 128:(c + 1) * 128], xn[:, c * 128:(c + 1) * 128], self.C("ident", b16=True),
                 [xn, self.conb], P, inc=(c == 7), first=(c == 0))
        Pv = Pb.rearrange("p (c t) -> p c t", t=128)
        k.tt(t3[:], Pv, bc(AB[:, idx, 0, :].unsqueeze(2), [128, 8, 128]), ALU.mult, [P, AB], [t3])
        k.tt(hT[:, :, tok0:tok0 + 128], t3[:], bc(AB[:, idx, 1, :].unsqueeze(2), [128, 8, 128]), ALU.add,
             [t3, AB], [hT])

    def norm_tmp(self):
        fw = self.fw
        return [(fw.sbuf("junk%d" % i, [128, D], BF16), fw.sbuf("ss%d" % i, [128, 4], F32), fw.sbuf("xn%d" % i, [128, D], BF16),
                 fw.sbuf("t3%d" % i, [128, 8, 128], F32)) for i in range(2)]

    def setup_consts(self):
        Prog.setup_consts(self)
        self.epsb = self.fw.sbuf("epsb", [128, 1], F32)
        self.k.memset(self.epsb[:], EPS, [self.epsb])

    def phase_normmod0(self, hT):
        fw, k, dr = self.fw, self.k, self.dr
        fw.push()
        AB = self.load_AB(0, (0, 1), "gmix0")
        tmp = self.norm_tmp()
        X = [fw.sbuf("X%d" % i, [128, D], F32) for i in range(2)]
        for i in range(NT):
            x = X[i % 2]
            if i < 16:
                fw.dma(k.q(), x[:], dr["x"].t[i * 128:(i + 1) * 128, :], dr["x"], x)
            else:
                fw.dma(k.q(), x[:], dr["ctx"].t[(i - 16) * 128:(i - 15) * 128, :], dr["ctx"], x)
            self.normmod_tile(x, AB, 0 if i < 16 else 1, hT, i * 128, tmp[i % 2])
        fw.pop()

    def phase_evenproj(self, hT):
        fw, k, dr = self.fw, self.k, self.dr
        win = dr["ab_w_in"]
        sz = self.scratch("sz_tok", [T, D])
        dtk = self.scratch("dt_tok", [T, 32])
        xtok = self.scratch("x_tok", [T, D], BF16)
        btok = self.scratch("b_tok", [T, 512], BF16)
        BT = self.scratch("BT", [4, 128, T], BF16)
        CT = self.scratch("CT", [4, 128, T], BF16)
        U = self.scratch("U_cf", [8, 128, T])
        fw.push()
        Wz = fw.sbuf("Wz", [128, 8, D], BF16)
        fw.dma("pool", Wz[:], _wsrc(win.t, 0, D), win, Wz)
        Wdt = fw.sbuf("Wdt", [128, 8, 32], BF16)
        fw.dma("pool", Wdt[:], _wsrc(win.t, 3072, 32), win, Wdt)
        dtb = fw.sbuf("dtb", [128, 32], F32)
        fw.dma("sp", dtb[:], self.rowbc("dtb", 32), dr["rows"], dtb)
        zs = [fw.sbuf("zs%d" % i, [128, D], F32) for i in range(2)]
        dts = [fw.sbuf("dts%d" % i, [128, 32], F32) for i in range(2)]
        for i in range(NT):
            z = zs[i % 2]
            for h in range(2):
                P = k.bank()
                for kk in range(8):
                    k.mm(P[:, :], hT[:, kk, i * 128:(i + 1) * 128], Wz[:, kk, h * 512:(h + 1) * 512],
                         kk == 0, kk == 7, [hT, Wz], P)
                k.act(z[:, h * 512:(h + 1) * 512], P[:, :], AF.Silu, [P], [z])
            fw.dma(k.q(), sz.t[i * 128:(i + 1) * 128, :], z[:], z, sz)
            P = k.bank()
            for kk in range(8):
                k.mm(P[:, 0:32], hT[:, kk, i * 128:(i + 1) * 128], Wdt[:, kk, :], kk == 0, kk == 7, [hT, Wdt], P)
            d = dts[i % 2]
            k.tt(d[:], P[:, 0:32], dtb[:], ALU.add, [P, dtb], [d])
            k.act(d[:], d[:], AF.Exp, [d], [d])
            k.act(d[:], d[:], AF.Ln, [d], [d], bias=1.0)
            fw.dma(k.q(), dtk.t[i * 128:(i + 1) * 128, :], d[:], d, dtk)
        Wg = [fw.sbuf("Wg%d" % i, [128, 8, 512], BF16) for i in range(2)]
        xinL = [fw.sbuf("xinL%d" % i, [128, TL + 4], F32) for i in range(2)]
        xinC = [fw.sbuf("xinC%d" % i, [128, TC + 4], F32) for i in range(2)]
        for b_ in xinL:
            k.memset(b_[:, 0:2], 0.0, [b_]); k.memset(b_[:, TL + 2:TL + 4], 0.0, [b_])
        for b_ in xinC:
            k.memset(b_[:, 0:2], 0.0, [b_]); k.memset(b_[:, TC + 2:TC + 4], 0.0, [b_])
        acc = [fw.sbuf("acc%d" % i, [128, T], F32) for i in range(2)]
        xc = [fw.sbuf("xc%d" % i, [128, T], BF16) for i in range(2)]
        stg = [fw.sbuf("stg%d" % i, [128, 4, 128], BF16) for i in range(2)]
        blocks = [(0, 512), (512, 512), (1024, 512), (1536, 512), (2048, 256)]
        si = 0
        for cc in range(16):
            if cc % 4 == 0:
                w = Wg[(cc // 4) % 2]
                fw.dma("pool", w[:], _wsrc(win.t, 1024 + cc * 128, 512), win, w)
            xl, xcx, a, o = xinL[cc % 2], xinC[cc % 2], acc[cc % 2], xc[cc % 2]
            for (t0, n) in blocks:
                P = k.bank()
                for kk in range(8):
                    k.mm(P[:, 0:n], w[:, kk, (cc % 4) * 128:(cc % 4 + 1) * 128], hT[:, kk, t0:t0 + n],
                         kk == 0, kk == 7, [hT, w], P)
                if t0 < TL:
                    k.act(xl[:, 2 + t0:2 + t0 + n], P[:, 0:n], AF.Copy, [P], [xl])
                else:
                    k.act(xcx[:, 2:2 + n], P[:, 0:n], AF.Copy, [P], [xcx])
            for (xi, lo, n) in ((xl, 0, TL), (xcx, TL, TC)):
                k.ts(a[:, lo:lo + n], xi[:, 0:n], self.V("convw", cc * 5), self.V("convb", cc), ALU.mult, ALU.add,
                     [xi, self.vec], [a])
                for kt in range(1, 5):
                    k.stt(a[:, lo:lo + n], xi[:, kt:kt + n], self.V("convw", cc * 5 + kt), a[:, lo:lo + n],
                          ALU.mult, ALU.add, [xi, self.vec, a], [a])
            k.act(o[:], a[:], AF.Silu, [a], [o])
            if cc >= 8:
                dst = BT if cc < 12 else CT
                fw.dma(k.q(), dst.t[cc % 4], o[:], o, dst)
            if cc < 12:
                for t4 in range(0, NT, 4):
                    nt = min(4, NT - t4)
                    P = k.bank()
                    Pb = P[:, :].bitcast(BF16)
                    for j in range(nt):
                        k.tr(Pb[:, j * 128:(j + 1) * 128], o[:, (t4 + j) * 128:(t4 + j + 1) * 128],
                             self.C("ident", b16=True), [o, self.conb], P, inc=(j == nt - 1), first=(j == 0))
                    s = stg[si % 2]; si += 1
                    k.cp(s[:, 0:nt, :], Pb[:, 0:nt * 128].rearrange("p (a c) -> p a c", c=128), [P], [s])
                    if cc < 8:
                        dd = xtok.t[t4 * 128:(t4 + nt) * 128, cc * 128:(cc + 1) * 128]
                        dbuf = xtok
                    else:
                        dd = btok.t[t4 * 128:(t4 + nt) * 128, (cc - 8) * 128:(cc - 7) * 128]
                        dbuf = btok
                    fw.dma(k.q(), dd.rearrange("(a p) c -> p a c", p=128), s[:, 0:nt, :], s, dbuf)
        ub = [fw.sbuf("ub%d" % i, [128, T], F32) for i in range(2)]
        sg = [fw.sbuf("sg%d" % i, [128, 512], F32) for i in range(2)]
        gi = 0
        for c in range(8):
            if c % 4 == 0:
                wv = Wg[0]; wg_ = Wg[1]
                fw.dma("pool", wv[:], _wsrc(win.t, 3104 + c * 128, 512), win, wv)
                fw.dma("pool", wg_[:], _wsrc(win.t, 4128 + c * 128, 512), win, wg_)
            u = ub[c % 2]
            for (t0, n) in blocks:
                Pv = k.bank(); Pg = k.bank()
                for kk in range(8):
                    k.mm(Pv[:, 0:n], wv[:, kk, (c % 4) * 128:(c % 4 + 1) * 128], hT[:, kk, t0:t0 + n],
                         kk == 0, kk == 7, [hT, wv], Pv)
                for kk in range(8):
                    k.mm(Pg[:, 0:n], wg_[:, kk, (c % 4) * 128:(c % 4 + 1) * 128], hT[:, kk, t0:t0 + n],
                         kk == 0, kk == 7, [hT, wg_], Pg)
                s = sg[gi % 2]; gi += 1
                k.act(s[:, 0:n], Pg[:, 0:n], AF.Sigmoid, [Pg], [s])
                k.tt(u[:, t0:t0 + n], Pv[:, 0:n], s[:, 0:n], ALU.mult, [Pv, s], [u])
            fw.dma(k.q(), U.t[c], u[:], u, U)
        fw.pop()


class Prog3(Prog2):
    def phase_ssd(self):
        fw, k, dr = self.fw, self.k, self.dr
        xtok, btok, BT, CT, dtk, sz = (dr[n] for n in ("x_tok", "b_tok", "BT", "CT", "dt_tok", "sz_tok"))
        yf = self.scratch("yf_tok", [T, D])
        oT = self.scratch("oT", [16, 128, T], BF16)
        fw.push()
        al = fw.sbuf("al", [128, 32], F32)
        fw.dma("sp", al[:], self.rowbc("alog", 32), dr["rows"], al)
        aneg = fw.sbuf("aneg", [128, 32], F32)
        k.act(aneg[:], al[:], AF.Exp, [al], [aneg])
        k.ts(aneg[:], aneg[:], -1.0, None, ALU.mult, ALU.bypass, [aneg], [aneg])
        dsk = fw.sbuf("dsk", [128, 16], F32)
        fw.dma("act", dsk[:], self.rowbc("ssdd", 16), dr["rows"], dsk)
        S32 = fw.sbuf("S32", [128, D], F32)
        Sbf = fw.sbuf("Sbf", [128, D], BF16)
        Xt = [fw.sbuf("Xt%d" % i, [128, D], BF16) for i in range(2)]
        Bt = [fw.sbuf("Bt%d" % i, [128, 512], BF16) for i in range(2)]
        BTc = [fw.sbuf("BTc%d" % i, [128, 4, 128], BF16) for i in range(2)]
        CTc = [fw.sbuf("CTc%d" % i, [128, 4, 128], BF16) for i in range(2)]
        dtc = [fw.sbuf("dtc%d" % i, [128, 16], F32) for i in range(2)]
        WK = []
        for i in range(2):
            WK.append(dict(
                la=fw.sbuf("la%d" % i, [128, 16], F32), cum=fw.sbuf("cum%d" % i, [128, 16], F32),
                rhs2=fw.sbuf("rhs2%d" % i, [128, 16, 128], F32), Dm=fw.sbuf("Dm%d" % i, [128, 16, 128], F32),
                Eb=fw.sbuf("Eb%d" % i, [128, 16, 128], BF16), cbT=fw.sbuf("cbT%d" % i, [128, 4, 128], BF16),
                M=fw.sbuf("M%d" % i, [128, 16, 128], BF16), eR=fw.sbuf("eR%d" % i, [128, 16, 128], BF16),
                ECT=fw.sbuf("ECT%d" % i, [128, 16, 128], BF16), Rl=fw.sbuf("Rl%d" % i, [128, 16], F32),
                t16=fw.sbuf("t16%d" % i, [128, 16], F32), te=fw.sbuf("te%d" % i, [128, 16], F32),
                cd=fw.sbuf("cd%d" % i, [128, 16], F32), w2=fw.sbuf("w2%d" % i, [128, 16], F32),
                xdt=fw.sbuf("xdt%d" % i, [128, 16, 64], BF16), xdte=fw.sbuf("xdte%d" % i, [128, 16, 64], BF16)))
        yst = [fw.sbuf("yst%d" % i, [128, D], F32) for i in range(2)]
        yft = fw.sbuf("yft", [128, D], F32)
        szt = fw.sbuf("szt", [128, D], F32)
        xd = fw.sbuf("xd", [128, 16, 64], F32)
        junk = fw.sbuf("junk", [128, D], BF16)
        ss = fw.sbuf("ss", [128, 4], F32)
        yn = fw.sbuf("yn", [128, D], BF16)
        ost = [fw.sbuf("ost%d" % i, [128, 8, 128], BF16) for i in range(2)]
        identb = self.C("ident", b16=True)
        for d in range(2):
            tri = self.C("triU" if d == 0 else "triL")
            neg = self.C("negF" if d == 0 else "negB")
            last = 127 if d == 0 else 0
            k.memset(S32[:], 0.0, [S32])
            k.memset(Sbf[:], 0.0, [Sbf])
            order = [16, 17] + list(range(16)) if d == 0 else [17, 16] + list(range(15, -1, -1))
            def head(ci, i):
                t0 = i * 128
                X, B_, BTt, CTt, dt_ = Xt[ci % 2], Bt[ci % 2], BTc[ci % 2], CTc[ci % 2], dtc[ci % 2]
                wk = WK[ci % 2]
                la, cum, rhs2, Dm, Eb, cbT, M, eR, ECT, Rl, t16, te, cd, w2, xdt, xdte = (wk[n_] for n_ in (
                    "la", "cum", "rhs2", "Dm", "Eb", "cbT", "M", "eR", "ECT", "Rl", "t16", "te", "cd", "w2", "xdt", "xdte"))
                fw.dma("sp", X[:], xtok.t[t0:t0 + 128, :], xtok, X)
                fw.dma("act", B_[:], btok.t[t0:t0 + 128, :], btok, B_)
                fw.dma("sp", BTt[:], BT.t[:, :, t0:t0 + 128].rearrange("g n t -> n g t"), BT, BTt)
                fw.dma("act", CTt[:], CT.t[:, :, t0:t0 + 128].rearrange("g n t -> n g t"), CT, CTt)
                fw.dma("sp", dt_[:], dtk.t[t0:t0 + 128, d * 16:(d + 1) * 16], dtk, dt_)
                k.tt(la[:], dt_[:], aneg[:, d * 16:(d + 1) * 16], ALU.mult, [dt_, aneg], [la])
                Pc = k.bank()
                k.mm(Pc[:, 0:16], tri, la[:], True, True, [self.con, la], Pc)
                k.act(cum[:], Pc[:, 0:16], AF.Copy, [Pc], [cum])
                k.tt(rhs2[:], bc(tri.unsqueeze(1), [128, 16, 128]), bc(la[:].unsqueeze(2), [128, 16, 128]), ALU.mult,
                     [self.con, la], [rhs2])
                Rb = []
                for g in range(4):
                    P = k.bank()
                    k.mm(P[:, :], self.C("ones"), rhs2[:, 4 * g:4 * g + 4, :].rearrange("p a b -> p (a b)"), True, True,
                         [self.con, rhs2], P)
                    Rb.append(P)
                for h in range(16):
                    P = Rb[h // 4]
                    k.stt(Dm[:, h, :], P[:, (h % 4) * 128:(h % 4 + 1) * 128], cum[:, h:h + 1], neg, ALU.subtract, ALU.add,
                          [P, cum, self.con], [Dm])
                for g in range(4):
                    P = Rb[g]
                    k.act(eR[:, 4 * g:4 * g + 4, :].rearrange("p a b -> p (a b)"), P[:, :], AF.Exp, [P], [eR])
                    k.cp(Rl[:, 4 * g:4 * g + 4], P[:, :].rearrange("p (a b) -> p a b", b=128)[:, :, last], [P], [Rl])
                k.act(Eb[:].rearrange("p a b -> p (a b)"), Dm[:].rearrange("p a b -> p (a b)"), AF.Exp, [Dm], [Eb])
                Pcb = k.bank()
                for g in range(4):
                    k.mm(Pcb[:, g * 128:(g + 1) * 128], BTt[:, g, :], CTt[:, g, :], True, True, [BTt, CTt], Pcb)
                k.act(cbT[:].rearrange("p a b -> p (a b)"), Pcb[:, :], AF.Copy, [Pcb], [cbT])
                for g in range(4):
                    k.tt(M[:, 4 * g:4 * g + 4, :], Eb[:, 4 * g:4 * g + 4, :], bc(cbT[:, g:g + 1, :], [128, 4, 128]), ALU.mult,
                         [Eb, cbT], [M])
                    k.tt(ECT[:, 4 * g:4 * g + 4, :], eR[:, 4 * g:4 * g + 4, :], bc(CTt[:, g:g + 1, :], [128, 4, 128]), ALU.mult,
                         [eR, CTt], [ECT])
                k.tt(t16[:], Rl[:], cum[:], ALU.subtract, [Rl, cum], [t16])
                k.act(te[:], t16[:], AF.Exp, [t16], [te])
                k.act(cd[:], Rl[:], AF.Exp, [Rl], [cd])
                k.tt(w2[:], dt_[:], te[:], ALU.mult, [dt_, te], [w2])
                Xv = X[:].rearrange("p (h e) -> p h e", e=64)
                k.tt(xdt[:], Xv, bc(dt_[:].unsqueeze(2), [128, 16, 64]), ALU.mult, [X, dt_], [xdt])
                k.tt(xdte[:], Xv, bc(w2[:].unsqueeze(2), [128, 16, 64]), ALU.mult, [X, w2], [xdte])

            def tail(ci, i):
                t0 = i * 128
                X, B_, dt_ = Xt[ci % 2], Bt[ci % 2], dtc[ci % 2]
                wk = WK[ci % 2]
                M, ECT, cd, xdt, xdte = (wk[n_] for n_ in ("M", "ECT", "cd", "xdt", "xdte"))
                Xv = X[:].rearrange("p (h e) -> p h e", e=64)
                Y = [k.bank(), k.bank()]
                for h in range(16):
                    P = Y[h // 8]
                    cs_ = slice((h % 8) * 64, (h % 8 + 1) * 64)
                    k.mm(P[:, cs_], M[:, h, :], xdt[:, h, :], True, False, [M, xdt], P)
                    k.mm(P[:, cs_], ECT[:, h, :], Sbf[:, h * 64:(h + 1) * 64], False, True, [ECT, Sbf], P)
                CS = [k.bank(), k.bank()]
                for g in range(4):
                    P = CS[g // 2]
                    k.mm(P[:, (g % 2) * 256:(g % 2 + 1) * 256], B_[:, g * 128:(g + 1) * 128],
                         xdte[:, 4 * g:4 * g + 4, :].rearrange("p a b -> p (a b)"), True, True, [B_, xdte], P)
                Sv = S32[:].rearrange("p (h e) -> p h e", e=64)
                k.tt(Sv, Sv, bc(cd[:].unsqueeze(2), [128, 16, 64]), ALU.mult, [S32, cd], [S32])
                for j in range(2):
                    k.tt(S32[:, j * 512:(j + 1) * 512], S32[:, j * 512:(j + 1) * 512], CS[j][:, :], ALU.add, [S32, CS[j]], [S32])
                k.cp(Sbf[:], S32[:], [S32], [Sbf])
                if d == 0:
                    ys = yst[ci % 2]
                    for j in range(2):
                        k.act(ys[:, j * 512:(j + 1) * 512], Y[j][:, :], AF.Copy, [Y[j]], [ys])
                    fw.dma("act", yf.t[t0:t0 + 128, :], ys[:], ys, yf)
                else:
                    fw.dma("sp", yft[:], yf.t[t0:t0 + 128, :], yf, yft)
                    fw.dma("act", szt[:], sz.t[t0:t0 + 128, :], sz, szt)
                    ys = yst[ci % 2]
                    for j in range(2):
                        k.tt(ys[:, j * 512:(j + 1) * 512], Y[j][:, :], yft[:, j * 512:(j + 1) * 512], ALU.add, [Y[j], yft], [ys])
                    k.tt(xd[:], Xv, bc(dsk[:].unsqueeze(2), [128, 16, 64]), ALU.mult, [X, dsk], [xd])
                    k.tt(ys[:], ys[:], xd[:].rearrange("p a b -> p (a b)"), ALU.add, [ys, xd], [ys])
                    k.tt(ys[:], ys[:], szt[:], ALU.mult, [ys, szt], [ys])
                    k.act(junk[:], ys[:], AF.Square, [ys], [junk, ss], accum_out=ss[:, 0:1])
                    k.act(ss[:, 1:2], ss[:, 0:1], AF.Sqrt, [ss], [ss], scale=1.0 / D, bias=self.epsb[:, 0:1])
                    fw.op("dve", lambda e: e.reciprocal(ss[:, 2:3], ss[:, 1:2]), [ss], [ss])
                    k.act(yn[:], ys[:], AF.Copy, [ys, ss], [yn], scale=ss[:, 2:3])
                    P = k.bank()
                    Pb = P[:, :].bitcast(BF16)
                    for c in range(8):
                        k.tr(Pb[:, c * 128:(c + 1) * 128], yn[:, c * 128:(c + 1) * 128], identb, [yn, self.conb], P,
                             inc=(c == 7), first=(c == 0))
                    os_ = ost[ci % 2]
                    k.tt(os_[:], Pb.rearrange("p (c t) -> p c t", t=128), bc(self.V("ssdg", 0, 8).unsqueeze(2), [128, 8, 128]),
                         ALU.mult, [P, self.vec], [os_])
                    fw.dma("sp", oT.t[0:8, :, t0:t0 + 128].rearrange("c p t -> p c t"), os_[:], os_, oT)

            head(0, order[0])
            for ci, i in enumerate(order):
                if ci + 1 < len(order):
                    head(ci + 1, order[ci + 1])
                tail(ci, i)
            fw.barrier()
        fw.pop()

    def phase_conformer(self):
        fw, k, dr = self.fw, self.k, self.dr
        U, oT = dr["U_cf"], dr["oT"]
        fw.push()
        cv = [fw.sbuf("cv%d" % c, [128, T], F32) for c in range(8)]
        ub = [fw.sbuf("cu%d" % i, [128, T], F32) for i in range(2)]
        upL = [fw.sbuf("upL%d" % i, [128, 32, 94], BF16) for i in range(2)]
        upC = [fw.sbuf("upC%d" % i, [128, TC + 30], BF16) for i in range(2)]
        dg = [fw.sbuf("dg%d" % i, [128, 31, 128], BF16) for i in range(2)]
        for b_ in upL + upC:
            k.memset(b_[:], 0.0, [b_])
        identb = self.C("ident", b16=True)
        for c in range(8):
            u = ub[c % 2]; pl_, pc_, dgc = upL[c % 2], upC[c % 2], dg[c % 2]
            fw.dma(k.q(), u[:], U.t[c], U, u)
            k.cp(pl_[:, :, 15:79], u[:, 0:TL].rearrange("p (r w) -> p r w", w=64), [u], [pl_], eng="pool")
            k.act(pc_[:, 15:15 + TC], u[:, TL:T], AF.Copy, [u], [pc_])
            k.tt(dgc[:], bc(identb.unsqueeze(1), [128, 31, 128]),
                 bc(self.vec[:, VOFF["cfw"] + c * 31:VOFF["cfw"] + (c + 1) * 31].unsqueeze(2), [128, 31, 128]),
                 ALU.mult, [self.conb, self.vec], [dgc])
            for b in range(5):
                P = k.bank()
                n = 512 if b < 4 else TC
                for j in range(31):
                    rhs = pl_[:, 8 * b:8 * b + 8, j:j + 64] if b < 4 else pc_[:, j:j + TC]
                    k.mm(P[:, 0:n], dgc[:, j, :], rhs, j == 0, j == 30, [dgc, pl_ if b < 4 else pc_], P)
                k.act(cv[c][:, b * 512:b * 512 + n], P[:, 0:n], AF.Identity, [P, self.vec], [cv[c]], bias=self.V("cfb", c))
        sq = [fw.sbuf("sq%d" % i, [128, 512], F32) for i in range(2)]
        mean = fw.sbuf("mean", [128, 512], F32)
        var = fw.sbuf("var", [128, 512], F32)
        tmpb = [fw.sbuf("ct%d" % i, [128, 512], F32) for i in range(2)]
        ob = [fw.sbuf("cob%d" % i, [128, 512], BF16) for i in range(2)]
        ones = self.C("ones")
        qi = 0
        for (t0, n) in [(0, 512), (512, 512), (1024, 512), (1536, 512), (2048, 256)]:
            P1 = k.bank(); P2 = k.bank()
            for c in range(8):
                k.mm(P1[:, 0:n], ones, cv[c][:, t0:t0 + n], c == 0, c == 7, [self.con, cv[c]], P1)
            for c in range(8):
                s = sq[c % 2]
                k.act(s[:, 0:n], cv[c][:, t0:t0 + n], AF.Square, [cv[c]], [s])
                k.mm(P2[:, 0:n], ones, s[:, 0:n], c == 0, c == 7, [self.con, s], P2, inc=True)
            k.ts(mean[:, 0:n], P1[:, 0:n], 1.0 / D, None, ALU.mult, ALU.bypass, [P1], [mean])
            k.tt(var[:, 0:n], mean[:, 0:n], mean[:, 0:n], ALU.mult, [mean], [var])
            k.stt(var[:, 0:n], P2[:, 0:n], 1.0 / D, var[:, 0:n], ALU.mult, ALU.subtract, [P2, var], [var])
            k.act(var[:, 0:n], var[:, 0:n], AF.Sqrt, [var], [var], bias=self.epsb[:, 0:1])
            fw.op("dve", lambda e: e.reciprocal(var[:, 0:n], var[:, 0:n]), [var], [var])
            for c in range(8):
                tb = tmpb[c % 2]; o = ob[c % 2]
                k.tt(tb[:, 0:n], cv[c][:, t0:t0 + n], mean[:, 0:n], ALU.subtract, [cv[c], mean], [tb])
                k.tt(tb[:, 0:n], tb[:, 0:n], var[:, 0:n], ALU.mult, [tb, var], [tb])
                k.act(o[:, 0:n], tb[:, 0:n], AF.Silu, [tb, self.vec], [o], scale=self.V("cflg", c), bias=self.V("cflb", c))
                fw.dma(k.q(), oT.t[8 + c, :, t0:t0 + n], o[:, 0:n], o, oT)
        fw.pop()


class Prog4(Prog3):
    def phase_outproj(self, l, oT, nk, wname, xsrc, hT2, xmid_name, tiles):
        fw, k, dr = self.fw, self.k, self.dr
        xmid = self.scratch(xmid_name, [T, D])
        fw.push()
        W = fw.sbuf("Wout", [128, nk, D], BF16)
        fw.dma("pool", W[:], _wsrc(dr[wname].t, 0, D), dr[wname], W)
        m2 = fw.sbuf("m2", [128, 2, D], F32)
        mr = dr["modrow%d" % l]
        for idx in range(2):
            fw.dma(k.q(), m2[:, idx, :], mr.t[idx, 2 * D:3 * D].partition_broadcast(128), mr, m2)
        AB = self.load_AB(l, (3, 4), "gffn%d" % l)
        tmp = self.norm_tmp()
        ot = [fw.sbuf("ot%d" % i, [128, nk, 128], BF16) for i in range(2)]
        X = [fw.sbuf("Xo%d" % i, [128, D], F32) for i in range(2)]
        for n_, i in enumerate(tiles):
            o = ot[n_ % 2]; x = X[n_ % 2]
            idx = 0 if i < 16 else 1
            fw.dma("sp", o[:], oT.t[:, :, i * 128:(i + 1) * 128].rearrange("c p t -> p c t"), oT, o)
            for (p0, p1, sb, ap) in xsrc(i):
                fw.dma("act", x[p0:p1, :], ap, sb, x)
            for h in range(2):
                P = k.bank()
                for kk in range(nk):
                    k.mm(P[:, :], o[:, kk, :], W[:, kk, h * 512:(h + 1) * 512], kk == 0, kk == nk - 1, [o, W], P)
                hs = slice(h * 512, (h + 1) * 512)
                tq = tmp[n_ % 2]
                k.tt(tq[3][:].rearrange("p a b -> p (a b)")[:, hs], P[:, :], m2[:, idx, hs], ALU.mult, [P, m2], [tq[3]])
                k.tt(x[:, hs], x[:, hs], tq[3][:].rearrange("p a b -> p (a b)")[:, hs], ALU.add, [x, tq[3]], [x])
            fw.dma("sp", xmid.t[i * 128:(i + 1) * 128, :], x[:], x, xmid)
            self.normmod_tile(x, AB, idx, hT2, i * 128, tmp[n_ % 2])
        fw.pop()

    def phase_moe(self, l, hT2, xmid_name, xout, tiles, final=False, hook=None):
        fw, k, dr = self.fw, self.k, self.dr
        xmid = dr[xmid_name]
        nt = len(tiles)
        fw.push()
        Wr = fw.sbuf("Wr", [128, 8, 16], BF16)
        fw.dma("pool", Wr[:], _wsrc(dr["router_w"].t, 0, 16), dr["router_w"], Wr)
        rb = fw.sbuf("rb", [128, 16], F32)
        fw.dma("sp", rb[:], self.rowbc("rb", 16), dr["rows"], rb)
        sc = fw.sbuf("sc", [128, NT, 16], F32)
        sel = fw.sbuf("sel", [128, NT, 16], F32)
        for j, i in enumerate(tiles):
            P = k.bank()
            for kk in range(8):
                k.mm(P[:, 0:16], hT2[:, kk, i * 128:(i + 1) * 128], Wr[:, kk, :], kk == 0, kk == 7, [hT2, Wr], P)
            k.act(sc[:, j, :], P[:, 0:16], AF.Sigmoid, [P], [sc])
        S3 = lambda b_: b_[:, 0:nt, :]
        S4 = lambda b_: b_[:, 0:nt, :].rearrange("p t (g e) -> p t g e", e=4)
        k.tt(S3(sel), S3(sc), bc(rb[:].unsqueeze(1), [128, nt, 16]), ALU.add, [sc, rb], [sel])
        m1 = fw.sbuf("m1", [128, NT, 4], F32)
        m2_ = fw.sbuf("m2_", [128, NT, 4], F32)
        eq = fw.sbuf("eq", [128, NT, 16], F32)
        gs = fw.sbuf("gs", [128, NT, 4], F32)
        gm = fw.sbuf("gm", [128, NT, 1], F32)
        comb = fw.sbuf("comb", [128, NT, 16], F32)
        den = fw.sbuf("den", [128, NT, 1], F32)
        M3 = lambda b_: b_[:, 0:nt, :]
        fw.op("dve", lambda e: e.tensor_reduce(M3(m1), S4(sel), AX.X, ALU.max), [sel], [m1])
        k.tt(S4(eq), S4(sel), bc(M3(m1).unsqueeze(3), [128, nt, 4, 4]), ALU.is_equal, [sel, m1], [eq])
        k.stt(S3(eq), S3(eq), -1e9, S3(sel), ALU.mult, ALU.add, [eq, sel], [eq])
        fw.op("dve", lambda e: e.tensor_reduce(M3(m2_), S4(eq), AX.X, ALU.max), [eq], [m2_])
        k.tt(M3(gs), M3(m1), M3(m2_), ALU.add, [m1, m2_], [gs])
        fw.op("dve", lambda e: e.tensor_reduce(M3(gm), M3(gs), AX.X, ALU.max), [gs], [gm])
        k.tt(M3(gs), M3(gs), bc(M3(gm), [128, nt, 4]), ALU.is_equal, [gs, gm], [gs])
        k.tt(S4(eq), S4(sel), bc(M3(m2_).unsqueeze(3), [128, nt, 4, 4]), ALU.is_ge, [sel, m2_], [eq])
        k.tt(S4(eq), S4(eq), bc(M3(gs).unsqueeze(3), [128, nt, 4, 4]), ALU.mult, [eq, gs], [eq])
        k.tt(S3(comb), S3(eq), S3(sc), ALU.mult, [eq, sc], [comb])
        fw.op("dve", lambda e: e.tensor_reduce(M3(den), S3(comb), AX.X, ALU.add), [comb], [den])
        fw.op("dve", lambda e: e.reciprocal(M3(den), M3(den)), [den], [den])
        k.tt(S3(comb), S3(comb), bc(M3(den), [128, nt, 16]), ALU.mult, [comb, den], [comb])
        if "comb%d" % l in self.dbg:
            cdb = self.scratch("comb%d" % l, [128, NT, 16])
            fw.dma("sp", cdb.t[:], comb[:], comb, cdb)
        acc = fw.sbuf("acc", [128, NT, D], F32)
        k.memset(acc[:, 0:nt // 2, :], 0.0, [acc], eng="pool")
        k.memset(acc[:, nt // 2:nt, :], 0.0, [acc], eng="dve")
        Wg = [fw.sbuf("Wg%d" % i, [128, 8, 512], BF16) for i in range(2)]
        Wu = [fw.sbuf("Wu%d" % i, [128, 8, 512], BF16) for i in range(2)]
        Wd = [fw.sbuf("Wd%d" % i, [128, 4, D], BF16) for i in range(2)]
        sg = [fw.sbuf("sg%d" % i, [128, 512], F32) for i in range(2)]
        aT = [fw.sbuf("aT%d" % i, [128, 4, 512], BF16) for i in range(2)]
        blocks = [tiles[j:j + 4] for j in range(0, nt, 4)]
        si = 0
        if hook:
            fw.push()
            self.fw_mod_double = False
        hst = hook[0]() if hook else None
        for e_ in range(16):
            if hook and e_ >= 2 and e_ - 2 < 12:
                hook[1](hst, e_ - 2)
            wg, wu, wd = Wg[e_ % 2], Wu[e_ % 2], Wd[e_ % 2]
            fw.dma("pool", wg[:], _wsrc(dr["moe_w_gate"].t[l, e_], 0, 512), dr["moe_w_gate"], wg)
            fw.dma("pool", wu[:], _wsrc(dr["moe_w_up"].t[l, e_], 0, 512), dr["moe_w_up"], wu)
            fw.dma("pool", wd[:], _wsrc(dr["moe_w_down"].t[l, e_], 0, D), dr["moe_w_down"], wd)
            for bi, blk in enumerate(blocks):
                t0 = blk[0] * 128
                n = len(blk) * 128
                a = aT[bi % 2]
                for fc in range(4):
                    Pg = k.bank(); Pu = k.bank()
                    for kk in range(8):
                        k.mm(Pg[:, 0:n], wg[:, kk, fc * 128:(fc + 1) * 128], hT2[:, kk, t0:t0 + n], kk == 0, kk == 7, [wg, hT2], Pg)
                    for kk in range(8):
                        k.mm(Pu[:, 0:n], wu[:, kk, fc * 128:(fc + 1) * 128], hT2[:, kk, t0:t0 + n], kk == 0, kk == 7, [wu, hT2], Pu)
                    s = sg[si % 2]; si += 1
                    k.act(s[:, 0:n], Pg[:, 0:n], AF.Silu, [Pg], [s])
                    k.tt(a[:, fc, 0:n], Pu[:, 0:n], s[:, 0:n], ALU.mult, [Pu, s], [a])
                for jt, i in enumerate(blk):
                    j = tiles.index(i)
                    for h in range(2):
                        P = k.bank()
                        for fc in range(4):
                            k.mm(P[:, :], a[:, fc, jt * 128:(jt + 1) * 128], wd[:, fc, h * 512:(h + 1) * 512], fc == 0, fc == 3, [a, wd], P)
                        k.stt(acc[:, j, h * 512:(h + 1) * 512], P[:, :], comb[:, j, e_:e_ + 1], acc[:, j, h * 512:(h + 1) * 512],
                              ALU.mult, ALU.add, [P, comb, acc], [acc])
        if hook:
            hook[2](hst)
            fw.pop()
        m5 = fw.sbuf("m5", [128, 2, D], F32)
        mr = dr["modrow%d" % l]
        for idx in range(2):
            fw.dma(k.q(), m5[:, idx, :], mr.t[idx, 5 * D:6 * D].partition_broadcast(128), mr, m5)
        X = [fw.sbuf("Xm%d" % i, [128, D], F32) for i in range(2)]
        if final:
            fg = fw.sbuf("fg", [128, D], F32)
            fw.dma("sp", fg[:], self.rowbc("fng", D), dr["rows"], fg)
            junk = fw.sbuf("junkf", [128, D], BF16)
            ss = fw.sbuf("ssf", [128, 4], F32)
        for j, i in enumerate(tiles):
            x = X[j % 2]
            idx = 0 if i < 16 else 1
            fw.dma("sp", x[:], xmid.t[i * 128:(i + 1) * 128, :], xmid, x)
            k.tt(acc[:, j, :], acc[:, j, :], m5[:, idx, :], ALU.mult, [acc, m5], [acc])
            k.tt(x[:], x[:], acc[:, j, :], ALU.add, [x, acc], [x])
            if final:
                k.act(junk[:], x[:], AF.Square, [x], [junk, ss], accum_out=ss[:, 0:1])
                k.act(ss[:, 1:2], ss[:, 0:1], AF.Sqrt, [ss], [ss], scale=1.0 / D, bias=self.epsb[:, 0:1])
                fw.op("dve", lambda e: e.reciprocal(ss[:, 2:3], ss[:, 1:2]), [ss], [ss])
                k.stt(x[:], x[:], ss[:, 2:3], fg[:], ALU.mult, ALU.mult, [x, ss, fg], [x])
                for (p0, p1, sb, ap) in self.cm_rows(xout, i):
                    fw.dma("act", ap, x[p0:p1, :], x, xout)
            else:
                fw.dma("act", xout.t[i * 128:(i + 1) * 128, :], x[:], x, xout)
        fw.pop()


class Prog5(Prog4):
    def cm_rows(self, db, i):
        v = db.t[0:TL, :].rearrange("(r w) d -> w r d", w=64)
        return [(32 * j, 32 * j + 32, db, v[4 * i + j]) for j in range(4)]

    def phase_normmod1(self, hT, x1):
        fw, k = self.fw, self.k
        fw.push()
        AB = self.load_AB(1, (0, 1), "gmix1")
        tmp = self.norm_tmp()
        X = [fw.sbuf("X%d" % i, [128, D], F32) for i in range(2)]
        for i in range(NT):
            x = X[i % 2]
            if i < 16:
                for (p0, p1, sb, ap) in self.cm_rows(x1, i):
                    fw.dma(k.q(), x[p0:p1, :], ap, sb, x)
            else:
                fw.dma(k.q(), x[:], x1.t[i * 128:(i + 1) * 128, :], x1, x)
            self.normmod_tile(x, AB, 0 if i < 16 else 1, hT, i * 128, tmp[i % 2])
        fw.pop()

    def phase_hgproj(self, hT):
        fw, k, dr = self.fw, self.k, self.dr
        win = dr["hg_w_in"]
        QT = self.scratch("QT", [8, 128, TL])
        LF = self.scratch("LF", [2, 8, 128, T])
        KT = self.scratch("KT", [2, 8, 128, T])
        sgt = self.scratch("sg_tok", [TL, D])
        vtk = self.scratch("v_tok", [T, D], BF16)
        fw.push()
        lb = fw.sbuf("lb", [128, 8], F32)
        oml = fw.sbuf("oml", [128, 8], F32)
        k.tt(lb[:], self.V("hglb1", 0, 8), self.V("hglb0", 0, 8), ALU.subtract, [self.vec], [lb])
        k.act(lb[:], lb[:], AF.Sigmoid, [lb], [lb])
        k.ts(oml[:], lb[:], -1.0, 1.0, ALU.mult, ALU.add, [lb], [oml])
        W = [fw.sbuf("Wh%d" % i, [128, 8, 512], BF16) for i in range(2)]
        wi = 0
        stf = [fw.sbuf("stf%d" % i, [128, 512], F32) for i in range(2)]
        stb = [fw.sbuf("stb%d" % i, [128, 512], BF16) for i in range(2)]
        si = 0
        for (c0, ntl, isg) in ((1024, 16, True), (1536, 16, True), (2048, NT, False), (2560, NT, False)):
            w = W[wi % 2]; wi += 1
            fw.dma("pool", w[:], _wsrc(win.t, c0, 512), win, w)
            for i in range(ntl):
                P = k.bank()
                for kk in range(8):
                    k.mm(P[:, :], hT[:, kk, i * 128:(i + 1) * 128], w[:, kk, :], kk == 0, kk == 7, [hT, w], P)
                if isg:
                    s = stf[si % 2]; si += 1
                    k.act(s[:], P[:, :], AF.Silu, [P], [s])
                    fw.dma(k.q(), sgt.t[i * 128:(i + 1) * 128, c0 - 1024:c0 - 512], s[:], s, sgt)
                else:
                    s = stb[si % 2]; si += 1
                    k.act(s[:], P[:, :], AF.Copy, [P], [s])
                    fw.dma(k.q(), vtk.t[i * 128:(i + 1) * 128, c0 - 2048:c0 - 1536], s[:], s, vtk)
        blocks = [(0, 512), (512, 512), (1024, 512), (1536, 512), (2048, 256)]
        ob = [fw.sbuf("hob%d" % i, [128, T], F32) for i in range(2)]
        ob2 = [fw.sbuf("hob2%d" % i, [128, T], F32) for i in range(2)]
        sgm = [fw.sbuf("sgm%d" % i, [128, 512], F32) for i in range(2)]
        oi = 0
        for grp in range(6):
            c0 = (0, 512, 3072, 3584, 4096, 4608)[grp]
            w = W[wi % 2]; wi += 1
            fw.dma("pool", w[:], _wsrc(win.t, c0, 512), win, w)
            for hh in range(4):
                h = (grp % 2) * 4 + hh
                o = ob[oi % 2]; o2 = ob2[oi % 2]; oi += 1
                for (t0, n) in (blocks[:4] if grp < 2 else blocks):
                    P = k.bank()
                    for kk in range(8):
                        k.mm(P[:, 0:n], w[:, kk, hh * 128:(hh + 1) * 128], hT[:, kk, t0:t0 + n], kk == 0, kk == 7, [hT, w], P)
                    if grp < 2:
                        k.act(o[:, t0:t0 + n], P[:, 0:n], AF.Silu, [P], [o])
                    else:
                        sg_ = sgm[si % 2]; si += 1
                        k.act(sg_[:, 0:n], P[:, 0:n], AF.Sigmoid, [P], [sg_])
                        k.ts(o[:, t0:t0 + n], sg_[:, 0:n], oml[:, h:h + 1], lb[:, h:h + 1], ALU.mult, ALU.add, [sg_, oml, lb], [o])
                        k.ts(o2[:, t0:t0 + n], o[:, t0:t0 + n], -1.0, 1.0, ALU.mult, ALU.add, [o], [o2])
                if grp < 2:
                    fw.dma(k.q(), QT.t[h], o[:, 0:TL], o, QT)
                else:
                    dd = (grp - 2) // 2
                    fw.dma(k.q(), KT.t[dd, h], o2[:], o2, KT)
                    k.act(o[:], o[:], AF.Ln, [o], [o])
                    fw.dma(k.q(), LF.t[dd, h], o[:], o, LF)
        fw.pop()

    def phase_hgscan(self):
        fw, k, dr = self.fw, self.k, self.dr
        QT, LF, KT, vtk = dr["QT"], dr["LF"], dr["KT"], dr["v_tok"]
        otot = self.scratch("o_tot", [TL, D])
        fw.push()
        NCK = T // 64
        rst = fw.sbuf("rst", [128, T], F32)
        k.memset(rst[:], 1.0, [rst])
        k.memset(rst[:].rearrange("p (c l) -> p c l", l=64)[:, :, 0:1], 0.0, [rst])
        Vh = [fw.sbuf("Vh%d" % i, [64, NCK, 128], BF16) for i in range(1)] * 2
        qb = [fw.sbuf("hq%d" % i, [128, TL], F32) for i in range(1)] * 2
        lf = [fw.sbuf("hlf%d" % i, [128, T], F32) for i in range(1)] * 2
        kt = [fw.sbuf("hkt%d" % i, [128, T], F32) for i in range(1)] * 2
        G = fw.sbuf("hG", [128, T], F32)
        D1 = fw.sbuf("hD1", [128, T], F32)
        D2 = fw.sbuf("hD2", [128, T], F32)
        E = [fw.sbuf("hE%d" % i, [128, T], F32) for i in range(4)]
        qrel = [fw.sbuf("qrel%d" % i, [128, TL], BF16) for i in range(2)]
        krel = [fw.sbuf("krel%d" % i, [128, TL], BF16) for i in range(2)]
        qdec = [fw.sbuf("qdec%d" % i, [128, TL], BF16) for i in range(2)]
        kend = [fw.sbuf("kend%d" % i, [128, T], BF16) for i in range(2)]
        dec = [fw.sbuf("hdec%d" % i, [128, NCK], F32) for i in range(2)]
        S32 = [fw.sbuf("hS32%d" % i, [128, 128], F32) for i in range(2)]
        Sbf = [fw.sbuf("hSbf%d" % i, [128, 128], BF16) for i in range(2)]
        ktok = [fw.sbuf("ktok%d" % i, [64, NCK, 128], BF16) for i in range(2)]
        attT = [fw.sbuf("attT%d" % i, [64, 32, 64], BF16) for i in range(2)]
        Oall = [fw.sbuf("Oall%d" % i, [64, 32, 128], F32) for i in range(2)]
        identb = self.C("ident", b16=True)
        c3 = lambda ap: ap.rearrange("p (c l) -> p c l", l=64)
        for h in range(8):
            V = Vh[h % 2]; q = qb[h % 2]
            fw.dma("sp", V[:], vtk.t[:, h * 128:(h + 1) * 128].rearrange("(c p) v -> p c v", p=64), vtk, V)
            fw.dma("act", q[:], QT.t[h], QT, q)
            for dd in range(2):
                l_, k_ = lf[dd], kt[dd]
                fw.dma("sp", l_[:], LF.t[dd, h], LF, l_)
                fw.dma("act", k_[:], KT.t[dd, h], KT, k_)
                mid, tot = (31, 63) if dd == 0 else (32, 0)
                fw.op("dve", lambda e: e.tensor_tensor_scan(G[:], rst[:], l_[:], 0.0, ALU.mult, ALU.add), [rst, l_], [G])
                if dd == 1:
                    k.tt(D1[:], l_[:], G[:], ALU.subtract, [l_, G], [D1])
                    k.tt(c3(G[:]), c3(D1[:]), bc(c3(G[:])[:, :, 63:64], [128, NCK, 64]), ALU.add, [D1, G], [G])
                Gm = bc(c3(G[:])[:, :, mid:mid + 1], [128, NCK, 64])
                Gt = bc(c3(G[:])[:, :, tot:tot + 1], [128, NCK, 64])
                E0, E1, E2, E3 = E
                k.tt(c3(D1[:]), c3(G[:]), Gm, ALU.subtract, [G], [D1])
                k.tt(c3(D2[:]), Gt, c3(G[:]), ALU.subtract, [G], [D2])
                k.act(E0[:, 0:TL], D1[:, 0:TL], AF.Exp, [D1], [E0])
                k.act(E1[:, 0:TL], D1[:, 0:TL], AF.Exp, [D1], [E1], scale=-1.0)
                k.act(E2[:], D2[:], AF.Exp, [D2], [E2])
                k.act(E3[:, 0:TL], G[:, 0:TL], AF.Exp, [G], [E3])
                k.tt(qrel[dd][:], q[:], E0[:, 0:TL], ALU.mult, [q, E0], [qrel[dd]])
                k.tt(krel[dd][:], k_[:, 0:TL], E1[:, 0:TL], ALU.mult, [k_, E1], [krel[dd]])
                k.tt(kend[dd][:], k_[:], E2[:], ALU.mult, [k_, E2], [kend[dd]])
                k.tt(qdec[dd][:], q[:], E3[:, 0:TL], ALU.mult, [q, E3], [qdec[dd]])
                k.act(dec[dd][:], c3(G[:])[:, :, tot], AF.Exp, [G], [dec[dd]])
                k.memset(S32[dd][:], 0.0, [S32[dd]])
                k.memset(Sbf[dd][:], 0.0, [Sbf[dd]])
                for c0 in range(0, NCK, 8):
                    nb = min(8, NCK - c0)
                    Pt = k.bank()
                    Ptb = Pt[:, :].bitcast(BF16)
                    for j in range(nb):
                        k.tr(Ptb[0:64, j * 128:(j + 1) * 128], kend[dd][:, (c0 + j) * 64:(c0 + j + 1) * 64], identb,
                             [kend[dd], self.conb], Pt, inc=(j == nb - 1), first=(j == 0))
                    k.act(ktok[dd][:, c0:c0 + nb, :].rearrange("p a b -> p (a b)"), Ptb[0:64, 0:nb * 128], AF.Copy, [Pt], [ktok[dd]])
                mask = self.C("mF64" if dd == 0 else "mB64", w=64, rows=64)
                for c0 in range(0, 32, 8):
                    Pa = k.bank()
                    for j in range(8):
                        c = c0 + j
                        k.mm(Pa[0:64, j * 64:(j + 1) * 64], krel[dd][:, c * 64:(c + 1) * 64], qrel[dd][:, c * 64:(c + 1) * 64],
                             True, True, [krel[dd], qrel[dd]], Pa, inc=(j == 7))
                    k.tt(attT[dd][:, c0:c0 + 8, :], Pa[0:64, :].rearrange("p (a b) -> p a b", b=64),
                         bc(mask.unsqueeze(1), [64, 8, 64]), ALU.mult, [Pa, self.con], [attT[dd]])
            orders = [list(range(32, 36)) + list(range(32)), list(range(35, 31, -1)) + list(range(31, -1, -1))]
            Po = [None, None]
            Pcn = [None, None]

            def emit_pc(dd, step):
                c_ = orders[dd][step]
                Pn = k.bank()
                k.mm(Pn[:, 0:128], ktok[dd][:, c_, :], V[:, c_, :], True, True, [ktok[dd], V], Pn)
                Pcn[dd] = Pn
            for dd in range(2):
                emit_pc(dd, 0)
            for step in range(NCK):
                for dd in range(2):
                    c = orders[dd][step]
                    lat = c < 32
                    Pc = Pcn[dd]
                    if step + 1 < NCK:
                        emit_pc(dd, step + 1)
                    if lat:
                        j = c % 4
                        first = (j == 0) if dd == 0 else (j == 3)
                        last_ = (j == 3) if dd == 0 else (j == 0)
                        if first:
                            Po[dd] = k.bank()
                        P = Po[dd]
                        k.mm(P[0:64, j * 128:(j + 1) * 128], attT[dd][:, c, :], V[:, c, :], True, False, [attT[dd], V], P)
                        k.mm(P[0:64, j * 128:(j + 1) * 128], qdec[dd][:, c * 64:(c + 1) * 64], Sbf[dd][:], False, True,
                             [qdec[dd], Sbf[dd]], P, inc=True)
                        if last_:
                            cg = c - j
                            k.act(Oall[dd][:, cg:cg + 4, :].rearrange("p a b -> p (a b)"), P[0:64, :], AF.Copy, [P], [Oall[dd]])
                    k.stt(S32[dd][:], S32[dd][:], dec[dd][:, c:c + 1], Pc[:, 0:128], ALU.mult, ALU.add, [S32[dd], dec[dd], Pc], [S32[dd]])
                    k.cp(Sbf[dd][:], S32[dd][:], [S32[dd]], [Sbf[dd]])
            k.tt(Oall[0][:, 0:16, :], Oall[0][:, 0:16, :], Oall[1][:, 0:16, :], ALU.add, [Oall[0], Oall[1]], [Oall[0]])
            k.tt(Oall[0][:, 16:32, :], Oall[0][:, 16:32, :], Oall[1][:, 16:32, :], ALU.add, [Oall[0], Oall[1]], [Oall[0]], eng="pool")
            for half in range(2):
                fw.dma(k.q(), otot.t[half * 1024:(half + 1) * 1024, h * 128:(h + 1) * 128].rearrange("(c p) v -> p c v", p=64),
                       Oall[0][:, half * 16:(half + 1) * 16, :], Oall[0], otot)
        fw.pop()

    def phase_hgout(self):
        fw, k, dr = self.fw, self.k, self.dr
        otot, sgt = dr["o_tot"], dr["sg_tok"]
        oT2 = self.scratch("oT2", [8, 128, T], BF16)
        fw.push()
        gb = fw.sbuf("hgng", [128, 128], F32)
        fw.dma("sp", gb[:], self.rowbc("hgng", 128), dr["rows"], gb)
        O = [fw.sbuf("hO%d" % i, [128, 8, 128], F32) for i in range(2)]
        SG = [fw.sbuf("hSG%d" % i, [128, 8, 128], F32) for i in range(2)]
        sq = fw.sbuf("hsq", [128, 8, 128], F32)
        ss = fw.sbuf("hss", [128, 8], F32)
        on = fw.sbuf("hon", [128, 8, 128], BF16)
        stg = [fw.sbuf("hstg%d" % i, [128, 8, 128], BF16) for i in range(2)]
        identb = self.C("ident", b16=True)
        f2 = lambda b_: b_[:].rearrange("p a b -> p (a b)")
        for i in range(16):
            o, sg = O[i % 2], SG[i % 2]
            fw.dma("sp", f2(o), otot.t[i * 128:(i + 1) * 128, :], otot, o)
            fw.dma("act", f2(sg), sgt.t[i * 128:(i + 1) * 128, :], sgt, sg)
            k.tt(sq[:], o[:], o[:], ALU.mult, [o], [sq])
            fw.op("dve", lambda e: e.tensor_reduce(ss[:], sq[:], AX.X, ALU.add), [sq], [ss])
            k.act(ss[:], ss[:], AF.Sqrt, [ss], [ss], scale=1.0 / 128, bias=self.epsb[:, 0:1])
            fw.op("dve", lambda e: e.reciprocal(ss[:], ss[:]), [ss], [ss])
            k.tt(o[:], o[:], bc(ss[:].unsqueeze(2), [128, 8, 128]), ALU.mult, [o, ss], [o])
            k.tt(o[:], o[:], bc(gb[:].unsqueeze(1), [128, 8, 128]), ALU.mult, [o, gb], [o])
            k.tt(on[:], o[:], sg[:], ALU.mult, [o, sg], [on])
            P = k.bank()
            Pb = P[:, :].bitcast(BF16)
            for c in range(8):
                k.tr(Pb[:, c * 128:(c + 1) * 128], on[:, c, :], identb, [on, self.conb], P, inc=(c == 7), first=(c == 0))
            s = stg[i % 2]
            k.act(f2(s), Pb, AF.Copy, [P], [s])
            fw.dma("sp", oT2.t[:, :, i * 128:(i + 1) * 128].rearrange("c p t -> p c t"), s[:], s, oT2)
        fw.pop()

    def build_full(self):
        fw, dr = self.fw, self.dr
        self.declare_io()
        out = self.scratch("out", [TL, D], out=True)
        self.setup_consts()
        self.phase_mod(0)
        fw.push()
        hT = fw.sbuf("hT", [128, 8, T], BF16)
        self.phase_normmod0(hT)
        self.phase_evenproj(hT)
        fw.pop()
        self.phase_ssd()
        self.phase_conformer()
        fw.push()
        hT2 = fw.sbuf("hT2", [128, 8, T], BF16)
        xsrc0 = lambda i: [(0, 128, dr["x"], dr["x"].t[i * 128:(i + 1) * 128, :])] if i < 16 else \
            [(0, 128, dr["ctx"], dr["ctx"].t[(i - 16) * 128:(i - 15) * 128, :])]
        self.phase_outproj(0, dr["oT"], 16, "ab_w_out", xsrc0, hT2, "xmid0", list(range(NT)))
        x1 = self.scratch("x1", [T, D])
        self.phase_moe(0, hT2, "xmid0", x1, list(range(NT)),
                       hook=(lambda: self.mod_setup(1), self.mod_group, self.mod_finish))
        fw.pop()
        fw.push()
        hT = fw.sbuf("hTb", [128, 8, T], BF16)
        self.phase_normmod1(hT, x1)
        self.phase_hgproj(hT)
        fw.pop()
        self.phase_hgscan()
        self.phase_hgout()
        fw.push()
        hT2 = fw.sbuf("hT2b", [128, 8, T], BF16)
        self.phase_outproj(1, dr["oT2"], 8, "hg_w_out", lambda i: self.cm_rows(x1, i), hT2, "xmid1", list(range(16)))
        self.phase_moe(1, hT2, "xmid1", out, list(range(16)), final=True)
        fw.pop()
        fw.barrier()
        st = simulate(fw)
        assert not st, "sync deadlock: %r" % (st,)
        return self.nc


_CACHE = {}


def kernel(**inputs):
    n = 8
    if "nc" not in _CACHE:
        _CACHE["nc"] = Prog5().build_full()
    nc = _CACHE["nc"]
    maps = make_inmaps(inputs, list(range(n)))
    res = run_bass_kernel_spmd(nc, maps, core_ids=list(range(n)))
    return np.stack([np.asarray(r["out"], np.float32) for r in res.results], axis=0)
```

```python
import numpy as np
from contextlib import ExitStack
import concourse.bass as bass
import concourse.mybir as mybir
from concourse.bass_utils import run_bass_kernel_spmd

F32 = mybir.dt.float32
BF16 = mybir.dt.bfloat16
AF = mybir.ActivationFunctionType
ALU = mybir.AluOpType
AX = mybir.AxisListType

D = 1024
TL = 2048
TC = 256
T = TL + TC
NT = T // 128
EPS = 1e-6
NEG = -30000.0


class Buf:
    __slots__ = ("name", "t", "writer", "readers", "ld", "st", "onchip", "excl")

    def __init__(self, name, t, onchip):
        self.name = name
        self.t = t
        self.writer = None
        self.readers = []
        self.ld = None
        self.st = None
        self.onchip = onchip
        self.excl = False

    def __getitem__(self, k):
        return self.t[k]


class FW:
    ENG = ("pe", "act", "dve", "pool", "sp")

    def __init__(self, nc, es, n_dma_sems=90):
        self.nc = nc
        self.es = es
        self.scopes = [es]
        self.eng = {"pe": nc.tensor, "act": nc.scalar, "dve": nc.vector, "pool": nc.gpsimd, "sp": nc.sync}
        self.sems = {}
        self.cnt = {}
        for e in self.ENG:
            self.sems[e] = es.enter_context(nc.semaphore("s_" + e))
            self.cnt[e] = 0
        self.free_dma = []
        for i in range(n_dma_sems):
            k = "d%d" % i
            self.sems[k] = es.enter_context(nc.semaphore("s_" + k))
            self.cnt[k] = 0
            self.free_dma.append(k)
        self.phase_dma = []
        self.seen = {e: {} for e in self.ENG}
        self.bufs = []
        self.n_inst = 0
        self.uid = 0
        self.log = {e: [] for e in self.ENG}

    def push(self):
        s = ExitStack()
        self.scopes.append(s)
        s._bufs0 = len(self.bufs)

    def pop(self):
        self.barrier()
        s = self.scopes.pop()
        del self.bufs[s._bufs0:]
        s.close()

    def _nm(self, name):
        self.uid += 1
        return "%s_%d" % (name, self.uid)

    def sbuf(self, name, shape, dtype):
        t = self.scopes[-1].enter_context(self.nc.sbuf_tensor(self._nm(name), list(shape), dtype))
        b = Buf(name, t, True)
        self.bufs.append(b)
        return b

    def psum(self, name, shape, dtype):
        t = self.scopes[-1].enter_context(self.nc.psum_tensor(self._nm(name), list(shape), dtype))
        b = Buf(name, t, True)
        b.excl = True
        self.bufs.append(b)
        return b

    def dram(self, name, t):
        b = Buf(name, t, False)
        self.bufs.append(b)
        return b

    def _need(self, e, tok, waits):
        if tok is None:
            return
        k, v = tok
        if self.seen[e].get(k, 0) >= v:
            return
        if waits.get(k, 0) < v:
            waits[k] = v

    def _deps(self, e, reads, writes, pe_accum=False, attach=False):
        waits = {}
        for b in reads:
            self._need(e, b.writer, waits)
            if b.excl:
                for r in b.readers:
                    if r[0] != e:
                        self._need(e, r, waits)
        for b in writes:
            if not (e == "pe" and b.writer is not None and b.writer[0] == "pe"):
                self._need(e, b.writer, waits)
            for r in b.readers:
                self._need(e, r, waits)
        items = list(waits.items())
        self.pending = None
        if attach and items:
            self.pending = items.pop()
        for k, v in items:
            self.eng[e].wait_ge(self.sems[k], v)
            self.seen[e][k] = v
            self.log[e].append(("w", k, v))
        if self.pending is not None:
            k, v = self.pending
            self.seen[e][k] = v
            self.log[e].append(("w", k, v))

    def op(self, e, fn, reads=(), writes=(), inc=True, pe_accum=False):
        self._deps(e, reads, writes, pe_accum, attach=True)
        ins = fn(self.eng[e])
        if self.pending is not None:
            ins._wait_ge(self.sems[self.pending[0]], self.pending[1])
        self.n_inst += 1
        tok = (e, self.cnt[e] + 1)
        if inc:
            self.cnt[e] += 1
            ins.then_inc(self.sems[e], 1)
            self.log[e].append(("i", e, 1))
        for b in reads:
            b.readers.append(tok)
            if len(b.readers) > 24:
                b.readers = _compact(b.readers)
        for b in writes:
            b.writer = tok
            b.readers = []
        return ins

    def _dma_sem(self):
        k = self.free_dma.pop()
        self.phase_dma.append(k)
        return k

    def dma(self, q, out_ap, in_ap, src, dst, **kw):
        self._deps(q, [src], [dst], attach=True)
        pend = self.pending
        if dst.onchip:
            if dst.ld is None:
                dst.ld = self._dma_sem()
            k = dst.ld
        else:
            if src.st is None:
                src.st = self._dma_sem()
            k = src.st
        self.cnt[k] += 16
        ins = self.eng[q].dma_start(out=out_ap, in_=in_ap, **kw)
        if pend is not None:
            ins._wait_ge(self.sems[pend[0]], pend[1])
        ins.then_inc(self.sems[k], 16)
        self.log[q].append(("i", k, 16))
        self.n_inst += 1
        tok = (k, self.cnt[k])
        src.readers.append(tok)
        if len(src.readers) > 24:
            src.readers = _compact(src.readers)
        if dst.onchip:
            dst.writer = tok
            dst.readers = []
        return ins

    def barrier(self):
        sp = self.eng["sp"]
        keys = [k for k in self.ENG if k != "sp"] + self.phase_dma
        for k in keys:
            v = self.cnt[k]
            if v > 0 and self.seen["sp"].get(k, 0) < v:
                sp.wait_ge(self.sems[k], v)
                self.seen["sp"][k] = v
                self.log["sp"].append(("w", k, v))
        self.cnt["sp"] += 1
        sp.nop().then_inc(self.sems["sp"], 1)
        self.log["sp"].append(("i", "sp", 1))
        v = self.cnt["sp"]
        for e in self.ENG:
            if e == "sp":
                continue
            self.log[e].append(("w", "sp", v))
            self.eng[e].wait_ge(self.sems["sp"], v)
            self.seen[e]["sp"] = v
            for k in keys:
                self.seen[e][k] = self.cnt[k]
        for b in self.bufs:
            b.writer = None
            b.readers = []
            b.ld = None
            b.st = None
        self.free_dma.extend(self.phase_dma)
        self.phase_dma = []


def simulate(fw):
    pos = {e: 0 for e in fw.ENG}
    val = {}
    prog = True
    while prog:
        prog = False
        for e in fw.ENG:
            L = fw.log[e]
            while pos[e] < len(L):
                kind, k, v = L[pos[e]]
                if kind == "w":
                    if val.get(k, 0) < v:
                        break
                else:
                    val[k] = val.get(k, 0) + v
                pos[e] += 1
                prog = True
    stuck = {e: (pos[e], len(fw.log[e]), fw.log[e][pos[e]], val.get(fw.log[e][pos[e]][1], 0))
             for e in fw.ENG if pos[e] < len(fw.log[e])}
    return stuck


def _compact(toks):
    m = {}
    for k, v in toks:
        if m.get(k, 0) < v:
            m[k] = v
    return list(m.items())


class KB:
    def __init__(self, nc, fw):
        self.nc = nc
        self.fw = fw
        self.banks = [fw.psum("bank%d" % i, [128, 512], F32) for i in range(8)]
        self.bi = 0
        self.dq = 0

    def bank(self):
        b = self.banks[self.bi]
        self.bi = (self.bi + 1) % 8
        return b

    def q(self):
        self.dq ^= 1
        return "sp" if self.dq else "act"

    def mm(self, out, lhsT, rhs, start, stop, reads, wr, inc=None):
        if inc is None:
            inc = stop
        return self.fw.op("pe", lambda e: e.matmul(out, lhsT, rhs, start=start, stop=stop),
                          reads=reads, writes=[wr], inc=inc, pe_accum=not start)

    def tr(self, out, in_, ident, reads, wr, inc=True, first=False):
        return self.fw.op("pe", lambda e: e.transpose(out, in_, ident), reads=reads, writes=[wr],
                          inc=inc, pe_accum=not first)

    def act(self, out, in_, func, reads, writes, **kw):
        return self.fw.op("act", lambda e: e.activation(out, in_, func, **kw), reads=reads, writes=writes)

    def tt(self, out, a, b, op, reads, writes, eng="dve"):
        return self.fw.op(eng, lambda e: e.tensor_tensor(out, a, b, op), reads=reads, writes=writes)

    def ts(self, out, a, s1, s2, op0, op1, reads, writes, eng="dve", **kw):
        return self.fw.op(eng, lambda e: e.tensor_scalar(out, a, s1, s2, op0, op1, **kw), reads=reads, writes=writes)

    def stt(self, out, a, s, b, op0, op1, reads, writes):
        return self.fw.op("dve", lambda e: e.scalar_tensor_tensor(out, a, s, b, op0, op1), reads=reads, writes=writes)

    def cp(self, out, a, reads, writes, eng="dve"):
        return self.fw.op(eng, lambda e: e.tensor_copy(out, a), reads=reads, writes=writes)

    def memset(self, ap, val, writes, eng="pool"):
        return self.fw.op(eng, lambda e: e.memset(ap, val), reads=[], writes=writes)


def bc(ap, shape):
    return ap.broadcast_to(list(shape))


VEC_SPEC = [("modb0", 48), ("modb1", 48), ("gmix0", 8), ("gmix1", 8), ("gffn0", 8), ("gffn1", 8),
            ("convw", 80), ("convb", 16), ("ssdg", 8), ("cfw", 248), ("cfb", 8), ("cflg", 8), ("cflb", 8),
            ("hglb0", 8), ("hglb1", 8)]
VOFF = {}
_o = 0
for _n, _c in VEC_SPEC:
    VOFF[_n] = _o
    _o += _c
NV = _o
ROW_SPEC = [("modb0", 6144), ("modb1", 6144), ("dtb", 32), ("alog", 32), ("ssdd", 16), ("rb", 16),
            ("fng", 1024), ("hgng", 128)]
ROFF = {}
_o = 0
for _n, _c in ROW_SPEC:
    ROFF[_n] = _o
    _o += _c
NR = _o
CONST_SPEC = [("ident", 128), ("triU", 128), ("triL", 128), ("negF", 128), ("negB", 128), ("ones", 128),
              ("mF64", 64), ("mB64", 64)]
COFF = {}
_o = 0
for _n, _c in CONST_SPEC:
    COFF[_n] = _o
    _o += _c
NCON = _o


def _col(v):
    v = np.asarray(v, np.float32).reshape(-1, 128)
    return np.ascontiguousarray(v.T)


def pack_shared(I):
    vec = np.zeros((128, NV), np.float32)

    def put(n, a):
        vec[:, VOFF[n]:VOFF[n] + a.shape[1]] = a
    put("modb0", _col(I["mod_b"][0])); put("modb1", _col(I["mod_b"][1]))
    put("gmix0", _col(I["norm_mix_g"][0])); put("gmix1", _col(I["norm_mix_g"][1]))
    put("gffn0", _col(I["norm_ffn_g"][0])); put("gffn1", _col(I["norm_ffn_g"][1]))
    cw = np.asarray(I["ssd_conv_w"][0], np.float32)
    put("convw", np.ascontiguousarray(cw.reshape(5, 16, 128).transpose(2, 1, 0)).reshape(128, 80))
    put("convb", _col(I["ssd_conv_b"][0]))
    put("ssdg", _col(I["ssd_norm_g"][0]))
    fw_ = np.asarray(I["cf_dw_w"][0], np.float32)
    put("cfw", np.ascontiguousarray(fw_.reshape(31, 8, 128).transpose(2, 1, 0)).reshape(128, 248))
    put("cfb", _col(I["cf_dw_b"][0])); put("cflg", _col(I["cf_ln_g"][0])); put("cflb", _col(I["cf_ln_b"][0]))
    put("hglb0", _col(I["hg_lb"][0])); put("hglb1", _col(I["hg_lb"][1]))
    row = np.zeros((1, NR), np.float32)

    def putr(n, a):
        a = np.asarray(a, np.float32).reshape(-1)
        row[0, ROFF[n]:ROFF[n] + a.size] = a
    putr("modb0", I["mod_b"][0]); putr("modb1", I["mod_b"][1]); putr("dtb", I["ssd_dt_bias"][0])
    putr("alog", I["ssd_a_log"][0]); putr("ssdd", I["ssd_d"][0]); putr("rb", I["router_b"])
    putr("fng", I["final_norm_g"]); putr("hgng", I["hg_norm_g"][0])
    con = np.zeros((128, NCON), np.float32)
    i = np.arange(128)
    k, l = i[:, None], i[None, :]

    def putc(n, a):
        con[:a.shape[0], COFF[n]:COFF[n] + a.shape[1]] = a
    putc("ident", (k == l).astype(np.float32))
    putc("triU", (k <= l).astype(np.float32))
    putc("triL", (k >= l).astype(np.float32))
    putc("negF", np.where(k <= l, 0.0, NEG).astype(np.float32))
    putc("negB", np.where(k >= l, 0.0, NEG).astype(np.float32))
    putc("ones", np.ones((128, 128), np.float32))
    putc("mF64", (k[:64] <= l[:, :64]).astype(np.float32))
    putc("mB64", (k[:64] >= l[:, :64]).astype(np.float32))
    return vec, row, con


class Prog:
    def __init__(self, dbg=()):
        self.dbg = set(dbg)
        nc = self.nc = bass.Bass("TRN2", target_bir_lowering=False)
        self.es = ExitStack()
        self.fw = FW(nc, self.es)
        self.k = KB(nc, self.fw)
        self.dr = {}

    def inp(self, name, shape, dtype=F32):
        t = self.nc.dram_tensor(name, list(shape), dtype, kind="ExternalInput")
        self.dr[name] = self.fw.dram(name, t)
        return self.dr[name]

    def scratch(self, name, shape, dtype=F32, out=False):
        kind = "ExternalOutput" if (out or name in self.dbg) else "Internal"
        t = self.nc.dram_tensor(name, list(shape), dtype, kind=kind)
        self.dr[name] = self.fw.dram(name, t)
        return self.dr[name]

    def declare_io(self):
        self.inp("x", [TL, D]); self.inp("ctx", [TC, D]); self.inp("cvec", [128, 8, 2])
        self.inp("vecs", [128, NV]); self.inp("rows", [1, NR]); self.inp("consts", [128, NCON])
        self.inp("mod_w", [2, D, 6 * D]); self.inp("router_w", [D, 16])
        self.inp("moe_w_gate", [2, 16, D, 512]); self.inp("moe_w_up", [2, 16, D, 512])
        self.inp("moe_w_down", [2, 16, 512, D])
        self.inp("ab_w_in", [D, 5152]); self.inp("ab_w_out", [2048, D])
        self.inp("hg_w_in", [D, 5120]); self.inp("hg_w_out", [D, D])

    def setup_consts(self):
        fw, k = self.fw, self.k
        self.con = fw.sbuf("con", [128, NCON], F32)
        fw.dma("sp", self.con[:], self.dr["consts"][:], self.dr["consts"], self.con)
        self.conb = fw.sbuf("conb", [128, NCON], BF16)
        k.cp(self.conb[:], self.con[:], [self.con], [self.conb])
        self.vec = fw.sbuf("vec", [128, NV], F32)
        fw.dma("act", self.vec[:], self.dr["vecs"][:], self.dr["vecs"], self.vec)

    def C(self, n, w=128, rows=128, b16=False):
        t = self.conb if b16 else self.con
        return t[0:rows, COFF[n]:COFF[n] + w]

    def V(self, n, j=0, w=1):
        return self.vec[:, VOFF[n] + j:VOFF[n] + j + w]

    def rowbc(self, n, w, off=0):
        return self.dr["rows"].t[0, ROFF[n] + off:ROFF[n] + off + w].partition_broadcast(128)

    fw_mod_double = True

    def phase_mod(self, l):
        self.fw.push()
        self.fw_mod_double = True
        st = self.mod_setup(l)
        for g in range(12):
            self.mod_group(st, g)
        self.mod_finish(st)
        self.fw.pop()

    def mod_setup(self, l):
        fw, k, dr = self.fw, self.k, self.dr
        modrow = self.scratch("modrow%d" % l, [2, 6 * D])
        modcol = self.scratch("modcol%d" % l, [128, 48, 2])
        cv = fw.sbuf("cv", [128, 8, 2], F32)
        fw.dma("sp", cv[:], dr["cvec"][:], dr["cvec"], cv)
        cs = fw.sbuf("cs", [128, 8, 2], BF16)
        k.act(cs[:], cv[:], AF.Silu, [cv], [cs])
        csb = fw.sbuf("csb", [128, 8, 128], BF16)
        k.cp(csb[:, :, 0:64], bc(cs[:, :, 0:1], [128, 8, 64]), [cs], [csb])
        k.cp(csb[:, :, 64:128], bc(cs[:, :, 1:2], [128, 8, 64]), [cs], [csb])
        nb_ = 2 if self.fw_mod_double else 1
        mb = [fw.sbuf("mb%d" % i, [128, 512], F32) for i in range(nb_)] * (3 - nb_)
        mcol = fw.sbuf("mcol", [128, 48, 2], F32)
        W = [fw.sbuf("modW%d" % i, [128, 8, 512], BF16) for i in range(nb_)] * (3 - nb_)
        rowb = [fw.sbuf("rowb%d" % i, [128, 512], F32) for i in range(nb_)] * (3 - nb_)
        wsrc = dr["mod_w"].t[l].rearrange("(k p) n -> p k n", p=128)
        return dict(l=l, modrow=modrow, modcol=modcol, cs=cs, csb=csb, mb=mb, mcol=mcol, W=W, rowb=rowb, wsrc=wsrc)

    def mod_group(self, st, g):
        fw, k, dr = self.fw, self.k, self.dr
        l, modrow, cs, csb, mcol = st["l"], st["modrow"], st["cs"], st["csb"], st["mcol"]
        w = st["W"][g % 2]
        fw.dma("pool", w[:], st["wsrc"][:, :, g * 512:(g + 1) * 512], dr["mod_w"], w)
        mb = st["mb"][g % 2]
        fw.dma("act", mb[:], self.rowbc("modb%d" % l, 512, off=g * 512), dr["rows"], mb)
        P = k.bank()
        for kk in range(8):
            k.mm(P[:, :], csb[:, kk, :], w[:, kk, :], kk == 0, kk == 7, [csb, w], P)
        rb = st["rowb"][g % 2]
        k.tt(rb[:], P[:, :], mb[:], ALU.add, [P, mb], [rb])
        fw.dma("sp", modrow.t[0:1, g * 512:(g + 1) * 512], rb[0:1, :], rb, modrow)
        fw.dma("act", modrow.t[1:2, g * 512:(g + 1) * 512], rb[64:65, :], rb, modrow)
        P2 = k.bank()
        for fc in range(4):
            for kk in range(8):
                k.mm(P2[:, fc * 2:fc * 2 + 2], w[:, kk, fc * 128:(fc + 1) * 128], cs[:, kk, :],
                     kk == 0, kk == 7, [cs, w], P2)
        k.tt(mcol[:, g * 4:(g + 1) * 4, :], P2[:, 0:8].rearrange("p (a b) -> p a b", b=2),
             bc(self.V("modb%d" % l, g * 4, 4).unsqueeze(2), [128, 4, 2]), ALU.add, [P2, self.vec], [mcol])

    def mod_finish(self, st):
        self.fw.dma("sp", st["modcol"].t[:], st["mcol"][:], st["mcol"], st["modcol"])


def make_inmaps(I, cores):
    vec, row, con = pack_shared(I)
    shared = {"vecs": vec, "rows": row, "consts": con}
    for n in ("mod_w", "router_w", "moe_w_gate", "moe_w_up", "moe_w_down", "hg_w_out"):
        shared[n] = np.ascontiguousarray(np.asarray(I[n], np.float32))
    shared["ab_w_in"] = np.ascontiguousarray(np.asarray(I["ab_w_in"][0], np.float32))
    shared["ab_w_out"] = np.ascontiguousarray(np.asarray(I["ab_w_out"][0], np.float32))
    shared["hg_w_in"] = np.ascontiguousarray(np.asarray(I["hg_w_in"][0], np.float32))
    shared["hg_w_out"] = np.ascontiguousarray(np.asarray(I["hg_w_out"][0], np.float32))
    maps = []
    for b in cores:
        m = dict(shared)
        m["x"] = np.ascontiguousarray(np.asarray(I["x"][b], np.float32))
        m["ctx"] = np.ascontiguousarray(np.asarray(I["ctx"][b], np.float32))
        cv = np.stack([_col(I["c"][b]), _col(I["c_ctx"])], axis=-1)
        m["cvec"] = np.ascontiguousarray(cv.astype(np.float32))
        maps.append(m)
    return maps


def _wsrc(dt, col0, ncols):
    return dt.rearrange("(k p) n -> p k n", p=128)[:, :, col0:col0 + ncols]


class Prog2(Prog):
    def load_AB(self, l, which, gname):
        fw, k = self.fw, self.k
        mc = fw.sbuf("mc", [128, 48, 2], F32)
        fw.dma("sp", mc[:], self.dr["modcol%d" % l].t[:], self.dr["modcol%d" % l], mc)
        AB = fw.sbuf("AB", [128, 2, 2, 8], F32)
        sh, sc = which
        for idx in range(2):
            k.ts(AB[:, idx, 0, :], mc[:, sc * 8:sc * 8 + 8, idx], 1.0, None, ALU.add, ALU.bypass, [mc], [AB])
            k.tt(AB[:, idx, 0, :], AB[:, idx, 0, :], self.V(gname, 0, 8), ALU.mult, [AB, self.vec], [AB])
            k.cp(AB[:, idx, 1, :], mc[:, sh * 8:sh * 8 + 8, idx], [mc], [AB])
        return AB

    def normmod_tile(self, X, AB, idx, hT, tok0, tmp):
        fw, k = self.fw, self.k
        junk, ss, xn, t3 = tmp
        k.act(junk[:], X[:], AF.Square, [X], [junk, ss], accum_out=ss[:, 0:1])
        k.act(ss[:, 1:2], ss[:, 0:1], AF.Sqrt, [ss], [ss], scale=1.0 / D, bias=self.epsb[:, 0:1])
        self.fw.op("dve", lambda e: e.reciprocal(ss[:, 2:3], ss[:, 1:2]), [ss], [ss])
        k.act(xn[:], X[:], AF.Copy, [X, ss], [xn], scale=ss[:, 2:3])
        P = k.bank()
        Pb = P[:, :].bitcast(BF16)
        for c in range(8):
            k.tr(Pb[:, c * 128:(c + 1) * 128], xn[:, c * 128:(c + 1) * 128], self.C("ident", b16=True),
                 [xn, self.conb], P, inc=(c == 7), first=(c == 0))
        Pv = Pb.rearrange("p (c t) -> p c t", t=128)
        k.tt(t3[:], Pv, bc(AB[:, idx, 0, :].unsqueeze(2), [128, 8, 128]), ALU.mult, [P, AB], [t3])
        k.tt(hT[:, :, tok0:tok0 + 128], t3[:], bc(AB[:, idx, 1, :].unsqueeze(2), [128, 8, 128]), ALU.add,
             [t3, AB], [hT])

    def norm_tmp(self):
        fw = self.fw
        return [(fw.sbuf("junk%d" % i, [128, D], BF16), fw.sbuf("ss%d" % i, [128, 4], F32), fw.sbuf("xn%d" % i, [128, D], BF16),
                 fw.sbuf("t3%d" % i, [128, 8, 128], F32)) for i in range(2)]

    def setup_consts(self):
        Prog.setup_consts(self)
        self.epsb = self.fw.sbuf("epsb", [128, 1], F32)
        self.k.memset(self.epsb[:], EPS, [self.epsb])

    def phase_normmod0(self, hT):
        fw, k, dr = self.fw, self.k, self.dr
        fw.push()
        AB = self.load_AB(0, (0, 1), "gmix0")
        tmp = self.norm_tmp()
        X = [fw.sbuf("X%d" % i, [128, D], F32) for i in range(2)]
        for i in range(NT):
            x = X[i % 2]
            if i < 16:
                fw.dma(k.q(), x[:], dr["x"].t[i * 128:(i + 1) * 128, :], dr["x"], x)
            else:
                fw.dma(k.q(), x[:], dr["ctx"].t[(i - 16) * 128:(i - 15) * 128, :], dr["ctx"], x)
            self.normmod_tile(x, AB, 0 if i < 16 else 1, hT, i * 128, tmp[i % 2])
        fw.pop()

    def phase_evenproj(self, hT):
        fw, k, dr = self.fw, self.k, self.dr
        win = dr["ab_w_in"]
        sz = self.scratch("sz_tok", [T, D])
        dtk = self.scratch("dt_tok", [T, 32])
        xtok = self.scratch("x_tok", [T, D], BF16)
        btok = self.scratch("b_tok", [T, 512], BF16)
        BT = self.scratch("BT", [4, 128, T], BF16)
        CT = self.scratch("CT", [4, 128, T], BF16)
        U = self.scratch("U_cf", [8, 128, T])
        fw.push()
        Wz = fw.sbuf("Wz", [128, 8, D], BF16)
        fw.dma("pool", Wz[:], _wsrc(win.t, 0, D), win, Wz)
        Wdt = fw.sbuf("Wdt", [128, 8, 32], BF16)
        fw.dma("pool", Wdt[:], _wsrc(win.t, 3072, 32), win, Wdt)
        dtb = fw.sbuf("dtb", [128, 32], F32)
        fw.dma("sp", dtb[:], self.rowbc("dtb", 32), dr["rows"], dtb)
        zs = [fw.sbuf("zs%d" % i, [128, D], F32) for i in range(2)]
        dts = [fw.sbuf("dts%d" % i, [128, 32], F32) for i in range(2)]
        for i in range(NT):
            z = zs[i % 2]
            for h in range(2):
                P = k.bank()
                for kk in range(8):
                    k.mm(P[:, :], hT[:, kk, i * 128:(i + 1) * 128], Wz[:, kk, h * 512:(h + 1) * 512],
                         kk == 0, kk == 7, [hT, Wz], P)
                k.act(z[:, h * 512:(h + 1) * 512], P[:, :], AF.Silu, [P], [z])
            fw.dma(k.q(), sz.t[i * 128:(i + 1) * 128, :], z[:], z, sz)
            P = k.bank()
            for kk in range(8):
                k.mm(P[:, 0:32], hT[:, kk, i * 128:(i + 1) * 128], Wdt[:, kk, :], kk == 0, kk == 7, [hT, Wdt], P)
            d = dts[i % 2]
            k.tt(d[:], P[:, 0:32], dtb[:], ALU.add, [P, dtb], [d])
            k.act(d[:], d[:], AF.Exp, [d], [d])
            k.act(d[:], d[:], AF.Ln, [d], [d], bias=1.0)
            fw.dma(k.q(), dtk.t[i * 128:(i + 1) * 128, :], d[:], d, dtk)
        Wg = [fw.sbuf("Wg%d" % i, [128, 8, 512], BF16) for i in range(2)]
        xinL = [fw.sbuf("xinL%d" % i, [128, TL + 4], F32) for i in range(2)]
        xinC = [fw.sbuf("xinC%d" % i, [128, TC + 4], F32) for i in range(2)]
        for b_ in xinL:
            k.memset(b_[:, 0:2], 0.0, [b_]); k.memset(b_[:, TL + 2:TL + 4], 0.0, [b_])
        for b_ in xinC:
            k.memset(b_[:, 0:2], 0.0, [b_]); k.memset(b_[:, TC + 2:TC + 4], 0.0, [b_])
        acc = [fw.sbuf("acc%d" % i, [128, T], F32) for i in range(2)]
        xc = [fw.sbuf("xc%d" % i, [128, T], BF16) for i in range(2)]
        stg = [fw.sbuf("stg%d" % i, [128, 4, 128], BF16) for i in range(2)]
        blocks = [(0, 512), (512, 512), (1024, 512), (1536, 512), (2048, 256)]
        si = 0
        for cc in range(16):
            if cc % 4 == 0:
                w = Wg[(cc // 4) % 2]
                fw.dma("pool", w[:], _wsrc(win.t, 1024 + cc * 128, 512), win, w)
            xl, xcx, a, o = xinL[cc % 2], xinC[cc % 2], acc[cc % 2], xc[cc % 2]
            for (t0, n) in blocks:
                P = k.bank()
                for kk in range(8):
                    k.mm(P[:, 0:n], w[:, kk, (cc % 4) * 128:(cc % 4 + 1) * 128], hT[:, kk, t0:t0 + n],
                         kk == 0, kk == 7, [hT, w], P)
                if t0 < TL:
                    k.act(xl[:, 2 + t0:2 + t0 + n], P[:, 0:n], AF.Copy, [P], [xl])
                else:
                    k.act(xcx[:, 2:2 + n], P[:, 0:n], AF.Copy, [P], [xcx])
            for (xi, lo, n) in ((xl, 0, TL), (xcx, TL, TC)):
                k.ts(a[:, lo:lo + n], xi[:, 0:n], self.V("convw", cc * 5), self.V("convb", cc), ALU.mult, ALU.add,
                     [xi, self.vec], [a])
                for kt in range(1, 5):
                    k.stt(a[:, lo:lo + n], xi[:, kt:kt + n], self.V("convw", cc * 5 + kt), a[:, lo:lo + n],
                          ALU.mult, ALU.add, [xi, self.vec, a], [a])
            k.act(o[:], a[:], AF.Silu, [a], [o])
            if cc >= 8:
                dst = BT if cc < 12 else CT
                fw.dma(k.q(), dst.t[cc % 4], o[:], o, dst)
            if cc < 12:
                for t4 in range(0, NT, 4):
                    nt = min(4, NT - t4)
                    P = k.bank()
                    Pb = P[:, :].bitcast(BF16)
                    for j in range(nt):
                        k.tr(Pb[:, j * 128:(j + 1) * 128], o[:, (t4 + j) * 128:(t4 + j + 1) * 128],
                             self.C("ident", b16=True), [o, self.conb], P, inc=(j == nt - 1), first=(j == 0))
                    s = stg[si % 2]; si += 1
                    k.cp(s[:, 0:nt, :], Pb[:, 0:nt * 128].rearrange("p (a c) -> p a c", c=128), [P], [s])
                    if cc < 8:
                        dd = xtok.t[t4 * 128:(t4 + nt) * 128, cc * 128:(cc + 1) * 128]
                        dbuf = xtok
                    else:
                        dd = btok.t[t4 * 128:(t4 + nt) * 128, (cc - 8) * 128:(cc - 7) * 128]
                        dbuf = btok
                    fw.dma(k.q(), dd.rearrange("(a p) c -> p a c", p=128), s[:, 0:nt, :], s, dbuf)
        ub = [fw.sbuf("ub%d" % i, [128, T], F32) for i in range(2)]
        sg = [fw.sbuf("sg%d" % i, [128, 512], F32) for i in range(2)]
        gi = 0
        for c in range(8):
            if c % 4 == 0:
                wv = Wg[0]; wg_ = Wg[1]
                fw.dma("pool", wv[:], _wsrc(win.t, 3104 + c * 128, 512), win, wv)
                fw.dma("pool", wg_[:], _wsrc(win.t, 4128 + c * 128, 512), win, wg_)
            u = ub[c % 2]
            for (t0, n) in blocks:
                Pv = k.bank(); Pg = k.bank()
                for kk in range(8):
                    k.mm(Pv[:, 0:n], wv[:, kk, (c % 4) * 128:(c % 4 + 1) * 128], hT[:, kk, t0:t0 + n],
                         kk == 0, kk == 7, [hT, wv], Pv)
                for kk in range(8):
                    k.mm(Pg[:, 0:n], wg_[:, kk, (c % 4) * 128:(c % 4 + 1) * 128], hT[:, kk, t0:t0 + n],
                         kk == 0, kk == 7, [hT, wg_], Pg)
                s = sg[gi % 2]; gi += 1
                k.act(s[:, 0:n], Pg[:, 0:n], AF.Sigmoid, [Pg], [s])
                k.tt(u[:, t0:t0 + n], Pv[:, 0:n], s[:, 0:n], ALU.mult, [Pv, s], [u])
            fw.dma(k.q(), U.t[c], u[:], u, U)
        fw.pop()


class Prog3(Prog2):
    def phase_ssd(self):
        fw, k, dr = self.fw, self.k, self.dr
        xtok, btok, BT, CT, dtk, sz = (dr[n] for n in ("x_tok", "b_tok", "BT", "CT", "dt_tok", "sz_tok"))
        yf = self.scratch("yf_tok", [T, D])
        oT = self.scratch("oT", [16, 128, T], BF16)
        fw.push()
        al = fw.sbuf("al", [128, 32], F32)
        fw.dma("sp", al[:], self.rowbc("alog", 32), dr["rows"], al)
        aneg = fw.sbuf("aneg", [128, 32], F32)
        k.act(aneg[:], al[:], AF.Exp, [al], [aneg])
        k.ts(aneg[:], aneg[:], -1.0, None, ALU.mult, ALU.bypass, [aneg], [aneg])
        dsk = fw.sbuf("dsk", [128, 16], F32)
        fw.dma("act", dsk[:], self.rowbc("ssdd", 16), dr["rows"], dsk)
        S32 = fw.sbuf("S32", [128, D], F32)
        Sbf = fw.sbuf("Sbf", [128, D], BF16)
        Xt = [fw.sbuf("Xt%d" % i, [128, D], BF16) for i in range(2)]
        Bt = [fw.sbuf("Bt%d" % i, [128, 512], BF16) for i in range(2)]
        BTc = [fw.sbuf("BTc%d" % i, [128, 4, 128], BF16) for i in range(2)]
        CTc = [fw.sbuf("CTc%d" % i, [128, 4, 128], BF16) for i in range(2)]
        dtc = [fw.sbuf("dtc%d" % i, [128, 16], F32) for i in range(2)]
        WK = []
        for i in range(2):
            WK.append(dict(
                la=fw.sbuf("la%d" % i, [128, 16], F32), cum=fw.sbuf("cum%d" % i, [128, 16], F32),
                rhs2=fw.sbuf("rhs2%d" % i, [128, 16, 128], F32), Dm=fw.sbuf("Dm%d" % i, [128, 16, 128], F32),
                Eb=fw.sbuf("Eb%d" % i, [128, 16, 128], BF16), cbT=fw.sbuf("cbT%d" % i, [128, 4, 128], BF16),
                M=fw.sbuf("M%d" % i, [128, 16, 128], BF16), eR=fw.sbuf("eR%d" % i, [128, 16, 128], BF16),
                ECT=fw.sbuf("ECT%d" % i, [128, 16, 128], BF16), Rl=fw.sbuf("Rl%d" % i, [128, 16], F32),
                t16=fw.sbuf("t16%d" % i, [128, 16], F32), te=fw.sbuf("te%d" % i, [128, 16], F32),
                cd=fw.sbuf("cd%d" % i, [128, 16], F32), w2=fw.sbuf("w2%d" % i, [128, 16], F32),
                xdt=fw.sbuf("xdt%d" % i, [128, 16, 64], BF16), xdte=fw.sbuf("xdte%d" % i, [128, 16, 64], BF16)))
        yst = [fw.sbuf("yst%d" % i, [128, D], F32) for i in range(2)]
        yft = fw.sbuf("yft", [128, D], F32)
        szt = fw.sbuf("szt", [128, D], F32)
        xd = fw.sbuf("xd", [128, 16, 64], F32)
        junk = fw.sbuf("junk", [128, D], BF16)
        ss = fw.sbuf("ss", [128, 4], F32)
        yn = fw.sbuf("yn", [128, D], BF16)
        ost = [fw.sbuf("ost%d" % i, [128, 8, 128], BF16) for i in range(2)]
        identb = self.C("ident", b16=True)
        for d in range(2):
            tri = self.C("triU" if d == 0 else "triL")
            neg = self.C("negF" if d == 0 else "negB")
            last = 127 if d == 0 else 0
            k.memset(S32[:], 0.0, [S32])
            k.memset(Sbf[:], 0.0, [Sbf])
            order = [16, 17] + list(range(16)) if d == 0 else [17, 16] + list(range(15, -1, -1))
            def head(ci, i):
                t0 = i * 128
                X, B_, BTt, CTt, dt_ = Xt[ci % 2], Bt[ci % 2], BTc[ci % 2], CTc[ci % 2], dtc[ci % 2]
                wk = WK[ci % 2]
                la, cum, rhs2, Dm, Eb, cbT, M, eR, ECT, Rl, t16, te, cd, w2, xdt, xdte = (wk[n_] for n_ in (
                    "la", "cum", "rhs2", "Dm", "Eb", "cbT", "M", "eR", "ECT", "Rl", "t16", "te", "cd", "w2", "xdt", "xdte"))
                fw.dma("sp", X[:], xtok.t[t0:t0 + 128, :], xtok, X)
                fw.dma("act", B_[:], btok.t[t0:t0 + 128, :], btok, B_)
                fw.dma("sp", BTt[:], BT.t[:, :, t0:t0 + 128].rearrange("g n t -> n g t"), BT, BTt)
                fw.dma("act", CTt[:], CT.t[:, :, t0:t0 + 128].rearrange("g n t -> n g t"), CT, CTt)
                fw.dma("sp", dt_[:], dtk.t[t0:t0 + 128, d * 16:(d + 1) * 16], dtk, dt_)
                k.tt(la[:], dt_[:], aneg[:, d * 16:(d + 1) * 16], ALU.mult, [dt_, aneg], [la])
                Pc = k.bank()
                k.mm(Pc[:, 0:16], tri, la[:], True, True, [self.con, la], Pc)
                k.act(cum[:], Pc[:, 0:16], AF.Copy, [Pc], [cum])
                k.tt(rhs2[:], bc(tri.unsqueeze(1), [128, 16, 128]), bc(la[:].unsqueeze(2), [128, 16, 128]), ALU.mult,
                     [self.con, la], [rhs2])
                Rb = []
                for g in range(4):
                    P = k.bank()
                    k.mm(P[:, :], self.C("ones"), rhs2[:, 4 * g:4 * g + 4, :].rearrange("p a b -> p (a b)"), True, True,
                         [self.con, rhs2], P)
                    Rb.append(P)
                for h in range(16):
                    P = Rb[h // 4]
                    k.stt(Dm[:, h, :], P[:, (h % 4) * 128:(h % 4 + 1) * 128], cum[:, h:h + 1], neg, ALU.subtract, ALU.add,
                          [P, cum, self.con], [Dm])
                for g in range(4):
                    P = Rb[g]
                    k.act(eR[:, 4 * g:4 * g + 4, :].rearrange("p a b -> p (a b)"), P[:, :], AF.Exp, [P], [eR])
                    k.cp(Rl[:, 4 * g:4 * g + 4], P[:, :].rearrange("p (a b) -> p a b", b=128)[:, :, last], [P], [Rl])
                k.act(Eb[:].rearrange("p a b -> p (a b)"), Dm[:].rearrange("p a b -> p (a b)"), AF.Exp, [Dm], [Eb])
                Pcb = k.bank()
                for g in range(4):
                    k.mm(Pcb[:, g * 128:(g + 1) * 128], BTt[:, g, :], CTt[:, g, :], True, True, [BTt, CTt], Pcb)
                k.act(cbT[:].rearrange("p a b -> p (a b)"), Pcb[:, :], AF.Copy, [Pcb], [cbT])
                for g in range(4):
                    k.tt(M[:, 4 * g:4 * g + 4, :], Eb[:, 4 * g:4 * g + 4, :], bc(cbT[:, g:g + 1, :], [128, 4, 128]), ALU.mult,
                         [Eb, cbT], [M])
                    k.tt(ECT[:, 4 * g:4 * g + 4, :], eR[:, 4 * g:4 * g + 4, :], bc(CTt[:, g:g + 1, :], [128, 4, 128]), ALU.mult,
                         [eR, CTt], [ECT])
                k.tt(t16[:], Rl[:], cum[:], ALU.subtract, [Rl, cum], [t16])
                k.act(te[:], t16[:], AF.Exp, [t16], [te])
                k.act(cd[:], Rl[:], AF.Exp, [Rl], [cd])
                k.tt(w2[:], dt_[:], te[:], ALU.mult, [dt_, te], [w2])
                Xv = X[:].rearrange("p (h e) -> p h e", e=64)
                k.tt(xdt[:], Xv, bc(dt_[:].unsqueeze(2), [128, 16, 64]), ALU.mult, [X, dt_], [xdt])
                k.tt(xdte[:], Xv, bc(w2[:].unsqueeze(2), [128, 16, 64]), ALU.mult, [X, w2], [xdte])

            def tail(ci, i):
                t0 = i * 128
                X, B_, dt_ = Xt[ci % 2], Bt[ci % 2], dtc[ci % 2]
                wk = WK[ci % 2]
                M, ECT, cd, xdt, xdte = (wk[n_] for n_ in ("M", "ECT", "cd", "xdt", "xdte"))
                Xv = X[:].rearrange("p (h e) -> p h e", e=64)
                Y = [k.bank(), k.bank()]
                for h in range(16):
                    P = Y[h // 8]
                    cs_ = slice((h % 8) * 64, (h % 8 + 1) * 64)
                    k.mm(P[:, cs_], M[:, h, :], xdt[:, h, :], True, False, [M, xdt], P)
                    k.mm(P[:, cs_], ECT[:, h, :], Sbf[:, h * 64:(h + 1) * 64], False, True, [ECT, Sbf], P)
                CS = [k.bank(), k.bank()]
                for g in range(4):
                    P = CS[g // 2]
                    k.mm(P[:, (g % 2) * 256:(g % 2 + 1) * 256], B_[:, g * 128:(g + 1) * 128],
                         xdte[:, 4 * g:4 * g + 4, :].rearrange("p a b -> p (a b)"), True, True, [B_, xdte], P)
                Sv = S32[:].rearrange("p (h e) -> p h e", e=64)
                k.tt(Sv, Sv, bc(cd[:].unsqueeze(2), [128, 16, 64]), ALU.mult, [S32, cd], [S32])
                for j in range(2):
                    k.tt(S32[:, j * 512:(j + 1) * 512], S32[:, j * 512:(j + 1) * 512], CS[j][:, :], ALU.add, [S32, CS[j]], [S32])
                k.cp(Sbf[:], S32[:], [S32], [Sbf])
                if d == 0:
                    ys = yst[ci % 2]
                    for j in range(2):
                        k.act(ys[:, j * 512:(j + 1) * 512], Y[j][:, :], AF.Copy, [Y[j]], [ys])
                    fw.dma("act", yf.t[t0:t0 + 128, :], ys[:], ys, yf)
                else:
                    fw.dma("sp", yft[:], yf.t[t0:t0 + 128, :], yf, yft)
                    fw.dma("act", szt[:], sz.t[t0:t0 + 128, :], sz, szt)
                    ys = yst[ci % 2]
                    for j in range(2):
                        k.tt(ys[:, j * 512:(j + 1) * 512], Y[j][:, :], yft[:, j * 512:(j + 1) * 512], ALU.add, [Y[j], yft], [ys])
                    k.tt(xd[:], Xv, bc(dsk[:].unsqueeze(2), [128, 16, 64]), ALU.mult, [X, dsk], [xd])
                    k.tt(ys[:], ys[:], xd[:].rearrange("p a b -> p (a b)"), ALU.add, [ys, xd], [ys])
                    k.tt(ys[:], ys[:], szt[:], ALU.mult, [ys, szt], [ys])
                    k.act(junk[:], ys[:], AF.Square, [ys], [junk, ss], accum_out=ss[:, 0:1])
                    k.act(ss[:, 1:2], ss[:, 0:1], AF.Sqrt, [ss], [ss], scale=1.0 / D, bias=self.epsb[:, 0:1])
                    fw.op("dve", lambda e: e.reciprocal(ss[:, 2:3], ss[:, 1:2]), [ss], [ss])
                    k.act(yn[:], ys[:], AF.Copy, [ys, ss], [yn], scale=ss[:, 2:3])
                    P = k.bank()
                    Pb = P[:, :].bitcast(BF16)
                    for c in range(8):
                        k.tr(Pb[:, c * 128:(c + 1) * 128], yn[:, c * 128:(c + 1) * 128], identb, [yn, self.conb], P,
                             inc=(c == 7), first=(c == 0))
                    os_ = ost[ci % 2]
                    k.tt(os_[:], Pb.rearrange("p (c t) -> p c t", t=128), bc(self.V("ssdg", 0, 8).unsqueeze(2), [128, 8, 128]),
                         ALU.mult, [P, self.vec], [os_])
                    fw.dma("sp", oT.t[0:8, :, t0:t0 + 128].rearrange("c p t -> p c t"), os_[:], os_, oT)

            head(0, order[0])
            for ci, i in enumerate(order):
                if ci + 1 < len(order):
                    head(ci + 1, order[ci + 1])
                tail(ci, i)
            fw.barrier()
        fw.pop()

    def phase_conformer(self):
        fw, k, dr = self.fw, self.k, self.dr
        U, oT = dr["U_cf"], dr["oT"]
        fw.push()
        cv = [fw.sbuf("cv%d" % c, [128, T], F32) for c in range(8)]
        ub = [fw.sbuf("cu%d" % i, [128, T], F32) for i in range(2)]
        upL = [fw.sbuf("upL%d" % i, [128, 32, 94], BF16) for i in range(2)]
        upC = [fw.sbuf("upC%d" % i, [128, TC + 30], BF16) for i in range(2)]
        dg = [fw.sbuf("dg%d" % i, [128, 31, 128], BF16) for i in range(2)]
        for b_ in upL + upC:
            k.memset(b_[:], 0.0, [b_])
        identb = self.C("ident", b16=True)
        for c in range(8):
            u = ub[c % 2]; pl_, pc_, dgc = upL[c % 2], upC[c % 2], dg[c % 2]
            fw.dma(k.q(), u[:], U.t[c], U, u)
            k.cp(pl_[:, :, 15:79], u[:, 0:TL].rearrange("p (r w) -> p r w", w=64), [u], [pl_])
            k.act(pc_[:, 15:15 + TC], u[:, TL:T], AF.Copy, [u], [pc_])
            k.tt(dgc[:], bc(identb.unsqueeze(1), [128, 31, 128]),
                 bc(self.vec[:, VOFF["cfw"] + c * 31:VOFF["cfw"] + (c + 1) * 31].unsqueeze(2), [128, 31, 128]),
                 ALU.mult, [self.conb, self.vec], [dgc])
            for b in range(5):
                P = k.bank()
                n = 512 if b < 4 else TC
                for j in range(31):
                    rhs = pl_[:, 8 * b:8 * b + 8, j:j + 64] if b < 4 else pc_[:, j:j + TC]
                    k.mm(P[:, 0:n], dgc[:, j, :], rhs, j == 0, j == 30, [dgc, pl_ if b < 4 else pc_], P)
                k.act(cv[c][:, b * 512:b * 512 + n], P[:, 0:n], AF.Identity, [P, self.vec], [cv[c]], bias=self.V("cfb", c))
        sq = [fw.sbuf("sq%d" % i, [128, 512], F32) for i in range(2)]
        mean = fw.sbuf("mean", [128, 512], F32)
        var = fw.sbuf("var", [128, 512], F32)
        tmpb = [fw.sbuf("ct%d" % i, [128, 512], F32) for i in range(2)]
        ob = [fw.sbuf("cob%d" % i, [128, 512], BF16) for i in range(2)]
        ones = self.C("ones")
        qi = 0
        for (t0, n) in [(0, 512), (512, 512), (1024, 512), (1536, 512), (2048, 256)]:
            P1 = k.bank(); P2 = k.bank()
            for c in range(8):
                k.mm(P1[:, 0:n], ones, cv[c][:, t0:t0 + n], c == 0, c == 7, [self.con, cv[c]], P1)
            for c in range(8):
                s = sq[c % 2]
                k.act(s[:, 0:n], cv[c][:, t0:t0 + n], AF.Square, [cv[c]], [s])
                k.mm(P2[:, 0:n], ones, s[:, 0:n], c == 0, c == 7, [self.con, s], P2, inc=True)
            k.ts(mean[:, 0:n], P1[:, 0:n], 1.0 / D, None, ALU.mult, ALU.bypass, [P1], [mean])
            k.tt(var[:, 0:n], mean[:, 0:n], mean[:, 0:n], ALU.mult, [mean], [var])
            k.stt(var[:, 0:n], P2[:, 0:n], 1.0 / D, var[:, 0:n], ALU.mult, ALU.subtract, [P2, var], [var])
            k.act(var[:, 0:n], var[:, 0:n], AF.Sqrt, [var], [var], bias=self.epsb[:, 0:1])
            fw.op("dve", lambda e: e.reciprocal(var[:, 0:n], var[:, 0:n]), [var], [var])
            for c in range(8):
                tb = tmpb[c % 2]; o = ob[c % 2]
                k.tt(tb[:, 0:n], cv[c][:, t0:t0 + n], mean[:, 0:n], ALU.subtract, [cv[c], mean], [tb])
                k.tt(tb[:, 0:n], tb[:, 0:n], var[:, 0:n], ALU.mult, [tb, var], [tb])
                k.act(o[:, 0:n], tb[:, 0:n], AF.Silu, [tb, self.vec], [o], scale=self.V("cflg", c), bias=self.V("cflb", c))
                fw.dma(k.q(), oT.t[8 + c, :, t0:t0 + n], o[:, 0:n], o, oT)
        fw.pop()


class Prog4(Prog3):
    def phase_outproj(self, l, oT, nk, wname, xsrc, hT2, xmid_name, tiles):
        fw, k, dr = self.fw, self.k, self.dr
        xmid = self.scratch(xmid_name, [T, D])
        fw.push()
        W = fw.sbuf("Wout", [128, nk, D], BF16)
        fw.dma("pool", W[:], _wsrc(dr[wname].t, 0, D), dr[wname], W)
        m2 = fw.sbuf("m2", [128, 2, D], F32)
        mr = dr["modrow%d" % l]
        for idx in range(2):
            fw.dma(k.q(), m2[:, idx, :], mr.t[idx, 2 * D:3 * D].partition_broadcast(128), mr, m2)
        AB = self.load_AB(l, (3, 4), "gffn%d" % l)
        tmp = self.norm_tmp()
        ot = [fw.sbuf("ot%d" % i, [128, nk, 128], BF16) for i in range(2)]
        X = [fw.sbuf("Xo%d" % i, [128, D], F32) for i in range(2)]
        for n_, i in enumerate(tiles):
            o = ot[n_ % 2]; x = X[n_ % 2]
            idx = 0 if i < 16 else 1
            fw.dma("sp", o[:], oT.t[:, :, i * 128:(i + 1) * 128].rearrange("c p t -> p c t"), oT, o)
            for (p0, p1, sb, ap) in xsrc(i):
                fw.dma("act", x[p0:p1, :], ap, sb, x)
            for h in range(2):
                P = k.bank()
                for kk in range(nk):
                    k.mm(P[:, :], o[:, kk, :], W[:, kk, h * 512:(h + 1) * 512], kk == 0, kk == nk - 1, [o, W], P)
                hs = slice(h * 512, (h + 1) * 512)
                tq = tmp[n_ % 2]
                k.tt(tq[3][:].rearrange("p a b -> p (a b)")[:, hs], P[:, :], m2[:, idx, hs], ALU.mult, [P, m2], [tq[3]])
                k.tt(x[:, hs], x[:, hs], tq[3][:].rearrange("p a b -> p (a b)")[:, hs], ALU.add, [x, tq[3]], [x])
            fw.dma("sp", xmid.t[i * 128:(i + 1) * 128, :], x[:], x, xmid)
            self.normmod_tile(x, AB, idx, hT2, i * 128, tmp[n_ % 2])
        fw.pop()

    def phase_moe(self, l, hT2, xmid_name, xout, tiles, final=False, hook=None):
        fw, k, dr = self.fw, self.k, self.dr
        xmid = dr[xmid_name]
        nt = len(tiles)
        fw.push()
        Wr = fw.sbuf("Wr", [128, 8, 16], BF16)
        fw.dma("pool", Wr[:], _wsrc(dr["router_w"].t, 0, 16), dr["router_w"], Wr)
        rb = fw.sbuf("rb", [128, 16], F32)
        fw.dma("sp", rb[:], self.rowbc("rb", 16), dr["rows"], rb)
        sc = fw.sbuf("sc", [128, NT, 16], F32)
        sel = fw.sbuf("sel", [128, NT, 16], F32)
        for j, i in enumerate(tiles):
            P = k.bank()
            for kk in range(8):
                k.mm(P[:, 0:16], hT2[:, kk, i * 128:(i + 1) * 128], Wr[:, kk, :], kk == 0, kk == 7, [hT2, Wr], P)
            k.act(sc[:, j, :], P[:, 0:16], AF.Sigmoid, [P], [sc])
        S3 = lambda b_: b_[:, 0:nt, :]
        S4 = lambda b_: b_[:, 0:nt, :].rearrange("p t (g e) -> p t g e", e=4)
        k.tt(S3(sel), S3(sc), bc(rb[:].unsqueeze(1), [128, nt, 16]), ALU.add, [sc, rb], [sel])
        m1 = fw.sbuf("m1", [128, NT, 4], F32)
        m2_ = fw.sbuf("m2_", [128, NT, 4], F32)
        eq = fw.sbuf("eq", [128, NT, 16], F32)
        gs = fw.sbuf("gs", [128, NT, 4], F32)
        gm = fw.sbuf("gm", [128, NT, 1], F32)
        comb = fw.sbuf("comb", [128, NT, 16], F32)
        den = fw.sbuf("den", [128, NT, 1], F32)
        M3 = lambda b_: b_[:, 0:nt, :]
        fw.op("dve", lambda e: e.tensor_reduce(M3(m1), S4(sel), AX.X, ALU.max), [sel], [m1])
        k.tt(S4(eq), S4(sel), bc(M3(m1).unsqueeze(3), [128, nt, 4, 4]), ALU.is_equal, [sel, m1], [eq])
        k.stt(S3(eq), S3(eq), -1e9, S3(sel), ALU.mult, ALU.add, [eq, sel], [eq])
        fw.op("dve", lambda e: e.tensor_reduce(M3(m2_), S4(eq), AX.X, ALU.max), [eq], [m2_])
        k.tt(M3(gs), M3(m1), M3(m2_), ALU.add, [m1, m2_], [gs])
        fw.op("dve", lambda e: e.tensor_reduce(M3(gm), M3(gs), AX.X, ALU.max), [gs], [gm])
        k.tt(M3(gs), M3(gs), bc(M3(gm), [128, nt, 4]), ALU.is_equal, [gs, gm], [gs])
        k.tt(S4(eq), S4(sel), bc(M3(m2_).unsqueeze(3), [128, nt, 4, 4]), ALU.is_ge, [sel, m2_], [eq])
        k.tt(S4(eq), S4(eq), bc(M3(gs).unsqueeze(3), [128, nt, 4, 4]), ALU.mult, [eq, gs], [eq])
        k.tt(S3(comb), S3(eq), S3(sc), ALU.mult, [eq, sc], [comb])
        fw.op("dve", lambda e: e.tensor_reduce(M3(den), S3(comb), AX.X, ALU.add), [comb], [den])
        fw.op("dve", lambda e: e.reciprocal(M3(den), M3(den)), [den], [den])
        k.tt(S3(comb), S3(comb), bc(M3(den), [128, nt, 16]), ALU.mult, [comb, den], [comb])
        if "comb%d" % l in self.dbg:
            cdb = self.scratch("comb%d" % l, [128, NT, 16])
            fw.dma("sp", cdb.t[:], comb[:], comb, cdb)
        acc = fw.sbuf("acc", [128, NT, D], F32)
        k.memset(acc[:, 0:nt // 2, :], 0.0, [acc], eng="pool")
        k.memset(acc[:, nt // 2:nt, :], 0.0, [acc], eng="dve")
        Wg = [fw.sbuf("Wg%d" % i, [128, 8, 512], BF16) for i in range(2)]
        Wu = [fw.sbuf("Wu%d" % i, [128, 8, 512], BF16) for i in range(2)]
        Wd = [fw.sbuf("Wd%d" % i, [128, 4, D], BF16) for i in range(2)]
        sg = [fw.sbuf("sg%d" % i, [128, 512], F32) for i in range(2)]
        aT = [fw.sbuf("aT%d" % i, [128, 4, 512], BF16) for i in range(2)]
        blocks = [tiles[j:j + 4] for j in range(0, nt, 4)]
        si = 0
        if hook:
            fw.push()
            self.fw_mod_double = False
        hst = hook[0]() if hook else None
        for e_ in range(16):
            if hook and e_ >= 2 and e_ - 2 < 12:
                hook[1](hst, e_ - 2)
            wg, wu, wd = Wg[e_ % 2], Wu[e_ % 2], Wd[e_ % 2]
            fw.dma("pool", wg[:], _wsrc(dr["moe_w_gate"].t[l, e_], 0, 512), dr["moe_w_gate"], wg)
            fw.dma("pool", wu[:], _wsrc(dr["moe_w_up"].t[l, e_], 0, 512), dr["moe_w_up"], wu)
            fw.dma("pool", wd[:], _wsrc(dr["moe_w_down"].t[l, e_], 0, D), dr["moe_w_down"], wd)
            for bi, blk in enumerate(blocks):
                t0 = blk[0] * 128
                n = len(blk) * 128
                a = aT[bi % 2]
                for fc in range(4):
                    Pg = k.bank(); Pu = k.bank()
                    for kk in range(8):
                        k.mm(Pg[:, 0:n], wg[:, kk, fc * 128:(fc + 1) * 128], hT2[:, kk, t0:t0 + n], kk == 0, kk == 7, [wg, hT2], Pg)
                    for kk in range(8):
                        k.mm(Pu[:, 0:n], wu[:, kk, fc * 128:(fc + 1) * 128], hT2[:, kk, t0:t0 + n], kk == 0, kk == 7, [wu, hT2], Pu)
                    s = sg[si % 2]; si += 1
                    k.act(s[:, 0:n], Pg[:, 0:n], AF.Silu, [Pg], [s])
                    k.tt(a[:, fc, 0:n], Pu[:, 0:n], s[:, 0:n], ALU.mult, [Pu, s], [a])
                for jt, i in enumerate(blk):
                    j = tiles.index(i)
                    for h in range(2):
                        P = k.bank()
                        for fc in range(4):
                            k.mm(P[:, :], a[:, fc, jt * 128:(jt + 1) * 128], wd[:, fc, h * 512:(h + 1) * 512], fc == 0, fc == 3, [a, wd], P)
                        k.stt(acc[:, j, h * 512:(h + 1) * 512], P[:, :], comb[:, j, e_:e_ + 1], acc[:, j, h * 512:(h + 1) * 512],
                              ALU.mult, ALU.add, [P, comb, acc], [acc])
        if hook:
            hook[2](hst)
            fw.pop()
        m5 = fw.sbuf("m5", [128, 2, D], F32)
        mr = dr["modrow%d" % l]
        for idx in range(2):
            fw.dma(k.q(), m5[:, idx, :], mr.t[idx, 5 * D:6 * D].partition_broadcast(128), mr, m5)
        X = [fw.sbuf("Xm%d" % i, [128, D], F32) for i in range(2)]
        if final:
            fg = fw.sbuf("fg", [128, D], F32)
            fw.dma("sp", fg[:], self.rowbc("fng", D), dr["rows"], fg)
            junk = fw.sbuf("junkf", [128, D], BF16)
            ss = fw.sbuf("ssf", [128, 4], F32)
        for j, i in enumerate(tiles):
            x = X[j % 2]
            idx = 0 if i < 16 else 1
            fw.dma("sp", x[:], xmid.t[i * 128:(i + 1) * 128, :], xmid, x)
            k.tt(acc[:, j, :], acc[:, j, :], m5[:, idx, :], ALU.mult, [acc, m5], [acc])
            k.tt(x[:], x[:], acc[:, j, :], ALU.add, [x, acc], [x])
            if final:
                k.act(junk[:], x[:], AF.Square, [x], [junk, ss], accum_out=ss[:, 0:1])
                k.act(ss[:, 1:2], ss[:, 0:1], AF.Sqrt, [ss], [ss], scale=1.0 / D, bias=self.epsb[:, 0:1])
                fw.op("dve", lambda e: e.reciprocal(ss[:, 2:3], ss[:, 1:2]), [ss], [ss])
                k.stt(x[:], x[:], ss[:, 2:3], fg[:], ALU.mult, ALU.mult, [x, ss, fg], [x])
                for (p0, p1, sb, ap) in self.cm_rows(xout, i):
                    fw.dma("act", ap, x[p0:p1, :], x, xout)
            else:
                fw.dma("act", xout.t[i * 128:(i + 1) * 128, :], x[:], x, xout)
        fw.pop()


class Prog5(Prog4):
    def cm_rows(self, db, i):
        v = db.t[0:TL, :].rearrange("(r w) d -> w r d", w=64)
        return [(32 * j, 32 * j + 32, db, v[4 * i + j]) for j in range(4)]

    def phase_normmod1(self, hT, x1):
        fw, k = self.fw, self.k
        fw.push()
        AB = self.load_AB(1, (0, 1), "gmix1")
        tmp = self.norm_tmp()
        X = [fw.sbuf("X%d" % i, [128, D], F32) for i in range(2)]
        for i in range(NT):
            x = X[i % 2]
            if i < 16:
                for (p0, p1, sb, ap) in self.cm_rows(x1, i):
                    fw.dma(k.q(), x[p0:p1, :], ap, sb, x)
            else:
                fw.dma(k.q(), x[:], x1.t[i * 128:(i + 1) * 128, :], x1, x)
            self.normmod_tile(x, AB, 0 if i < 16 else 1, hT, i * 128, tmp[i % 2])
        fw.pop()

    def phase_hgproj(self, hT):
        fw, k, dr = self.fw, self.k, self.dr
        win = dr["hg_w_in"]
        QT = self.scratch("QT", [8, 128, TL])
        LF = self.scratch("LF", [2, 8, 128, T])
        KT = self.scratch("KT", [2, 8, 128, T])
        sgt = self.scratch("sg_tok", [TL, D])
        vtk = self.scratch("v_tok", [T, D], BF16)
        fw.push()
        lb = fw.sbuf("lb", [128, 8], F32)
        oml = fw.sbuf("oml", [128, 8], F32)
        k.tt(lb[:], self.V("hglb1", 0, 8), self.V("hglb0", 0, 8), ALU.subtract, [self.vec], [lb])
        k.act(lb[:], lb[:], AF.Sigmoid, [lb], [lb])
        k.ts(oml[:], lb[:], -1.0, 1.0, ALU.mult, ALU.add, [lb], [oml])
        W = [fw.sbuf("Wh%d" % i, [128, 8, 512], BF16) for i in range(2)]
        wi = 0
        stf = [fw.sbuf("stf%d" % i, [128, 512], F32) for i in range(2)]
        stb = [fw.sbuf("stb%d" % i, [128, 512], BF16) for i in range(2)]
        si = 0
        for (c0, ntl, isg) in ((1024, 16, True), (1536, 16, True), (2048, NT, False), (2560, NT, False)):
            w = W[wi % 2]; wi += 1
            fw.dma("pool", w[:], _wsrc(win.t, c0, 512), win, w)
            for i in range(ntl):
                P = k.bank()
                for kk in range(8):
                    k.mm(P[:, :], hT[:, kk, i * 128:(i + 1) * 128], w[:, kk, :], kk == 0, kk == 7, [hT, w], P)
                if isg:
                    s = stf[si % 2]; si += 1
                    k.act(s[:], P[:, :], AF.Silu, [P], [s])
                    fw.dma(k.q(), sgt.t[i * 128:(i + 1) * 128, c0 - 1024:c0 - 512], s[:], s, sgt)
                else:
                    s = stb[si % 2]; si += 1
                    k.act(s[:], P[:, :], AF.Copy, [P], [s])
                    fw.dma(k.q(), vtk.t[i * 128:(i + 1) * 128, c0 - 2048:c0 - 1536], s[:], s, vtk)
        blocks = [(0, 512), (512, 512), (1024, 512), (1536, 512), (2048, 256)]
        ob = [fw.sbuf("hob%d" % i, [128, T], F32) for i in range(2)]
        ob2 = [fw.sbuf("hob2%d" % i, [128, T], F32) for i in range(2)]
        sgm = [fw.sbuf("sgm%d" % i, [128, 512], F32) for i in range(2)]
        oi = 0
        for grp in range(6):
            c0 = (0, 512, 3072, 3584, 4096, 4608)[grp]
            w = W[wi % 2]; wi += 1
            fw.dma("pool", w[:], _wsrc(win.t, c0, 512), win, w)
            for hh in range(4):
                h = (grp % 2) * 4 + hh
                o = ob[oi % 2]; o2 = ob2[oi % 2]; oi += 1
                for (t0, n) in (blocks[:4] if grp < 2 else blocks):
                    P = k.bank()
                    for kk in range(8):
                        k.mm(P[:, 0:n], w[:, kk, hh * 128:(hh + 1) * 128], hT[:, kk, t0:t0 + n], kk == 0, kk == 7, [hT, w], P)
                    if grp < 2:
                        k.act(o[:, t0:t0 + n], P[:, 0:n], AF.Silu, [P], [o])
                    else:
                        sg_ = sgm[si % 2]; si += 1
                        k.act(sg_[:, 0:n], P[:, 0:n], AF.Sigmoid, [P], [sg_])
                        k.ts(o[:, t0:t0 + n], sg_[:, 0:n], oml[:, h:h + 1], lb[:, h:h + 1], ALU.mult, ALU.add, [sg_, oml, lb], [o])
                        k.ts(o2[:, t0:t0 + n], o[:, t0:t0 + n], -1.0, 1.0, ALU.mult, ALU.add, [o], [o2])
                if grp < 2:
                    fw.dma(k.q(), QT.t[h], o[:, 0:TL], o, QT)
                else:
                    dd = (grp - 2) // 2
                    fw.dma(k.q(), KT.t[dd, h], o2[:], o2, KT)
                    k.act(o[:], o[:], AF.Ln, [o], [o])
                    fw.dma(k.q(), LF.t[dd, h], o[:], o, LF)
        fw.pop()

    def phase_hgscan(self):
        fw, k, dr = self.fw, self.k, self.dr
        QT, LF, KT, vtk = dr["QT"], dr["LF"], dr["KT"], dr["v_tok"]
        otot = self.scratch("o_tot", [TL, D])
        fw.push()
        NCK = T // 64
        rst = fw.sbuf("rst", [128, T], F32)
        k.memset(rst[:], 1.0, [rst])
        k.memset(rst[:].rearrange("p (c l) -> p c l", l=64)[:, :, 0:1], 0.0, [rst])
        Vh = [fw.sbuf("Vh%d" % i, [64, NCK, 128], BF16) for i in range(1)] * 2
        qb = [fw.sbuf("hq%d" % i, [128, TL], F32) for i in range(1)] * 2
        lf = [fw.sbuf("hlf%d" % i, [128, T], F32) for i in range(1)] * 2
        kt = [fw.sbuf("hkt%d" % i, [128, T], F32) for i in range(1)] * 2
        G = fw.sbuf("hG", [128, T], F32)
        D1 = fw.sbuf("hD1", [128, T], F32)
        D2 = fw.sbuf("hD2", [128, T], F32)
        E = [fw.sbuf("hE%d" % i, [128, T], F32) for i in range(4)]
        qrel = [fw.sbuf("qrel%d" % i, [128, TL], BF16) for i in range(2)]
        krel = [fw.sbuf("krel%d" % i, [128, TL], BF16) for i in range(2)]
        qdec = [fw.sbuf("qdec%d" % i, [128, TL], BF16) for i in range(2)]
        kend = [fw.sbuf("kend%d" % i, [128, T], BF16) for i in range(2)]
        dec = [fw.sbuf("hdec%d" % i, [128, NCK], F32) for i in range(2)]
        S32 = [fw.sbuf("hS32%d" % i, [128, 128], F32) for i in range(2)]
        Sbf = [fw.sbuf("hSbf%d" % i, [128, 128], BF16) for i in range(2)]
        ktok = [fw.sbuf("ktok%d" % i, [64, NCK, 128], BF16) for i in range(2)]
        attT = [fw.sbuf("attT%d" % i, [64, 32, 64], BF16) for i in range(2)]
        Oall = [fw.sbuf("Oall%d" % i, [64, 32, 128], F32) for i in range(2)]
        identb = self.C("ident", b16=True)
        c3 = lambda ap: ap.rearrange("p (c l) -> p c l", l=64)
        for h in range(8):
            V = Vh[h % 2]; q = qb[h % 2]
            fw.dma("sp", V[:], vtk.t[:, h * 128:(h + 1) * 128].rearrange("(c p) v -> p c v", p=64), vtk, V)
            fw.dma("act", q[:], QT.t[h], QT, q)
            for dd in range(2):
                l_, k_ = lf[dd], kt[dd]
                fw.dma("sp", l_[:], LF.t[dd, h], LF, l_)
                fw.dma("act", k_[:], KT.t[dd, h], KT, k_)
                mid, tot = (31, 63) if dd == 0 else (32, 0)
                fw.op("dve", lambda e: e.tensor_tensor_scan(G[:], rst[:], l_[:], 0.0, ALU.mult, ALU.add), [rst, l_], [G])
                if dd == 1:
                    k.tt(D1[:], l_[:], G[:], ALU.subtract, [l_, G], [D1])
                    k.tt(c3(G[:]), c3(D1[:]), bc(c3(G[:])[:, :, 63:64], [128, NCK, 64]), ALU.add, [D1, G], [G])
                Gm = bc(c3(G[:])[:, :, mid:mid + 1], [128, NCK, 64])
                Gt = bc(c3(G[:])[:, :, tot:tot + 1], [128, NCK, 64])
                E0, E1, E2, E3 = E
                k.tt(c3(D1[:]), c3(G[:]), Gm, ALU.subtract, [G], [D1])
                k.tt(c3(D2[:]), Gt, c3(G[:]), ALU.subtract, [G], [D2])
                k.act(E0[:, 0:TL], D1[:, 0:TL], AF.Exp, [D1], [E0])
                k.act(E1[:, 0:TL], D1[:, 0:TL], AF.Exp, [D1], [E1], scale=-1.0)
                k.act(E2[:], D2[:], AF.Exp, [D2], [E2])
                k.act(E3[:, 0:TL], G[:, 0:TL], AF.Exp, [G], [E3])
                k.tt(qrel[dd][:], q[:], E0[:, 0:TL], ALU.mult, [q, E0], [qrel[dd]])
                k.tt(krel[dd][:], k_[:, 0:TL], E1[:, 0:TL], ALU.mult, [k_, E1], [krel[dd]])
                k.tt(kend[dd][:], k_[:], E2[:], ALU.mult, [k_, E2], [kend[dd]])
                k.tt(qdec[dd][:], q[:], E3[:, 0:TL], ALU.mult, [q, E3], [qdec[dd]])
                k.act(dec[dd][:], c3(G[:])[:, :, tot], AF.Exp, [G], [dec[dd]])
                k.memset(S32[dd][:], 0.0, [S32[dd]])
                k.memset(Sbf[dd][:], 0.0, [Sbf[dd]])
                for c0 in range(0, NCK, 8):
                    nb = min(8, NCK - c0)
                    Pt = k.bank()
                    Ptb = Pt[:, :].bitcast(BF16)
                    for j in range(nb):
                        k.tr(Ptb[0:64, j * 128:(j + 1) * 128], kend[dd][:, (c0 + j) * 64:(c0 + j + 1) * 64], identb,
                             [kend[dd], self.conb], Pt, inc=(j == nb - 1), first=(j == 0))
                    k.act(ktok[dd][:, c0:c0 + nb, :].rearrange("p a b -> p (a b)"), Ptb[0:64, 0:nb * 128], AF.Copy, [Pt], [ktok[dd]])
                mask = self.C("mF64" if dd == 0 else "mB64", w=64, rows=64)
                for c0 in range(0, 32, 8):
                    Pa = k.bank()
                    for j in range(8):
                        c = c0 + j
                        k.mm(Pa[0:64, j * 64:(j + 1) * 64], krel[dd][:, c * 64:(c + 1) * 64], qrel[dd][:, c * 64:(c + 1) * 64],
                             True, True, [krel[dd], qrel[dd]], Pa, inc=(j == 7))
                    k.tt(attT[dd][:, c0:c0 + 8, :], Pa[0:64, :].rearrange("p (a b) -> p a b", b=64),
                         bc(mask.unsqueeze(1), [64, 8, 64]), ALU.mult, [Pa, self.con], [attT[dd]])
            orders = [list(range(32, 36)) + list(range(32)), list(range(35, 31, -1)) + list(range(31, -1, -1))]
            Po = [None, None]
            Pcn = [None, None]

            def emit_pc(dd, step):
                c_ = orders[dd][step]
                Pn = k.bank()
                k.mm(Pn[:, 0:128], ktok[dd][:, c_, :], V[:, c_, :], True, True, [ktok[dd], V], Pn)
                Pcn[dd] = Pn
            for dd in range(2):
                emit_pc(dd, 0)
            for step in range(NCK):
                for dd in range(2):
                    c = orders[dd][step]
                    lat = c < 32
                    Pc = Pcn[dd]
                    if step + 1 < NCK:
                        emit_pc(dd, step + 1)
                    if lat:
                        j = c % 4
                        first = (j == 0) if dd == 0 else (j == 3)
                        last_ = (j == 3) if dd == 0 else (j == 0)
                        if first:
                            Po[dd] = k.bank()
                        P = Po[dd]
                        k.mm(P[0:64, j * 128:(j + 1) * 128], attT[dd][:, c, :], V[:, c, :], True, False, [attT[dd], V], P)
                        k.mm(P[0:64, j * 128:(j + 1) * 128], qdec[dd][:, c * 64:(c + 1) * 64], Sbf[dd][:], False, True,
                             [qdec[dd], Sbf[dd]], P, inc=True)
                        if last_:
                            cg = c - j
                            k.act(Oall[dd][:, cg:cg + 4, :].rearrange("p a b -> p (a b)"), P[0:64, :], AF.Copy, [P], [Oall[dd]])
                    k.stt(S32[dd][:], S32[dd][:], dec[dd][:, c:c + 1], Pc[:, 0:128], ALU.mult, ALU.add, [S32[dd], dec[dd], Pc], [S32[dd]])
                    k.cp(Sbf[dd][:], S32[dd][:], [S32[dd]], [Sbf[dd]])
            k.tt(Oall[0][:, 0:16, :], Oall[0][:, 0:16, :], Oall[1][:, 0:16, :], ALU.add, [Oall[0], Oall[1]], [Oall[0]])
            k.tt(Oall[0][:, 16:32, :], Oall[0][:, 16:32, :], Oall[1][:, 16:32, :], ALU.add, [Oall[0], Oall[1]], [Oall[0]])
            for half in range(2):
                fw.dma(k.q(), otot.t[half * 1024:(half + 1) * 1024, h * 128:(h + 1) * 128].rearrange("(c p) v -> p c v", p=64),
                       Oall[0][:, half * 16:(half + 1) * 16, :], Oall[0], otot)
        fw.pop()

    def phase_hgout(self):
        fw, k, dr = self.fw, self.k, self.dr
        otot, sgt = dr["o_tot"], dr["sg_tok"]
        oT2 = self.scratch("oT2", [8, 128, T], BF16)
        fw.push()
        gb = fw.sbuf("hgng", [128, 128], F32)
        fw.dma("sp", gb[:], self.rowbc("hgng", 128), dr["rows"], gb)
        O = [fw.sbuf("hO%d" % i, [128, 8, 128], F32) for i in range(2)]
        SG = [fw.sbuf("hSG%d" % i, [128, 8, 128], F32) for i in range(2)]
        sq = fw.sbuf("hsq", [128, 8, 128], F32)
        ss = fw.sbuf("hss", [128, 8], F32)
        on = fw.sbuf("hon", [128, 8, 128], BF16)
        stg = [fw.sbuf("hstg%d" % i, [128, 8, 128], BF16) for i in range(2)]
        identb = self.C("ident", b16=True)
        f2 = lambda b_: b_[:].rearrange("p a b -> p (a b)")
        for i in range(16):
            o, sg = O[i % 2], SG[i % 2]
            fw.dma("sp", f2(o), otot.t[i * 128:(i + 1) * 128, :], otot, o)
            fw.dma("act", f2(sg), sgt.t[i * 128:(i + 1) * 128, :], sgt, sg)
            k.tt(sq[:], o[:], o[:], ALU.mult, [o], [sq])
            fw.op("dve", lambda e: e.tensor_reduce(ss[:], sq[:], AX.X, ALU.add), [sq], [ss])
            k.act(ss[:], ss[:], AF.Sqrt, [ss], [ss], scale=1.0 / 128, bias=self.epsb[:, 0:1])
            fw.op("dve", lambda e: e.reciprocal(ss[:], ss[:]), [ss], [ss])
            k.tt(o[:], o[:], bc(ss[:].unsqueeze(2), [128, 8, 128]), ALU.mult, [o, ss], [o])
            k.tt(o[:], o[:], bc(gb[:].unsqueeze(1), [128, 8, 128]), ALU.mult, [o, gb], [o])
            k.tt(on[:], o[:], sg[:], ALU.mult, [o, sg], [on])
            P = k.bank()
            Pb = P[:, :].bitcast(BF16)
            for c in range(8):
                k.tr(Pb[:, c * 128:(c + 1) * 128], on[:, c, :], identb, [on, self.conb], P, inc=(c == 7), first=(c == 0))
            s = stg[i % 2]
            k.act(f2(s), Pb, AF.Copy, [P], [s])
            fw.dma("sp", oT2.t[:, :, i * 128:(i + 1) * 128].rearrange("c p t -> p c t"), s[:], s, oT2)
        fw.pop()

    def build_full(self):
        fw, dr = self.fw, self.dr
        self.declare_io()
        out = self.scratch("out", [TL, D], out=True)
        self.setup_consts()
        self.phase_mod(0)
        fw.push()
        hT = fw.sbuf("hT", [128, 8, T], BF16)
        self.phase_normmod0(hT)
        self.phase_evenproj(hT)
        fw.pop()
        self.phase_ssd()
        self.phase_conformer()
        fw.push()
        hT2 = fw.sbuf("hT2", [128, 8, T], BF16)
        xsrc0 = lambda i: [(0, 128, dr["x"], dr["x"].t[i * 128:(i + 1) * 128, :])] if i < 16 else \
            [(0, 128, dr["ctx"], dr["ctx"].t[(i - 16) * 128:(i - 15) * 128, :])]
        self.phase_outproj(0, dr["oT"], 16, "ab_w_out", xsrc0, hT2, "xmid0", list(range(NT)))
        x1 = self.scratch("x1", [T, D])
        self.phase_moe(0, hT2, "xmid0", x1, list(range(NT)),
                       hook=(lambda: self.mod_setup(1), self.mod_group, self.mod_finish))
        fw.pop()
        fw.push()
        hT = fw.sbuf("hTb", [128, 8, T], BF16)
        self.phase_normmod1(hT, x1)
        self.phase_hgproj(hT)
        fw.pop()
        self.phase_hgscan()
        self.phase_hgout()
        fw.push()
        hT2 = fw.sbuf("hT2b", [128, 8, T], BF16)
        self.phase_outproj(1, dr["oT2"], 8, "hg_w_out", lambda i: self.cm_rows(x1, i), hT2, "xmid1", list(range(16)))
        self.phase_moe(1, hT2, "xmid1", out, list(range(16)), final=True)
        fw.pop()
        fw.barrier()
        st = simulate(fw)
        assert not st, "sync deadlock: %r" % (st,)
        return self.nc


_CACHE = {}


def kernel(**inputs):
    n = 8
    if "nc" not in _CACHE:
        _CACHE["nc"] = Prog5().build_full()
    nc = _CACHE["nc"]
    maps = make_inmaps(inputs, list(range(n)))
    res = run_bass_kernel_spmd(nc, maps, core_ids=list(range(n)))
    return np.stack([np.asarray(r["out"], np.float32) for r in res.results], axis=0)
```
